# Optimizing a Trainium2 kernel written in Bass

```python
import math
import jax, jax.numpy as jnp
from jax import lax
import numpy as np

D_MODEL = 2048
BATCH = 4
SEQ = 2048
DEPTH = 1

CTX_LEN = 256
GRID_W = 64
MIX_WIDTH = D_MODEL
POOL_WIDTH = MIX_WIDTH // 4
POOL_WINDOWS = (2, 4, 8, 16)
POOL_GROUPS = len(POOL_WINDOWS)
POOL_GROUP_DIM = POOL_WIDTH // POOL_GROUPS
ATTN_WIDTH = MIX_WIDTH - POOL_WIDTH
DIFF_VDIM = 128
DIFF_HEADS = ATTN_WIDTH // DIFF_VDIM
DIFF_QKDIM = DIFF_VDIM // 2
IN_WIDTH = POOL_WIDTH + 3 * ATTN_WIDTH
Q_BLOCK = 128
ROPE_BASE = 10000.0
ROPE_AXIS_DIM = DIFF_QKDIM // 2
PEER_HEADS = 8
PEER_NKEYS = 128
PEER_EXPERTS = PEER_NKEYS * PEER_NKEYS
PEER_QDIM = 256
PEER_HALF = PEER_QDIM // 2
PEER_TOPK = 16
TOKEN_BLOCK = 128
EPS = 1e-6

kernel_name = "hybrid_pool_diffattn_peer_dit_block"


def rmsnorm(x, g):
    xf = x.astype(jnp.float32)
    y = xf * lax.rsqrt(jnp.mean(xf * xf, axis=-1, keepdims=True) + EPS)
    return (y * g.astype(jnp.float32)).astype(x.dtype)


def modulate(h, shift, scale):
    return h * (1 + scale) + shift


def lambda_init(layer_idx):
    return 0.8 - 0.6 * math.exp(-0.3 * layer_idx)


def axial_rope_tables(L):
    n_rows = L // GRID_W
    row = jnp.repeat(jnp.arange(n_rows, dtype=jnp.float32), GRID_W, total_repeat_length=L)
    col = jnp.tile(jnp.arange(GRID_W, dtype=jnp.float32), n_rows)
    half = ROPE_AXIS_DIM // 2
    inv_freq = ROPE_BASE ** (-jnp.arange(half, dtype=jnp.float32) / half)
    ang = jnp.stack([row, col], axis=-1)[:, :, None] * inv_freq
    return jnp.cos(ang), jnp.sin(ang)


def apply_axial_rope(x, cos, sin):
    half = ROPE_AXIS_DIM // 2
    xf = x.astype(jnp.float32).reshape(x.shape[:-1] + (2, 2, half))
    x1, x2 = xf[..., 0, :], xf[..., 1, :]
    c = cos[:, None, None, :, :]
    s = sin[:, None, None, :, :]
    out = jnp.stack([x1 * c - x2 * s, x1 * s + x2 * c], axis=-2)
    return out.reshape(x.shape).astype(x.dtype)


def diff_attend(q, k, v, lam):
    s = jnp.einsum('bqhmd,bkhmd->bhmqk', q, k, preferred_element_type=jnp.float32)
    a = jax.nn.softmax(s * (DIFF_QKDIM ** -0.5), axis=-1)
    a = a[:, :, 0] - lam * a[:, :, 1]
    return jnp.einsum('bhqk,bkhv->bqhv', a.astype(v.dtype), v)


def diff_head_norm(o, subln_g, lam_init):
    B, L = o.shape[0], o.shape[1]
    return (rmsnorm(o, subln_g) * (1.0 - lam_init)).reshape(B, L, ATTN_WIDTH)


def pool_mix(z, pool_w, pool_b, pool_scale):
    B, L = z.shape[0], z.shape[1]
    zg = z.reshape(B, L, POOL_GROUPS, POOL_GROUP_DIM)
    cs = jnp.concatenate([jnp.zeros((B, 1, POOL_GROUPS, POOL_GROUP_DIM), jnp.float32),
                          jnp.cumsum(zg.astype(jnp.float32), axis=1)], axis=1)
    t = jnp.arange(L)
    means = []
    for g, w in enumerate(POOL_WINDOWS):
        lo = jnp.clip(t - w // 2, 0, L)
        hi = jnp.clip(t + w // 2, 0, L)
        total = cs[:, hi, g] - cs[:, lo, g]
        means.append(total / (hi - lo).astype(jnp.float32)[:, None])
    pooled = jnp.stack(means, axis=2)
    y = (pooled - zg.astype(jnp.float32)).astype(z.dtype)
    y = jnp.einsum('blgc,gcd->blgd', y, pool_w) + pool_b
    return y.reshape(B, L, POOL_WIDTH) * pool_scale


def peer(h, peer_wq, peer_keys, peer_u, peer_v):
    B, L, D = h.shape
    hb_all = h.reshape((B * L) // TOKEN_BLOCK, TOKEN_BLOCK, D)

    def block_fn(hb):
        q = (hb @ peer_wq).reshape(TOKEN_BLOCK, PEER_HEADS, 2, PEER_HALF)
        s = jnp.einsum('thpc,hpkc->thpk', q, peer_keys, preferred_element_type=jnp.float32)
        va, ia = lax.top_k(s[:, :, 0], PEER_TOPK)
        vb, ib = lax.top_k(s[:, :, 1], PEER_TOPK)
        cand = (va[..., :, None] + vb[..., None, :]).reshape(TOKEN_BLOCK, PEER_HEADS, PEER_TOPK * PEER_TOPK)
        cand_idx = (ia[..., :, None] * PEER_NKEYS + ib[..., None, :]).reshape(TOKEN_BLOCK, PEER_HEADS, PEER_TOPK * PEER_TOPK)
        top_s, pos = lax.top_k(cand, PEER_TOPK)
        eidx = jnp.take_along_axis(cand_idx, pos, axis=-1)
        gate = jax.nn.softmax(top_s, axis=-1).astype(h.dtype)
        u = peer_u[eidx]
        act = jax.nn.gelu(jnp.einsum('thkd,td->thk', u, hb), approximate=False)
        v = peer_v[eidx]
        return jnp.einsum('thk,thkd->td', gate * act, v)

    return lax.map(block_fn, hb_all).reshape(B, L, D)


def setup_inputs(seed: int = 0) -> dict:
    key = jax.random.key(seed)
    ks = jax.random.split(key, 24)
    f32 = jnp.float32
    nrm = lambda k, shape, s: jax.random.normal(k, shape, f32) * s
    return {
        "x": nrm(ks[0], (BATCH, SEQ, D_MODEL), 1.0),
        "c": nrm(ks[1], (BATCH, D_MODEL), 1.0),
        "ctx": nrm(ks[2], (BATCH, CTX_LEN, D_MODEL), 1.0),
        "c_ctx": nrm(ks[3], (D_MODEL,), 1.0),
        "ada_w": nrm(ks[4], (DEPTH, D_MODEL, 6 * D_MODEL), 0.5 * D_MODEL ** -0.5),
        "ada_b": nrm(ks[5], (DEPTH, 6 * D_MODEL), 0.02),
        "norm1_g": 1.0 + nrm(ks[6], (DEPTH, D_MODEL), 0.02),
        "w_in": nrm(ks[7], (DEPTH, D_MODEL, IN_WIDTH), D_MODEL ** -0.5),
        "pool_w": nrm(ks[8], (DEPTH, POOL_GROUPS, POOL_GROUP_DIM, POOL_GROUP_DIM), POOL_GROUP_DIM ** -0.5),
        "pool_b": nrm(ks[9], (DEPTH, POOL_GROUPS, POOL_GROUP_DIM), 0.02),
        "pool_scale": 1.0 + nrm(ks[10], (DEPTH, POOL_WIDTH), 0.02),
        "diff_lambda": nrm(ks[11], (DEPTH, 4, DIFF_QKDIM), 0.1),
        "subln_g": 1.0 + nrm(ks[12], (DEPTH, DIFF_VDIM), 0.02),
        "w_out": nrm(ks[13], (DEPTH, MIX_WIDTH, D_MODEL), MIX_WIDTH ** -0.5),
        "norm2_g": 1.0 + nrm(ks[14], (DEPTH, D_MODEL), 0.02),
        "peer_wq": nrm(ks[15], (DEPTH, D_MODEL, PEER_HEADS * PEER_QDIM), D_MODEL ** -0.5),
        "peer_keys": nrm(ks[16], (DEPTH, PEER_HEADS, 2, PEER_NKEYS, PEER_HALF), PEER_HALF ** -0.5),
        "peer_u": nrm(ks[17], (DEPTH, PEER_EXPERTS, D_MODEL), D_MODEL ** -0.5),
        "peer_v": nrm(ks[18], (DEPTH, PEER_EXPERTS, D_MODEL), 0.5),
        "final_g": 1.0 + nrm(ks[19], (D_MODEL,), 0.02),
    }


def reference(x, c, ctx, c_ctx, ada_w, ada_b, norm1_g, w_in, pool_w, pool_b, pool_scale,
              diff_lambda, subln_g, w_out, norm2_g, peer_wq, peer_keys, peer_u, peer_v, final_g):
    B, L, D = x.shape
    C = ctx.shape[1]
    n_blocks = L // Q_BLOCK
    cos, sin = axial_rope_tables(L)
    split_pts = [POOL_WIDTH, POOL_WIDTH + ATTN_WIDTH, POOL_WIDTH + 2 * ATTN_WIDTH]

    def project(h, l):
        z_pool, z_q, z_k, z_v = jnp.split(h @ w_in[l], split_pts, axis=-1)
        Bh, Lh = h.shape[0], h.shape[1]
        q = z_q.reshape(Bh, Lh, DIFF_HEADS, 2, DIFF_QKDIM)
        k = z_k.reshape(Bh, Lh, DIFF_HEADS, 2, DIFF_QKDIM)
        v = z_v.reshape(Bh, Lh, DIFF_HEADS, DIFF_VDIM)
        return z_pool, q, k, v

    for l in range(DEPTH):
        last = l == DEPTH - 1
        lam_init = lambda_init(l)
        lq = diff_lambda[l].astype(jnp.float32)
        lam = jnp.exp(jnp.sum(lq[0] * lq[1])) - jnp.exp(jnp.sum(lq[2] * lq[3])) + lam_init

        mod_lat = jax.nn.silu(c) @ ada_w[l] + ada_b[l]
        mod_ctx = jax.nn.silu(c_ctx) @ ada_w[l] + ada_b[l]
        sh1, sc1, g1, sh2, sc2, g2 = [m[:, None, :] for m in jnp.split(mod_lat, 6, axis=-1)]
        csh1, csc1, cg1, csh2, csc2, cg2 = jnp.split(mod_ctx, 6, axis=-1)

        h_lat = modulate(rmsnorm(x, norm1_g[l]), sh1, sc1)
        h_ctx = modulate(rmsnorm(ctx, norm1_g[l]), csh1, csc1)
        zp_lat, q_lat, k_lat, v_lat = project(h_lat, l)
        zp_ctx, q_ctx, k_ctx, v_ctx = project(h_ctx, l)
        q_lat = apply_axial_rope(q_lat, cos, sin)
        k_lat = apply_axial_rope(k_lat, cos, sin)
        k_all = jnp.concatenate([k_ctx, k_lat], axis=1)
        v_all = jnp.concatenate([v_ctx, v_lat], axis=1)

        q_blocks = q_lat.reshape(B, n_blocks, Q_BLOCK, DIFF_HEADS, 2, DIFF_QKDIM).transpose(1, 0, 2, 3, 4, 5)
        o_lat = lax.map(lambda qb: diff_attend(qb, k_all, v_all, lam), q_blocks)
        o_lat = o_lat.transpose(1, 0, 2, 3, 4).reshape(B, L, DIFF_HEADS, DIFF_VDIM)
        attn_lat = diff_head_norm(o_lat, subln_g[l], lam_init)
        pool_lat = pool_mix(zp_lat, pool_w[l], pool_b[l], pool_scale[l])
        x = x + g1 * (jnp.concatenate([pool_lat, attn_lat], axis=-1) @ w_out[l])

        if not last:
            o_ctx = diff_attend(q_ctx, k_ctx, v_ctx, lam)
            attn_ctx = diff_head_norm(o_ctx, subln_g[l], lam_init)
            pool_ctx = pool_mix(zp_ctx, pool_w[l], pool_b[l], pool_scale[l])
            ctx = ctx + cg1 * (jnp.concatenate([pool_ctx, attn_ctx], axis=-1) @ w_out[l])

        f_lat = modulate(rmsnorm(x, norm2_g[l]), sh2, sc2)
        x = x + g2 * peer(f_lat, peer_wq[l], peer_keys[l], peer_u[l], peer_v[l])
        if not last:
            f_ctx = modulate(rmsnorm(ctx, norm2_g[l]), csh2, csc2)
            ctx = ctx + cg2 * peer(f_ctx, peer_wq[l], peer_keys[l], peer_u[l], peer_v[l])

    return rmsnorm(x, final_g)
```

```python
import math
import numpy as np
import concourse.bass as bass
import concourse.mybir as mybir
from concourse.bass_utils import run_bass_kernel_spmd
from contextlib import ExitStack

F32 = mybir.dt.float32
BF16 = mybir.dt.bfloat16
I32 = mybir.dt.int32
U32 = mybir.dt.uint32
AF = mybir.ActivationFunctionType
ALU = mybir.AluOpType
AX = mybir.AxisListType

D = 2048
NT = 2304
NQ = 1024
NE = 16384
EPS = 1e-6
LAM_INIT = 0.8 - 0.6 * math.exp(0.0)


class Buf:
    __slots__ = ("name", "w", "r")

    def __init__(self, name=""):
        self.name = name
        self.w = None
        self.r = []


class Prog:
    CE = ("act", "dve", "pool", "pe")
    NDS = 12

    def __init__(self, nc, stack):
        self.nc = nc
        self.eng = {"sp": nc.sync, "act": nc.scalar, "dve": nc.vector, "pool": nc.gpsimd, "pe": nc.tensor}
        self.cnt = {e: 0 for e in self.CE}
        self.sem = {("c", e): stack.enter_context(nc.semaphore("c_" + e)) for e in self.CE}
        self.dcnt = {}
        self.dnext = {}
        for q in ("sp", "act", "pool"):
            for i in range(self.NDS):
                self.sem[("d", q, i)] = stack.enter_context(nc.semaphore("d_%s%d" % (q, i)))
                self.dcnt[(q, i)] = 0
            self.dnext[q] = 0
        self.known = {e: {} for e in self.eng}
        self.nops = 0

    def _deps(self, eng, reads, writes):
        deps = {}

        def add(ev):
            if ev is None:
                return
            k, v = ev
            if eng == "pe" and k == ("c", "pe"):
                return
            if deps.get(k, 0) < v:
                deps[k] = v
        for b in reads:
            add(b.w)
        for b in writes:
            add(b.w)
            for ev in b.r:
                add(ev)
        out = []
        kn = self.known[eng]
        for k, v in deps.items():
            if kn.get(k, 0) < v:
                kn[k] = v
                out.append((k, v))
        return out

    def _commit(self, ev, reads, writes):
        for b in reads:
            b.r.append(ev)
        for b in writes:
            b.w = ev
            b.r = []

    def _issue(self, eng, waits, fn, inc):
        e = self.eng[eng]
        for k, v in waits:
            e.wait_ge(self.sem[k], v)
        if fn is not None:
            fn(e).then_inc(self.sem[inc[0]], inc[1])
        self.nops += 1

    def op(self, eng, fn, reads=(), writes=()):
        waits = self._deps(eng, reads, writes)
        self.cnt[eng] += 1
        ev = (("c", eng), self.cnt[eng])
        self._commit(ev, reads, writes)
        self._issue(eng, waits, fn, (("c", eng), 1))
        return ev

    def dma(self, q, fn, reads=(), writes=()):
        waits = self._deps(q, reads, writes)
        i = self.dnext[q]
        self.dnext[q] = (i + 1) % self.NDS
        k = ("d", q, i)
        prev = self.dcnt[(q, i)]
        if prev > 0 and self.known[q].get(k, 0) < prev:
            self.known[q][k] = prev
            waits.append((k, prev))
        self.dcnt[(q, i)] = prev + 16
        ev = (k, prev + 16)
        self._commit(ev, reads, writes)
        self._issue(q, waits, fn, (k, 16))
        return ev

    def _all(self):
        waits = [(("d", q, i), v) for (q, i), v in self.dcnt.items() if v > 0]
        waits += [(("c", e), self.cnt[e]) for e in self.CE if self.cnt[e] > 0]
        return waits

    def barrier(self):
        allw = self._all()
        for eng in self.eng:
            kn = self.known[eng]
            w = []
            for k, v in allw:
                if kn.get(k, 0) < v:
                    kn[k] = v
                    w.append((k, v))
            self._issue(eng, w, None, None)

    def finish(self):
        self.barrier()


def build(stop=None, dbg=()):
    nc = bass.Bass("TRN2", target_bir_lowering=False)
    din = lambda n, s, dt=F32: nc.dram_tensor(n, list(s), dt, kind="ExternalInput").ap()
    xT = din("xT", [D, NT])
    cvec = din("cvec", [128, 32])
    ada_w = din("ada_w", [D, 6 * D])
    smallv = din("smallv", [128, 96 + 48 + 8 + 2])
    w_in = din("w_in", [D, 5120])
    pool_w = din("pool_w", [4, 128, 128])
    dlam = din("dlam", [128, 256])
    subg = din("subg", [128, 128])
    w_out = din("w_out", [D, D])
    wq = din("wq", [D, D])
    keysT = din("keysT", [16, 128, 128])
    uT = din("uT", [D, NE])
    pv = din("pv", [NE, D])
    ropeC = din("ropeC", [128, 2048])
    ropeS = din("ropeS", [128, 2048])
    cmat = din("cmat", [128, 256])
    icnt = din("icnt", [128, 4096])
    emat = din("emat", [128, 2, 4096])
    outT = nc.dram_tensor("outT", [D, NQ], F32, kind="ExternalOutput").ap()
    kT_s = nc.dram_tensor("kT_s", [12, 128, NT], BF16, kind="Internal").ap()
    qT_s = nc.dram_tensor("qT_s", [12, 128, NQ], BF16, kind="Internal").ap()
    V_s = nc.dram_tensor("V_s", [NT, 1536], BF16, kind="Internal").ap()
    Gd = nc.dram_tensor("Gd", [128, 128, NQ], BF16, kind="Internal").ap()
    x1_s = nc.dram_tensor("x1_s", [128, 16, NQ], F32, kind="Internal").ap()
    dbg_out = {}

    st = ExitStack()
    P = Prog(nc, st)
    off = [16384]

    def T(name, shape, dt, at=None):
        n = 1
        for s in shape[1:]:
            n *= s
        nb = n * (2 if dt == BF16 else 4)
        nb = (nb + 63) // 64 * 64
        if at is None:
            at = off[0]
            off[0] = at + nb
        assert at + nb <= 229000, (name, at, nb)
        return nc.alloc_sbuf_tensor_at(name, list(shape), dt, offset=at)

    def dump(name, ap, buf, shape, dt=F32):
        d = nc.dram_tensor("dbg_" + name, list(shape), dt, kind="ExternalOutput").ap()
        dbg_out[name] = d
        P.dma("sp", lambda e: e.dma_start(out=d, in_=ap), reads=buf)

    psum = nc.alloc_psum_tensor("psum", [128, 8, 512], F32)
    Bps = [Buf("ps%d" % i) for i in range(8)]

    def end():
        P.finish()
        st.close()
        return nc, dbg_out

    smv = T("smv", [128, 154], F32)
    cm = T("cm", [128, 256], F32)
    cvs = T("cvs", [128, 32], F32)
    dl = T("dl", [128, 256], F32)
    sgb = T("sgb", [128, 128], F32)
    identb = T("identb", [128, 128], BF16)
    pmb = T("pmb", [128, 128], BF16)
    onesb = T("onesb", [128, 128], BF16)
    modT = T("modT", [128, 96, 2], F32)
    gs1 = T("gs1", [128, 16, 2], F32)
    gs2 = T("gs2", [128, 16], F32)
    lamt = T("lamt", [128, 8], F32)
    iota_i = T("iota_i", [128, 128], I32)
    iota_f = T("iota_f", [128, 128], F32)
    Bc = Buf("consts")
    for t, d in ((smv, smallv), (cm, cmat), (cvs, cvec), (dl, dlam), (sgb, subg)):
        P.dma("sp", lambda e, t=t, d=d: e.dma_start(out=t[:], in_=d), writes=[Bc])
    identf = cm[:, 0:128]
    P.op("dve", lambda e: e.tensor_copy(out=identb[:], in_=cm[:, 0:128]), reads=[Bc], writes=[Bc])
    P.op("dve", lambda e: e.tensor_copy(out=pmb[:], in_=cm[:, 128:256]), reads=[Bc], writes=[Bc])
    P.op("dve", lambda e: e.memset(onesb[:], 1.0), writes=[Bc])
    epsb = T("epsb", [128, 1], F32)
    P.op("dve", lambda e: e.memset(epsb[:], EPS), writes=[Bc])
    P.op("pool", lambda e: e.iota(iota_i[:], pattern=[[1, 128]], base=0, channel_multiplier=0), writes=[Bc])
    P.op("dve", lambda e: e.tensor_copy(out=iota_f[:], in_=iota_i[:]), reads=[Bc], writes=[Bc])
    P.op("dve", lambda e: e.tensor_scalar_mul(out=sgb[:], in0=sgb[:], scalar1=1.0 - LAM_INIT), writes=[Bc])
    dlv = dl[:].rearrange("p (a b c) -> p a b c", a=2, b=2)
    prod = T("prod", [128, 2, 64], F32)
    P.op("dve", lambda e: e.tensor_tensor(out=prod[:], in0=dlv[:, :, 0, :], in1=dlv[:, :, 1, :], op=ALU.mult), writes=[Bc])
    P.op("dve", lambda e: e.tensor_reduce(out=lamt[:, 0:2], in_=prod[:], axis=AX.X, op=ALU.add), writes=[Bc])
    P.op("act", lambda e: e.activation(out=lamt[:, 2:4], in_=lamt[:, 0:2], func=AF.Exp), writes=[Bc])
    P.op("dve", lambda e: e.tensor_tensor(out=lamt[:, 4:5], in0=lamt[:, 3:4], in1=lamt[:, 2:3], op=ALU.subtract), writes=[Bc])
    P.op("dve", lambda e: e.tensor_scalar_add(out=lamt[:, 4:5], in0=lamt[:, 4:5], scalar1=-LAM_INIT), writes=[Bc])
    nlam = lamt[:, 4:5]
    adab = smv[:, 0:96]
    n1g = smv[:, 96:112]
    n2g = smv[:, 112:128]
    fg = smv[:, 128:144]
    poolb = smv[:, 144:148]
    pools = smv[:, 148:152]
    halo = smv[:, 152:154]
    P.op("act", lambda e: e.activation(out=cvs[:], in_=cvs[:], func=AF.Silu), writes=[Bc])
    base0 = off[0]
    R0 = base0
    R1 = R0 + 72 * 1024
    R2 = R1 + 32 * 1024

    off[0] = R1
    aw = [T("aw%d" % i, [128, 16, 512], F32) for i in range(2)]
    Baw = [Buf(), Buf()]
    modrow = T("modrow", [2, 6 * D], F32)
    Bmr = Buf()
    awv = ada_w.rearrange("(kc p) n -> p kc n", p=128)
    csv = cvs[:].rearrange("p (k r) -> p k r", r=2)
    for nt in range(24):
        b = nt % 2
        bank = nt % 4
        P.dma("sp", lambda e, nt=nt, b=b: e.dma_start(out=aw[b][:], in_=awv[:, :, nt * 512:(nt + 1) * 512]), writes=[Baw[b]])
        for kc in range(16):
            P.op("pe", lambda e, kc=kc, b=b, bank=bank: e.matmul(
                psum[0:2, bank, :], lhsT=csv[:, kc, :], rhs=aw[b][:, kc, :],
                start=(kc == 0), stop=(kc == 15)), reads=[Baw[b], Bc], writes=[Bps[bank]])
        P.op("act", lambda e, nt=nt, bank=bank: e.activation(out=modrow[:, nt * 512:(nt + 1) * 512], in_=psum[0:2, bank, :], func=AF.Copy),
             writes=[Bps[bank], Bmr])
    for j in range(96):
        P.op("pe", lambda e, j=j: e.transpose(out=psum[:, 4, 2 * j:2 * j + 2], in_=modrow[:, j * 128:(j + 1) * 128], identity=cm[0:2, 0:2]),
             reads=[Bmr, Bc], writes=[Bps[4]])
    P.op("dve", lambda e: e.tensor_tensor(
        out=modT[:], in0=psum[:, 4, 0:192].rearrange("p (j r) -> p j r", r=2),
        in1=adab.unsqueeze(2).to_broadcast([128, 96, 2]), op=ALU.add), reads=[Bc], writes=[Bps[4], Bc])
    P.op("dve", lambda e: e.tensor_scalar_add(out=gs1[:], in0=modT[:, 16:32, :], scalar1=1.0), writes=[Bc])
    P.op("dve", lambda e: e.tensor_tensor(out=gs1[:], in0=gs1[:], in1=n1g.unsqueeze(2).to_broadcast([128, 16, 2]), op=ALU.mult), writes=[Bc])
    P.op("dve", lambda e: e.tensor_scalar_add(out=gs2[:], in0=modT[:, 64:80, 0], scalar1=1.0), writes=[Bc])
    P.op("dve", lambda e: e.tensor_tensor(out=gs2[:], in0=gs2[:], in1=n2g, op=ALU.mult), writes=[Bc])
    sh1 = modT[:, 0:16, :]
    g1 = modT[:, 32:48, 0]
    sh2 = modT[:, 48:64, 0]
    g2 = modT[:, 80:96, 0]
    if "mod" in dbg:
        dump("mod", modT[:], [Bc], [128, 96, 2])
        dump("lam", lamt[:], [Bc], [128, 8])
    P.barrier()
    if stop == "A":
        return end()

    off[0] = R0
    hT = T("hT", [128, 16, NT], BF16)
    BhT = [Buf() for _ in range(5)]
    off[0] = R1
    xt = [T("xt%d" % i, [128, 16, 512], F32) for i in range(2)]
    Bxt = [[Buf() for _ in range(16)] for _ in range(2)]
    sq = T("sq", [128, 16, 512], BF16)
    Bsq = Buf()
    rstd = T("rstd", [128, 1024], F32)
    Brstd = Buf()
    xTv = xT.rearrange("(kc p) t -> p kc t", p=128)

    def rms_stats(src, Bsrc, n, ps_bank, rs_ap):
        P.op("act", lambda e: e.activation(out=sq[:, :, 0:n], in_=src, func=AF.Square), reads=Bsrc, writes=[Bsq])
        for kc in range(16):
            P.op("pe", lambda e, kc=kc: e.matmul(psum[:, ps_bank, 0:n], lhsT=onesb[:], rhs=sq[:, kc, 0:n],
                                                 start=(kc == 0), stop=(kc == 15)), reads=[Bsq, Bc], writes=[Bps[ps_bank]])
        P.op("act", lambda e: e.activation(out=rs_ap, in_=psum[:, ps_bank, 0:n], func=AF.Sqrt, bias=epsb[:], scale=1.0 / D),
             reads=[Bc], writes=[Bps[ps_bank], Brstd])
        P.op("dve", lambda e: e.reciprocal(out=rs_ap, in_=rs_ap), writes=[Brstd])

    for g in range(5):
        b = g % 2
        n = 512 if g < 4 else 256
        t0 = g * 512
        r = 0 if g < 4 else 1
        P.dma("sp", lambda e, b=b, t0=t0, n=n: e.dma_start(out=xt[b][:, :, 0:n], in_=xTv[:, :, t0:t0 + n]), writes=Bxt[b])
        rms_stats(xt[b][:, :, 0:n], Bxt[b], n, g % 2, rstd[:, 0:n])
        for kc in range(16):
            P.op("dve", lambda e, b=b, kc=kc, n=n, r=r: e.scalar_tensor_tensor(
                out=xt[b][:, kc, 0:n], in0=xt[b][:, kc, 0:n], scalar=gs1[:, kc, r:r + 1], in1=rstd[:, 0:n],
                op0=ALU.mult, op1=ALU.mult), reads=[Brstd, Bc], writes=[Bxt[b][kc]])
            P.op("act", lambda e, b=b, kc=kc, n=n, r=r, t0=t0: e.activation(
                out=hT[:, kc, t0:t0 + n], in_=xt[b][:, kc, 0:n], func=AF.Identity, bias=sh1[:, kc, r:r + 1], scale=1.0),
                reads=[Bxt[b][kc], Bc], writes=[BhT[g]])
    if "h" in dbg:
        dump("h", hT[:, :, 0:512], BhT, [128, 16, 512], BF16)
    P.barrier()
    if stop == "B":
        return end()

    off[0] = R1
    mixT = T("mixT", [128, 16, NQ], BF16)
    BmixT = [Buf() for _ in range(16)]
    off[0] = R2
    wt = [T("wt%d" % i, [128, 16, 512], BF16) for i in range(2)]
    Bwt = [Buf(), Buf()]
    rC = T("rC", [128, 2048], F32)
    rS = T("rS", [128, 2048], F32)
    Brope = Buf()
    P.dma("sp", lambda e: e.dma_start(out=rC[:], in_=ropeC), writes=[Brope])
    P.dma("sp", lambda e: e.dma_start(out=rS[:], in_=ropeS), writes=[Brope])
    wv = w_in.rearrange("(kc p) n -> p kc n", p=128)
    wcnt = [0]

    worder = [3584, 4096, 4608] + [c for hg in range(3) for c in (512 + hg * 512, 2048 + hg * 512)] + [0]

    def issue_w(i):
        if i < len(worder):
            c0 = worder[i]
            P.dma("pool", lambda e: e.dma_start(out=wt[i % 2][:], in_=wv[:, :, c0:c0 + 512]), writes=[Bwt[i % 2]])

    issue_w(0)

    def load_w(c0):
        i = wcnt[0]
        assert worder[i] == c0
        wcnt[0] += 1
        issue_w(i + 1)
        return i % 2

    NSTG = 8
    stg = [T("stg%d" % i, [128, 512], BF16) for i in range(NSTG)]
    Bstg = [Buf() for _ in range(NSTG)]
    scnt = [0]
    pcnt = [0]
    V_sv = V_s.rearrange("(tt p) c -> p tt c", p=128)
    for vc in range(3):
        b = load_w(3584 + vc * 512)
        for tt in range(18):
            bank = pcnt[0] % 4
            pcnt[0] += 1
            for kc in range(16):
                P.op("pe", lambda e, kc=kc, tt=tt, bank=bank, b=b: e.matmul(
                    psum[:, bank, :], lhsT=hT[:, kc, tt * 128:(tt + 1) * 128], rhs=wt[b][:, kc, :],
                    start=(kc == 0), stop=(kc == 15)), reads=[Bwt[b]] + BhT, writes=[Bps[bank]])
            s = scnt[0] % NSTG
            scnt[0] += 1
            P.op("act", lambda e, s=s, bank=bank: e.activation(out=stg[s][:], in_=psum[:, bank, :], func=AF.Copy),
                 writes=[Bps[bank], Bstg[s]])
            P.dma("sp", lambda e, s=s, tt=tt, vc=vc: e.dma_start(out=V_sv[:, tt, vc * 512:(vc + 1) * 512], in_=stg[s][:]),
                  reads=[Bstg[s]])
    t1 = [T("t1_%d" % i, [128, 512], F32) for i in range(2)]
    t2 = [T("t2_%d" % i, [128, 512], F32) for i in range(2)]
    Bt1 = [Buf(), Buf()]
    Bt2 = [Buf(), Buf()]
    rcnt = [0]

    def c2_tail(s, bank, t0, n, dst):
        if t0 >= 2048:
            P.dma("sp", lambda e: e.dma_start(out=dst, in_=stg[s][:, 0:n]), reads=[Bstg[s]])
            return
        rb = 4 + (rcnt[0] % 2)
        ri = rcnt[0] % 2
        rcnt[0] += 1
        P.op("pe", lambda e: e.matmul(psum[:, rb, :], lhsT=pmb[:], rhs=stg[s][:], start=True, stop=True),
             reads=[Bstg[s], Bc], writes=[Bps[rb]])
        P.op("dve", lambda e: e.tensor_tensor(out=t1[ri][:], in0=psum[:, bank, :], in1=rC[:, t0:t0 + 512], op=ALU.mult),
             reads=[Brope], writes=[Bps[bank], Bt1[ri]])
        P.op("dve", lambda e: e.tensor_tensor(out=t2[ri][:], in0=psum[:, rb, :], in1=rS[:, t0:t0 + 512], op=ALU.mult),
             reads=[Brope], writes=[Bps[rb], Bt2[ri]])
        s2 = scnt[0] % NSTG
        scnt[0] += 1
        P.op("pool", lambda e: e.tensor_tensor(out=stg[s2][:], in0=t1[ri][:], in1=t2[ri][:], op=ALU.add),
             reads=[Bt1[ri], Bt2[ri]], writes=[Bstg[s2]])
        P.dma("sp", lambda e: e.dma_start(out=dst, in_=stg[s2][:]), reads=[Bstg[s2]])

    pending = None
    for hg in range(3):
        for which in range(2):
            b = load_w((512 if which == 0 else 2048) + hg * 512)
            for hh in range(4):
                h = hg * 4 + hh
                groups = [(0, 512), (512, 512)] if which == 0 else [(0, 512), (512, 512), (1024, 512), (1536, 512), (2048, 256)]
                for (t0, n) in groups:
                    bank = pcnt[0] % 4
                    pcnt[0] += 1
                    for kc in range(16):
                        P.op("pe", lambda e, kc=kc, bank=bank, b=b, hh=hh, t0=t0, n=n: e.matmul(
                            psum[:, bank, 0:n], lhsT=wt[b][:, kc, hh * 128:(hh + 1) * 128], rhs=hT[:, kc, t0:t0 + n],
                            start=(kc == 0), stop=(kc == 15)), reads=[Bwt[b]] + BhT, writes=[Bps[bank]])
                    s = scnt[0] % NSTG
                    scnt[0] += 1
                    dst = (qT_s if which == 0 else kT_s)[h, :, t0:t0 + n]
                    P.op("act", lambda e, s=s, bank=bank, n=n: e.activation(out=stg[s][:, 0:n], in_=psum[:, bank, 0:n], func=AF.Copy),
                         writes=[Bps[bank], Bstg[s]])
                    if pending is not None:
                        c2_tail(*pending)
                    pending = (s, bank, t0, n, dst)
    c2_tail(*pending)
    b = load_w(0)
    pwb = T("pwb", [128, 4, 128], BF16)
    Bpw = Buf()
    P.dma("pool", lambda e: e.dma_start(out=pwb[:], in_=pool_w.rearrange("g c d -> c g d")), writes=[Bpw])
    ic = T("ic", [128, 4096], F32)
    P.dma("sp", lambda e: e.dma_start(out=ic[:], in_=icnt), writes=[Bpw])
    zT = T("zT", [128, 1040], F32)
    sA = T("sA", [128, 1040], F32)
    sB = T("sB", [128, 1040], F32)
    ymix = T("ymix", [128, 1024], BF16)
    Bz = Buf()
    for g in range(4):
        for th in range(2):
            bank = pcnt[0] % 4
            pcnt[0] += 1
            for kc in range(16):
                P.op("pe", lambda e, kc=kc, bank=bank, th=th, g=g: e.matmul(
                    psum[:, bank, :], lhsT=wt[b][:, kc, g * 128:(g + 1) * 128], rhs=hT[:, kc, th * 512:(th + 1) * 512],
                    start=(kc == 0), stop=(kc == 15)), reads=[Bwt[b]] + BhT, writes=[Bps[bank]])
            P.op("act", lambda e, bank=bank, th=th: e.activation(out=zT[:, 8 + th * 512:8 + (th + 1) * 512], in_=psum[:, bank, :], func=AF.Copy),
                 writes=[Bps[bank], Bz])
        bank = pcnt[0] % 4
        pcnt[0] += 1
        for hi, tsrc in enumerate((2040, 1024)):
            for kc in range(16):
                P.op("pe", lambda e, kc=kc, bank=bank, hi=hi, tsrc=tsrc, g=g: e.matmul(
                    psum[:, bank, hi * 8:hi * 8 + 8], lhsT=wt[b][:, kc, g * 128:(g + 1) * 128], rhs=hT[:, kc, tsrc:tsrc + 8],
                    start=(kc == 0), stop=(kc == 15)), reads=[Bwt[b]] + BhT, writes=[Bps[bank]])
        P.op("dve", lambda e, bank=bank: e.tensor_scalar_mul(out=zT[:, 0:8], in0=psum[:, bank, 0:8], scalar1=halo[:, 0:1]), reads=[Bc], writes=[Bps[bank], Bz])
        P.op("dve", lambda e, bank=bank: e.tensor_scalar_mul(out=zT[:, 1032:1040], in0=psum[:, bank, 8:16], scalar1=halo[:, 1:2]), reads=[Bc], writes=[Bps[bank], Bz])
        w = 2 << g
        src = zT
        ln = 1040
        step = 1
        bufs = [sA, sB]
        bi = 0
        while step < w:
            dst_ = bufs[bi]
            bi ^= 1
            ln2 = ln - step
            P.op("dve", lambda e, src=src, dst_=dst_, ln2=ln2, step=step: e.tensor_tensor(
                out=dst_[:, 0:ln2], in0=src[:, 0:ln2], in1=src[:, step:step + ln2], op=ALU.add), writes=[Bz])
            src = dst_
            ln = ln2
            step *= 2
        o0 = 8 - w // 2
        dst_ = bufs[bi]
        P.op("dve", lambda e, src=src, dst_=dst_, o0=o0, g=g: e.tensor_tensor(
            out=dst_[:, 0:1024], in0=src[:, o0:o0 + 1024], in1=ic[:, g * 1024:(g + 1) * 1024], op=ALU.mult), reads=[Bpw], writes=[Bz])
        P.op("dve", lambda e, dst_=dst_: e.tensor_tensor(out=ymix[:], in0=dst_[:, 0:1024], in1=zT[:, 8:1032], op=ALU.subtract), writes=[Bz])
        for th in range(2):
            bank = pcnt[0] % 4
            pcnt[0] += 1
            P.op("pe", lambda e, bank=bank, th=th, g=g: e.matmul(psum[:, bank, :], lhsT=pwb[:, g, :], rhs=ymix[:, th * 512:(th + 1) * 512],
                                                             start=True, stop=True), reads=[Bpw, Bz], writes=[Bps[bank]])
            P.op("dve", lambda e, bank=bank, th=th, g=g: e.tensor_scalar(
                out=mixT[:, g, th * 512:(th + 1) * 512], in0=psum[:, bank, :], scalar1=poolb[:, g:g + 1], scalar2=pools[:, g:g + 1],
                op0=ALU.add, op1=ALU.mult), reads=[Bc], writes=[Bps[bank], BmixT[g]])
    if "pool" in dbg:
        dump("pool", mixT[:, 0:4, :], BmixT, [128, 4, NQ], BF16)
    P.barrier()
    if stop == "C":
        if "qkv" in dbg:
            off[0] = R2
            for nm, src_ in (("qT0", qT_s[0]), ("kT0", kT_s[0])):
                tmpb = T(nm, list(src_.shape), BF16)
                bb = Buf()
                P.dma("sp", lambda e, tmpb=tmpb, src_=src_: e.dma_start(out=tmpb[:], in_=src_), writes=[bb])
                dump(nm, tmpb[:], [bb], list(src_.shape), BF16)
            tmpv = T("v0", [128, 18, 128], BF16)
            bb = Buf()
            P.dma("sp", lambda e: e.dma_start(out=tmpv[:], in_=V_sv[:, :, 0:128]), writes=[bb])
            dump("v0", tmpv[:], [bb], [128, 18, 128], BF16)
        return end()

    off[0] = R2
    kTh = [T("kTh%d" % i, [128, NT], BF16) for i in range(2)]
    qTh = [T("qTh%d" % i, [128, NQ], BF16) for i in range(2)]
    Vaug = [T("Vaug%d" % i, [128, 18, 129], BF16) for i in range(2)]
    Bkq = [Buf(), Buf()]
    for i in range(2):
        P.op("dve", lambda e, i=i: e.memset(Vaug[i][:, :, 128:129], 1.0), writes=[Bkq[i]])
    NEB = 3
    Eb = [T("Eb%d" % i, [128, 1024], BF16) for i in range(NEB)]
    BEb = [Buf() for _ in range(NEB)]
    posb = [T("posb%d" % i, [128, 4, 385], F32) for i in range(2)]
    Bpo = [Buf(), Buf()]
    rz = T("rz", [128, 4, 2], F32)
    nl = T("nl", [128, 4], F32)
    oo = T("oo", [128, 4, 128], F32)
    o2 = T("o2", [128, 4, 128], F32)
    ss = T("ss", [128, 8], F32)
    mhalf = T("mhalf", [128, 4], F32)
    ysb = [T("ysb%d" % i, [128, 4, 128], BF16) for i in range(2)]
    Bys = [Buf(), Buf()]
    Bpp = Buf()
    P.op("dve", lambda e: e.memset(mhalf[:], -0.5), writes=[Bpp])
    psb = psum[:, 0, 0:256].bitcast(BF16)

    def load_head(h):
        i = h % 2
        P.dma("sp", lambda e: e.dma_start(out=kTh[i][:], in_=kT_s[h]), writes=[Bkq[i]])
        P.dma("sp", lambda e: e.dma_start(out=qTh[i][:], in_=qT_s[h]), writes=[Bkq[i]])
        P.dma("sp", lambda e: e.dma_start(out=Vaug[i][:, :, 0:128], in_=V_sv[:, :, h * 128:(h + 1) * 128]), writes=[Bkq[i]])

    its = [(h, qh, kt) for h in range(12) for qh in range(2) for kt in range(18)]

    def qk(idx):
        h, qh, kt = its[idx]
        i = h % 2
        pi = idx % 2
        for m in range(2):
            bank = 2 * pi + m
            P.op("pe", lambda e, m=m, bank=bank: e.matmul(psum[:, bank, :], lhsT=kTh[i][64 * m:64 * m + 64, kt * 128:(kt + 1) * 128],
                                                          rhs=qTh[i][64 * m:64 * m + 64, qh * 512:(qh + 1) * 512], start=True, stop=True),
                 reads=[Bkq[i]], writes=[Bps[bank]])
        eb = idx % NEB
        P.op("act", lambda e: e.activation(out=Eb[eb][:].rearrange("p (m q) -> p m q", m=2), in_=psum[:, 2 * pi:2 * pi + 2, :], func=AF.Exp, scale=0.125),
             writes=[Bps[2 * pi], Bps[2 * pi + 1], BEb[eb]])

    def pvmm(idx):
        h, qh, kt = its[idx]
        i = h % 2
        eb = idx % NEB
        for m in range(2):
            for qt in range(4):
                P.op("pe", lambda e, qt=qt, m=m: e.matmul(psum[:, 4 + qt, m * 256:m * 256 + 129], lhsT=Eb[eb][:, m * 512 + qt * 128:m * 512 + (qt + 1) * 128],
                                                          rhs=Vaug[i][:, kt, :], start=(kt == 0 and m == 0), stop=(kt == 17),
                                                          skip_group_check=True),
                     reads=[BEb[eb], Bkq[i]], writes=[Bps[4 + qt]])

    def post_a(h, qh):
        pb_ = (h * 2 + qh) % 2
        po = posb[pb_]
        for qt in range(4):
            P.op("dve", lambda e, qt=qt: e.tensor_copy(out=po[:, qt, :], in_=psum[:, 4 + qt, 0:385]), writes=[Bps[4 + qt], Bpo[pb_]])
        B2 = [Bpo[pb_]]
        P.op("dve", lambda e: e.reciprocal(out=rz[:], in_=po[:, :, 128:385:256]), reads=B2, writes=[Bpp])
        P.op("dve", lambda e: e.tensor_scalar_mul(out=nl[:], in0=rz[:, :, 1], scalar1=nlam), reads=[Bc], writes=[Bpp])
        P.op("dve", lambda e: e.tensor_tensor(out=oo[:], in0=po[:, :, 0:128], in1=rz[:, :, 0:1].to_broadcast([128, 4, 128]), op=ALU.mult), reads=B2, writes=[Bpp])
        P.op("dve", lambda e: e.tensor_tensor(out=o2[:], in0=po[:, :, 256:384], in1=nl[:].unsqueeze(2).to_broadcast([128, 4, 128]), op=ALU.mult), reads=B2, writes=[Bpp])
        P.op("dve", lambda e: e.tensor_tensor(out=oo[:], in0=oo[:], in1=o2[:], op=ALU.add), writes=[Bpp])
        P.op("dve", lambda e: e.tensor_tensor(out=o2[:], in0=oo[:], in1=oo[:], op=ALU.mult), writes=[Bpp])
        P.op("dve", lambda e: e.tensor_reduce(out=ss[:, 0:4], in_=o2[:], axis=AX.X, op=ALU.add), writes=[Bpp])
        P.op("dve", lambda e: e.tensor_scalar(out=ss[:, 0:4], in0=ss[:, 0:4], scalar1=1.0 / 128, scalar2=EPS, op0=ALU.mult, op1=ALU.add), writes=[Bpp])
        P.op("pool", lambda e: e.tensor_tensor(out=ss[:, 4:8], in0=ss[:, 0:4], in1=mhalf[:], op=ALU.pow), writes=[Bpp])
        P.op("dve", lambda e: e.tensor_tensor(out=oo[:], in0=oo[:], in1=ss[:, 4:8].unsqueeze(2).to_broadcast([128, 4, 128]), op=ALU.mult), writes=[Bpp])
        P.op("dve", lambda e: e.tensor_tensor(out=ysb[pb_][:], in0=oo[:], in1=sgb[:].unsqueeze(1).to_broadcast([128, 4, 128]), op=ALU.mult), reads=[Bc, Bpp], writes=[Bys[pb_]])

    def post_b(h, qh):
        pb_ = (h * 2 + qh) % 2
        for qt in range(4):
            P.op("pe", lambda e, qt=qt: e.transpose(out=psb[:, qt * 128:(qt + 1) * 128], in_=ysb[pb_][:, qt, :], identity=identb[:]),
                 reads=[Bys[pb_], Bc], writes=[Bps[0]])
        P.op("dve", lambda e: e.tensor_copy(out=mixT[:, 4 + h, qh * 512:(qh + 1) * 512], in_=psb), reads=[Bys[pb_]], writes=[Bps[0], BmixT[4 + h]])

    load_head(0)
    NI = len(its)
    qk(0)
    pend = []
    for idx in range(NI):
        h, qh, kt = its[idx]
        if kt == 0 and qh == 0 and h + 1 < 12:
            load_head(h + 1)
        if idx + 1 < NI:
            qk(idx + 1)
        pvmm(idx)
        if kt == 17:
            post_a(h, qh)
            pend.append((idx + 4, h, qh))
        if pend and pend[0][0] <= idx:
            _, h_, qh_ = pend.pop(0)
            post_b(h_, qh_)
    for _, h_, qh_ in pend:
        post_b(h_, qh_)
    if "attn" in dbg:
        dump("attn", mixT[:, 4:16, :], BmixT, [128, 12, NQ], BF16)
    P.barrier()
    if stop == "D":
        return end()

    off[0] = R0
    x1T = T("x1T", [128, 16, NQ], F32)
    Bx1 = [[Buf() for _ in range(2)] for _ in range(16)]
    allx1 = [b_ for r_ in Bx1 for b_ in r_]
    off[0] = R2
    P.dma("sp", lambda e: e.dma_start(out=x1T[:], in_=xTv[:, :, 0:NQ]), writes=allx1)
    wov = w_out.rearrange("(kc p) n -> p kc n", p=128)
    wt2 = [T("wo%d" % i, [128, 16, 512], BF16) for i in range(2)]
    Bwo = [Buf(), Buf()]
    def issue_wo(dg):
        if dg < 4:
            P.dma("pool", lambda e: e.dma_start(out=wt2[dg % 2][:], in_=wov[:, :, dg * 512:(dg + 1) * 512]), writes=[Bwo[dg % 2]])

    issue_wo(0)
    for dg in range(4):
        b = dg % 2
        issue_wo(dg + 1)
        for dci in range(4):
            dc = dg * 4 + dci
            for th in range(2):
                bank = pcnt[0] % 4
                pcnt[0] += 1
                for kc in range(16):
                    P.op("pe", lambda e, kc=kc, bank=bank, b=b, dci=dci, th=th: e.matmul(
                        psum[:, bank, :], lhsT=wt2[b][:, kc, dci * 128:(dci + 1) * 128], rhs=mixT[:, kc, th * 512:(th + 1) * 512],
                        start=(kc == 0), stop=(kc == 15)), reads=[Bwo[b]] + BmixT, writes=[Bps[bank]])
                P.op("dve", lambda e, bank=bank, dc=dc, th=th: e.scalar_tensor_tensor(
                    out=x1T[:, dc, th * 512:(th + 1) * 512], in0=psum[:, bank, :], scalar=g1[:, dc:dc + 1],
                    in1=x1T[:, dc, th * 512:(th + 1) * 512], op0=ALU.mult, op1=ALU.add), reads=[Bc], writes=[Bps[bank], Bx1[dc][th]])
    if "x1" in dbg:
        dump("x1", x1T[:], allx1, [128, 16, NQ])
    P.barrier()
    if stop == "E":
        return end()

    off[0] = R1
    fT = T("fT", [128, 16, NQ], BF16)
    BfT = [Buf(), Buf()]
    off[0] = R2
    sq = T("sq2", [128, 16, 512], BF16)
    rstd = T("rstd2", [128, 1024], F32)
    ftmp = [T("ftmp%d" % i, [128, 512], F32) for i in range(2)]
    Bft = [Buf(), Buf()]
    fc = 0
    for th in range(2):
        rms_stats(x1T[:, :, th * 512:(th + 1) * 512], allx1, 512, th, rstd[:, th * 512:(th + 1) * 512])
        for kc in range(16):
            fi = fc % 2
            fc += 1
            P.op("dve", lambda e, kc=kc, th=th, fi=fi: e.scalar_tensor_tensor(
                out=ftmp[fi][:], in0=x1T[:, kc, th * 512:(th + 1) * 512], scalar=gs2[:, kc:kc + 1], in1=rstd[:, th * 512:(th + 1) * 512],
                op0=ALU.mult, op1=ALU.mult), reads=[Brstd, Bc] + allx1, writes=[Bft[fi]])
            P.op("act", lambda e, kc=kc, th=th, fi=fi: e.activation(
                out=fT[:, kc, th * 512:(th + 1) * 512], in_=ftmp[fi][:], func=AF.Identity, bias=sh2[:, kc:kc + 1], scale=1.0),
                reads=[Bft[fi], Bc], writes=[BfT[th]])
    if "f" in dbg:
        dump("f", fT[:], BfT, [128, 16, NQ], BF16)
    P.dma("sp", lambda e: e.dma_start(out=x1_s, in_=x1T[:]), reads=allx1)
    P.barrier()
    off[0] = R2
    qpT = T("qpT", [128, 16, NQ], BF16)
    BqpT = Buf()
    kTb = T("kTb", [128, 16, 128], BF16)
    BkTb = Buf()
    baseF = off[0]
    wq2 = [T("wqt%d" % i, [128, 16, 512], BF16) for i in range(2)]
    Bwq = [Buf(), Buf()]
    P.dma("pool", lambda e: e.dma_start(out=kTb[:], in_=keysT.rearrange("h c k -> c h k")), writes=[BkTb])
    wqv = wq.rearrange("(kc p) n -> p kc n", p=128)
    def issue_wq(c4):
        if c4 < 4:
            P.dma("pool", lambda e: e.dma_start(out=wq2[c4 % 2][:], in_=wqv[:, :, c4 * 512:(c4 + 1) * 512]), writes=[Bwq[c4 % 2]])

    issue_wq(0)
    for c4 in range(4):
        b = c4 % 2
        issue_wq(c4 + 1)
        for j in range(4):
            hp = c4 * 4 + j
            for th in range(2):
                bank = pcnt[0] % 4
                pcnt[0] += 1
                for kc in range(16):
                    P.op("pe", lambda e, kc=kc, bank=bank, b=b, j=j, th=th: e.matmul(
                        psum[:, bank, :], lhsT=wq2[b][:, kc, j * 128:(j + 1) * 128], rhs=fT[:, kc, th * 512:(th + 1) * 512],
                        start=(kc == 0), stop=(kc == 15)), reads=[Bwq[b]] + BfT, writes=[Bps[bank]])
                P.op("act", lambda e, bank=bank, hp=hp, th=th: e.activation(out=qpT[:, hp, th * 512:(th + 1) * 512], in_=psum[:, bank, :], func=AF.Copy),
                     writes=[Bps[bank], BqpT])
    P.barrier()
    off[0] = baseF
    sc = [T("sc0", [128, 16, 128], F32)]
    m16 = T("m16", [128, 16, 16], F32)
    ix = T("ix", [128, 16, 16], U32)
    ixf = T("ixf", [128, 16, 16], F32)
    cand = T("cand", [128, 8, 256], F32)
    t16 = T("t16", [128, 8, 16], F32)
    pos = T("pos", [128, 8, 16], U32)
    posf = T("posf", [128, 8, 16], F32)
    jf = T("jf", [128, 8, 16], F32)
    iff = T("iff", [128, 8, 16], F32)
    ee = T("ee", [128, 8, 16], F32)
    zs = T("zs", [128, 8], F32)
    sel = [T("sel%d" % i, [128, 3, 128], F32) for i in range(2)]
    selb = [T("selb%d" % i, [128, 2, 128], BF16) for i in range(2)]
    ohs = [T("oh%d" % i, [128, 8, 16, 16], F32) for i in range(2)]
    gT = [T("gT%d" % i, [128, 128], F32) for i in range(2)]
    Em = T("Em", [128, 2, 4096], BF16)
    BEm = Buf()
    P.dma("pool", lambda e: e.dma_start(out=Em[:], in_=emat), writes=[BEm])
    SIb = [T("SIb%d" % i, [128, 4, 128], BF16) for i in range(2)]
    BSI = [Buf(), Buf()]
    for i in range(2):
        P.op("dve", lambda e, i=i: e.memset(SIb[i][:], 1.0), writes=[BSI[i]])
    off[0] = R0
    QT = 32
    PA = [T("PA%d" % i, [128, QT, 128], BF16) for i in range(2)]
    PB = [T("PB%d" % i, [128, QT, 128], BF16) for i in range(2)]
    Gs = T("Gs", [128, 128, 128], BF16)
    Bsc = [Buf()]
    Bseg = [Buf() for _ in range(16)]
    Bch = [Buf() for _ in range(8)]
    Bixf, Bmisc, BGs = Buf(), Buf(), Buf()
    Boh = [Buf(), Buf()]
    Bsel = [Buf(), Buf()]
    BgT = [Buf(), Buf()]
    BPA = [Buf(), Buf()]
    BPB = [Buf(), Buf()]
    m16v = m16[:].rearrange("p (h s) k -> p h s k", s=2)
    ixfv = ixf[:].rearrange("p (h s) k -> p h s k", s=2)
    candv = cand[:].rearrange("p h (i j) -> p h i j", j=16)
    iota16 = iota_f[:, 0:16]
    thr16 = T("thr16", [128, 16], F32)
    P.op("dve", lambda e: e.tensor_scalar_mul(out=thr16[:], in0=iota16, scalar1=16.0), reads=[Bc], writes=[Bc])
    Gdv = Gd.rearrange("a b t -> b a t")
    qcnt = [0]
    dcnt = [0]
    gcnt = [0]
    OH_SCALE = 1.125
    GATE_FIX = 1.0 / (OH_SCALE * OH_SCALE)

    def scores(tt):
        si = 0
        for c4 in range(4):
            for j in range(4):
                hp = c4 * 4 + j
                P.op("pe", lambda e, j=j, hp=hp: e.matmul(psum[:, 0, j * 128:(j + 1) * 128], lhsT=qpT[:, hp, tt * 128:(tt + 1) * 128],
                                                          rhs=kTb[:, hp, :], start=True, stop=True), reads=[BqpT, BkTb], writes=[Bps[0]])
            P.op("act", lambda e, c4=c4: e.activation(out=sc[si][:, c4 * 4:(c4 + 1) * 4, :], in_=psum[:, 0, :].rearrange("p (j k) -> p j k", k=128), func=AF.Copy),
                 writes=[Bps[0], Bsc[si]])

    def topk(tt):
        si = tt % 2
        scur = sc[0]
        scores(tt)
        if "sc" in dbg and tt == 0:
            dump("sc", scur[:], [Bsc[0]], [128, 16, 128])
        R16 = range(16)
        for hp in R16:
            P.op("dve", lambda e, hp=hp: e.max(out=m16[:, hp, 0:8], in_=scur[:, hp, :]), reads=[Bsc[0]], writes=[Bseg[hp]])
        for hp in R16:
            P.op("dve", lambda e, hp=hp: e.max_index(out=ix[:, hp, 0:8], in_max=m16[:, hp, 0:8], in_values=scur[:, hp, :]), reads=[Bsc[0]], writes=[Bseg[hp]])
        for hp in R16:
            P.op("dve", lambda e, hp=hp: e.match_replace(out=scur[:, hp, :], in_to_replace=m16[:, hp, 0:8], in_values=scur[:, hp, :], imm_value=-1e30), writes=[Bseg[hp], Bsc[0]])
        for hp in R16:
            P.op("dve", lambda e, hp=hp: e.max(out=m16[:, hp, 8:16], in_=scur[:, hp, :]), reads=[Bsc[0]], writes=[Bseg[hp]])
        for hp in R16:
            P.op("dve", lambda e, hp=hp: e.max_index(out=ix[:, hp, 8:16], in_max=m16[:, hp, 8:16], in_values=scur[:, hp, :]), reads=[Bsc[0]], writes=[Bseg[hp]])
        P.op("dve", lambda e: e.tensor_copy(out=ixf[:], in_=ix[:]), reads=Bseg, writes=[Bixf])
        P.op("dve", lambda e: e.tensor_tensor(out=candv, in0=m16v[:, :, 0, :].unsqueeze(3).to_broadcast([128, 8, 16, 16]),
                                              in1=m16v[:, :, 1, :].unsqueeze(2).to_broadcast([128, 8, 16, 16]), op=ALU.add), reads=Bseg, writes=Bch)
        R8 = range(8)
        for h in R8:
            P.op("dve", lambda e, h=h: e.max(out=t16[:, h, 0:8], in_=cand[:, h, :]), writes=[Bch[h]])
        for h in R8:
            P.op("dve", lambda e, h=h: e.max_index(out=pos[:, h, 0:8], in_max=t16[:, h, 0:8], in_values=cand[:, h, :]), writes=[Bch[h]])
        for h in R8:
            P.op("dve", lambda e, h=h: e.match_replace(out=cand[:, h, :], in_to_replace=t16[:, h, 0:8], in_values=cand[:, h, :], imm_value=-1e30), writes=[Bch[h]])
        for h in R8:
            P.op("dve", lambda e, h=h: e.max(out=t16[:, h, 8:16], in_=cand[:, h, :]), writes=[Bch[h]])
        for h in R8:
            P.op("dve", lambda e, h=h: e.max_index(out=pos[:, h, 8:16], in_max=t16[:, h, 8:16], in_values=cand[:, h, :]), writes=[Bch[h]])
        selc = sel[si]
        P.op("dve", lambda e: e.tensor_copy(out=posf[:], in_=pos[:]), reads=Bch, writes=[Bmisc])
        P.op("dve", lambda e: e.tensor_tensor(
            out=ohs[0][:], in0=posf[:].unsqueeze(3).to_broadcast([128, 8, 16, 16]),
            in1=thr16[:].unsqueeze(1).unsqueeze(1).to_broadcast([128, 8, 16, 16]), op=ALU.is_ge), reads=[Bc, Bmisc], writes=[Boh[0]])
        P.op("dve", lambda e: e.tensor_tensor(out=ee[:], in0=t16[:], in1=t16[:, :, 0:1].to_broadcast([128, 8, 16]), op=ALU.subtract), reads=Bch, writes=[Bixf])
        P.op("act", lambda e: e.activation(out=ee[:], in_=ee[:], func=AF.Exp), writes=[Bixf])
        P.op("dve", lambda e: e.tensor_reduce(out=iff[:], in_=ohs[0][:], axis=AX.X, op=ALU.add), reads=[Boh[0]], writes=[Bmisc])
        P.op("dve", lambda e: e.tensor_scalar_add(out=iff[:], in0=iff[:], scalar1=-1.0), writes=[Bmisc])
        P.op("dve", lambda e: e.scalar_tensor_tensor(out=jf[:], in0=iff[:], scalar=-16.0, in1=posf[:], op0=ALU.mult, op1=ALU.add), writes=[Bmisc])
        P.op("dve", lambda e: e.tensor_reduce(out=zs[:], in_=ee[:], axis=AX.X, op=ALU.add), writes=[Bixf])
        P.op("dve", lambda e: e.reciprocal(out=zs[:], in_=zs[:]), writes=[Bixf])
        P.op("dve", lambda e: e.tensor_scalar_mul(out=zs[:], in0=zs[:], scalar1=GATE_FIX), writes=[Bixf])
        sides = ((0, iff), (1, jf))
        for side, selidx in sides:
            P.op("dve", lambda e, side=side, selidx=selidx: e.tensor_tensor(
                out=ohs[side][:], in0=iota16.unsqueeze(1).unsqueeze(1).to_broadcast([128, 8, 16, 16]),
                in1=selidx[:].unsqueeze(3).to_broadcast([128, 8, 16, 16]), op=ALU.is_equal), reads=[Bc, Bmisc], writes=[Boh[side]])
        P.op("dve", lambda e: e.tensor_tensor(out=selc[:, 2, :].rearrange("p (h k) -> p h k", k=16), in0=ee[:],
                                              in1=zs[:].unsqueeze(2).to_broadcast([128, 8, 16]), op=ALU.mult), reads=[Bixf], writes=[Bsel[si]])
        for side, selidx in sides:
            P.op("dve", lambda e, side=side: e.tensor_tensor(
                out=ohs[side][:], in0=ohs[side][:], in1=ixfv[:, :, side, :].unsqueeze(2).to_broadcast([128, 8, 16, 16]), op=ALU.mult),
                reads=[Bixf], writes=[Boh[side]])
        for side, selidx in sides:
            P.op("dve", lambda e, side=side: e.tensor_reduce(
                out=selc[:, side, :].rearrange("p (h k) -> p h k", k=16), in_=ohs[side][:], axis=AX.X, op=ALU.add),
                reads=[Boh[side]], writes=[Bsel[si]])
        if "sel" in dbg and tt == 0:
            dump("sel", selc[:], [Bsel[si]], [128, 3, 128])
        sb_ = selb[si]
        P.op("dve", lambda e: e.tensor_copy(out=sb_[:], in_=selc[:, 0:2, :]), writes=[Bsel[si]])
        for h2 in range(2):
            for side in range(2):
                P.dma("sp", lambda e, h2=h2, side=side: e.dma_start(out=SIb[si][0:128:2, h2 * 2 + side, :], in_=sb_[64 * h2:64 * h2 + 64, side, :]),
                      reads=[Bsel[si]], writes=[BSI[si]])

    def topk_tail(tt):
        si = tt % 2
        selc = sel[si]
        P.op("pe", lambda e: e.transpose(out=psum[:, 0, 0:128], in_=selc[:, 2, :], identity=identf), reads=[Bsel[si], Bc], writes=[Bps[0]])
        gTc = gT[si]
        P.op("dve", lambda e: e.tensor_copy(out=gTc[:], in_=psum[:, 0, 0:128]), writes=[Bps[0], BgT[si]])

    def stages(tt):
        si = tt % 2
        gTc = gT[si]
        def dstage(q):
            tb = q * QT
            h2, qq = q // 2, q % 2
            pi = qcnt[0] % 2
            qcnt[0] += 1
            for side, PX, BPX in ((0, PA, BPA), (1, PB, BPB)):
                for p in range(4):
                    bk = 4 + 2 * (dcnt[0] % 2)
                    dcnt[0] += 1
                    for j in range(2):
                        c0 = p * 1024 + j * 512
                        P.op("pe", lambda e, side=side, bk=bk, j=j, c0=c0: e.matmul(
                            psum[:, bk + j, :], lhsT=SIb[si][:, h2 * 2 + side, :], rhs=Em[:, qq, c0:c0 + 512],
                            start=True, stop=True), reads=[BSI[si], BEm], writes=[Bps[bk + j]])
                    P.op("act", lambda e, PX=PX, p=p, bk=bk: e.activation(
                        out=PX[pi][:, p * 8:(p + 1) * 8, :].rearrange("p (j u) a -> p j (u a)", j=2), in_=psum[:, bk:bk + 2, :],
                        func=AF.Derivative_Erf, scale=4.0), writes=[Bps[bk], Bps[bk + 1], BPX[pi]])
            P.op("pool", lambda e: e.tensor_tensor(
                out=PA[pi][:], in0=PA[pi][:], in1=gTc[:, tb:tb + QT].unsqueeze(2).to_broadcast([128, QT, 128]), op=ALU.mult), reads=[BgT[si]], writes=[BPA[pi]])
            return pi

        def gstage(q, pi):
            tb = q * QT
            for t4 in range(QT // 4):
                bank = 1 + (gcnt[0] % 3)
                gcnt[0] += 1
                for u in range(4):
                    tl = t4 * 4 + u
                    P.op("pe", lambda e, bank=bank, u=u, tl=tl: e.matmul(psum[:, bank, u * 128:(u + 1) * 128], lhsT=PB[pi][:, tl, :], rhs=PA[pi][:, tl, :],
                                                                      start=True, stop=True), reads=[BPA[pi], BPB[pi]], writes=[Bps[bank]])
                tg = tb + t4 * 4
                P.op("act", lambda e, bank=bank, tg=tg: e.activation(
                    out=Gs[:, :, tg:tg + 4], in_=psum[:, bank, :].rearrange("p (t a) -> p a t", a=128), func=AF.Copy),
                    writes=[Bps[bank], BGs])

        NQ4 = 128 // QT
        pis = [dstage(0)]
        for q in range(NQ4):
            if q + 1 < NQ4:
                pis.append(dstage(q + 1))
            gstage(q, pis[q])
        for a4 in range(4):
            P.dma("sp", lambda e, a4=a4: e.dma_start(out=Gdv[:, a4 * 32:(a4 + 1) * 32, tt * 128:(tt + 1) * 128], in_=Gs[:, a4 * 32:(a4 + 1) * 32, :]),
                  reads=[BGs])

    topk(0)
    topk_tail(0)
    for tt in range(8):
        if tt + 1 < 8:
            topk(tt + 1)
        stages(tt)
        if tt + 1 < 8:
            topk_tail(tt + 1)
    P.barrier()
    if stop == "F":
        if "G" in dbg:
            tmpg = T("tmpg", [128, 2, NQ], BF16)
            bb = Buf()
            P.dma("sp", lambda e: e.dma_start(out=tmpg[:], in_=Gdv[:, 0:2, :]), writes=[bb])
            dump("G", tmpg[:], [bb], [128, 2, NQ], BF16)
        return end()

    P.dma("sp", lambda e: e.dma_start(out=x1T[:], in_=x1_s), writes=allx1)
    off[0] = R2
    ut = [T("ut%d" % i, [128, 16, 256], BF16) for i in range(3)]
    vt = [T("vt%d" % i, [128, 4, D], BF16) for i in range(2)]
    gt = [T("gt%d" % i, [128, NQ], BF16) for i in range(3)]
    ga = [T("ga%d" % i, [128, NQ], BF16) for i in range(2)]
    AT = [T("AT%d" % i, [128, 4, NQ], BF16) for i in range(2)]
    But, Bvt, Bgt, Bga = [Buf(), Buf(), Buf()], [Buf(), Buf()], [Buf(), Buf(), Buf()], [Buf(), Buf()]
    BAT = [[Buf() for _ in range(4)] for _ in range(2)]
    uTv = uT.rearrange("(kc p) e -> p kc e", p=128)
    pvv = pv.rearrange("(c p) d -> p c d", p=128)
    NS = NE // 512

    def load_ut(cp):
        if cp < NE // 256:
            P.dma("pool", lambda e: e.dma_start(out=ut[cp % 3][:], in_=uTv[:, :, cp * 256:(cp + 1) * 256]), writes=[But[cp % 3]])

    load_ut(0)
    load_ut(1)

    def p1(s):
        si = s % 2
        for c in range(4):
            a = 4 * s + c
            cp = a // 2
            ui = cp % 3
            if a % 2 == 0:
                load_ut(cp + 2)
            if c == 3:
                P.dma("pool", lambda e: e.dma_start(out=vt[si][:], in_=pvv[:, 4 * s:4 * s + 4, :]), writes=[Bvt[si]])
            gi = a % 3
            P.dma("sp", lambda e, a=a, gi=gi: e.dma_start(out=gt[gi][:], in_=Gd[a]), writes=[Bgt[gi]])
            for th in range(2):
                bank = (a % 2) * 2 + th
                for kc in range(16):
                    P.op("pe", lambda e, kc=kc, bank=bank, ui=ui, a=a, th=th: e.matmul(
                        psum[:, bank, :], lhsT=ut[ui][:, kc, (a % 2) * 128:(a % 2) * 128 + 128], rhs=fT[:, kc, th * 512:(th + 1) * 512],
                        start=(kc == 0), stop=(kc == 15)), reads=[But[ui]] + BfT, writes=[Bps[bank]])
            gi2 = a % 2
            for th in range(2):
                bank = (a % 2) * 2 + th
                P.op("act", lambda e, bank=bank, gi2=gi2, th=th: e.activation(out=ga[gi2][:, th * 512:(th + 1) * 512], in_=psum[:, bank, :], func=AF.Gelu),
                     writes=[Bps[bank], Bga[gi2]])
            P.op("dve", lambda e, gi=gi, gi2=gi2, c=c, si=si: e.tensor_tensor(out=AT[si][:, c, :], in0=ga[gi2][:], in1=gt[gi][:], op=ALU.mult),
                 reads=[Bga[gi2], Bgt[gi]], writes=[BAT[si][c]])

    acnt = [0]

    def p2(s):
        si = s % 2
        for th in range(2):
            for dp in range(8):
                pair = acnt[0] % 2
                acnt[0] += 1
                for c in range(4):
                    for u in range(2):
                        dc = dp * 2 + u
                        bank = 4 + pair * 2 + u
                        P.op("pe", lambda e, c=c, dc=dc, bank=bank, th=th: e.matmul(
                            psum[:, bank, :], lhsT=vt[si][:, c, dc * 128:(dc + 1) * 128], rhs=AT[si][:, c, th * 512:(th + 1) * 512],
                            start=(c == 0), stop=(c == 3)), reads=[Bvt[si], BAT[si][c]], writes=[Bps[bank]])
                for u in range(2):
                    dc = dp * 2 + u
                    bank = 4 + pair * 2 + u
                    P.op("dve", lambda e, dc=dc, bank=bank, th=th: e.scalar_tensor_tensor(
                        out=x1T[:, dc, th * 512:(th + 1) * 512], in0=psum[:, bank, :], scalar=g2[:, dc:dc + 1],
                        in1=x1T[:, dc, th * 512:(th + 1) * 512], op0=ALU.mult, op1=ALU.add), reads=[Bc], writes=[Bps[bank], Bx1[dc][th]])

    nsuper = NS if stop != "G1" else 2
    p1(0)
    for s in range(nsuper):
        if s + 1 < nsuper:
            p1(s + 1)
        p2(s)
    if "x2" in dbg:
        dump("x2", x1T[:], allx1, [128, 16, NQ])
    P.barrier()

    off[0] = R2
    sq = T("sq3", [128, 16, 512], BF16)
    rstd = T("rstd3", [128, 1024], F32)
    ob = [T("ob%d" % i, [128, 16, 512], F32) for i in range(1)]
    Bob = Buf()
    outTv = outT.rearrange("(kc p) t -> p kc t", p=128)
    for th in range(2):
        rms_stats(x1T[:, :, th * 512:(th + 1) * 512], allx1, 512, th, rstd[:, th * 512:(th + 1) * 512])
        for kc in range(16):
            P.op("dve", lambda e, kc=kc, th=th: e.scalar_tensor_tensor(
                out=ob[0][:, kc, :], in0=x1T[:, kc, th * 512:(th + 1) * 512], scalar=fg[:, kc:kc + 1], in1=rstd[:, th * 512:(th + 1) * 512],
                op0=ALU.mult, op1=ALU.mult), reads=[Brstd, Bc] + allx1, writes=[Bob])
        P.dma("sp", lambda e, th=th: e.dma_start(out=outTv[:, :, th * 512:(th + 1) * 512], in_=ob[0][:]), reads=[Bob])
    return end()


def _rope_tables(pos):
    inv = (10000.0 ** (-np.arange(16, dtype=np.float32) / 16)).astype(np.float32)
    row = (pos // 64).astype(np.float32)
    col = (pos % 64).astype(np.float32)
    C = np.zeros((128, 2048), np.float32)
    S = np.zeros((128, 2048), np.float32)
    for p in range(128):
        j = p % 64
        axis, r = j // 32, j % 32
        hf, i = r // 16, r % 16
        ang = (row if axis == 0 else col) * inv[i]
        C[p] = np.cos(ang)
        S[p] = np.sin(ang) * (-1.0 if hf == 0 else 1.0)
    return C, S


def _emat():
    e = np.zeros((64, 2, 2, 32, 128), np.float32)
    bb = np.arange(128, dtype=np.float32)
    for tp in range(64):
        q, t = tp // 32, tp % 32
        e[tp, 0, q, t, :] = 1.0
        e[tp, 1, q, t, :] = -bb
    return np.ascontiguousarray(e.reshape(128, 2, 4096))


def _core_inputs(k, I, shared):
    b, half = k // 2, k % 2
    own = slice(half * 1024, half * 1024 + 1024)
    oth = slice((1 - half) * 1024, (1 - half) * 1024 + 1024)
    x = I["x"][b]
    xT = np.ascontiguousarray(np.concatenate([x[own], x[oth], I["ctx"][b]], 0).T)
    cv = np.stack([I["c"][b].reshape(16, 128).T, I["c_ctx"].reshape(16, 128).T], -1).reshape(128, 32)
    pos = np.concatenate([np.arange(2048)[own], np.arange(2048)[oth]])
    C, S = _rope_tables(pos)
    t = np.arange(2048)
    ic = np.zeros((4, 1024), np.float32)
    for g, w in enumerate((2, 4, 8, 16)):
        lo = np.clip(t - w // 2, 0, 2048)
        hi = np.clip(t + w // 2, 0, 2048)
        ic[g] = (1.0 / (hi - lo).astype(np.float32))[own]
    halo = np.array([1.0 if half == 1 else 0.0, 1.0 if half == 0 else 0.0], np.float32)
    smallv = np.concatenate([shared["smallv"], np.broadcast_to(halo, (128, 2))], 1)
    d = dict(shared["common"])
    d.update(xT=xT, cvec=np.ascontiguousarray(cv, np.float32), smallv=np.ascontiguousarray(smallv, np.float32),
             ropeC=C, ropeS=S, icnt=np.ascontiguousarray(np.broadcast_to(ic.reshape(1, 4096), (128, 4096))))
    return d


def _shared(I):
    f = lambda a: np.ascontiguousarray(a, dtype=np.float32)
    pc = lambda v, n: v.reshape(n, 128).T
    smallv = np.concatenate([pc(I["ada_b"][0], 96), pc(I["norm1_g"][0], 16), pc(I["norm2_g"][0], 16), pc(I["final_g"], 16),
                             I["pool_b"][0].T, pc(I["pool_scale"][0], 4)], 1)
    pm = np.zeros((128, 128), np.float32)
    for m in range(128):
        r = (m % 64) % 32
        pm[m + 16 if r < 16 else m - 16, m] = 1.0
    cmat = np.concatenate([np.eye(128, dtype=np.float32), pm], 1)
    common = dict(
        ada_w=f(I["ada_w"][0]), w_in=f(I["w_in"][0]), pool_w=f(I["pool_w"][0]),
        dlam=f(np.broadcast_to(I["diff_lambda"][0].reshape(1, 256), (128, 256))),
        subg=f(np.broadcast_to(I["subln_g"][0].reshape(1, 128), (128, 128))),
        w_out=f(I["w_out"][0]), wq=f(I["peer_wq"][0]),
        keysT=f(I["peer_keys"][0].reshape(16, 128, 128).transpose(0, 2, 1)),
        uT=f(I["peer_u"][0].T), pv=f(I["peer_v"][0]), cmat=f(cmat), emat=_emat())
    return dict(smallv=f(smallv), common=common)


_NC = None


def kernel(**inputs):
    global _NC
    I = {k: np.asarray(v) for k, v in inputs.items()}
    if _NC is None:
        _NC = build()[0]
    shared = _shared(I)
    in_maps = [_core_inputs(k, I, shared) for k in range(8)]
    res = run_bass_kernel_spmd(_NC, in_maps, core_ids=list(range(8)))
    out = np.empty((4, 2048, 2048), np.float32)
    for k in range(8):
        b, half = k // 2, k % 2
        out[b, half * 1024:(half + 1) * 1024, :] = res.results[k]["outT"].T
    return out
```

```python
import math
import numpy as np
import concourse.bass as bass
import concourse.mybir as mybir
from concourse.bass_utils import run_bass_kernel_spmd
from contextlib import ExitStack

F32 = mybir.dt.float32
BF16 = mybir.dt.bfloat16
I32 = mybir.dt.int32
U32 = mybir.dt.uint32
AF = mybir.ActivationFunctionType
ALU = mybir.AluOpType
AX = mybir.AxisListType

D = 2048
NT = 2304
NQ = 1024
NE = 16384
EPS = 1e-6
LAM_INIT = 0.8 - 0.6 * math.exp(0.0)


class Buf:
    __slots__ = ("name", "w", "r")

    def __init__(self, name=""):
        self.name = name
        self.w = None
        self.r = []


class Prog:
    CE = ("act", "dve", "pool", "pe")
    NDS = 12

    def __init__(self, nc, stack):
        self.nc = nc
        self.eng = {"sp": nc.sync, "act": nc.scalar, "dve": nc.vector, "pool": nc.gpsimd, "pe": nc.tensor}
        self.cnt = {e: 0 for e in self.CE}
        self.sem = {("c", e): stack.enter_context(nc.semaphore("c_" + e)) for e in self.CE}
        self.dcnt = {}
        self.dnext = {}
        for q in ("sp", "act", "pool"):
            for i in range(self.NDS):
                self.sem[("d", q, i)] = stack.enter_context(nc.semaphore("d_%s%d" % (q, i)))
                self.dcnt[(q, i)] = 0
            self.dnext[q] = 0
        self.known = {e: {} for e in self.eng}
        self.nops = 0

    def _deps(self, eng, reads, writes):
        deps = {}

        def add(ev):
            if ev is None:
                return
            k, v = ev
            if eng == "pe" and k == ("c", "pe"):
                return
            if deps.get(k, 0) < v:
                deps[k] = v
        for b in reads:
            add(b.w)
        for b in writes:
            add(b.w)
            for ev in b.r:
                add(ev)
        out = []
        kn = self.known[eng]
        for k, v in deps.items():
            if kn.get(k, 0) < v:
                kn[k] = v
                out.append((k, v))
        return out

    def _commit(self, ev, reads, writes):
        for b in reads:
            b.r.append(ev)
        for b in writes:
            b.w = ev
            b.r = []

    def _issue(self, eng, waits, fn, inc):
        e = self.eng[eng]
        for k, v in waits:
            e.wait_ge(self.sem[k], v)
        if fn is not None:
            fn(e).then_inc(self.sem[inc[0]], inc[1])
        self.nops += 1

    def op(self, eng, fn, reads=(), writes=()):
        waits = self._deps(eng, reads, writes)
        self.cnt[eng] += 1
        ev = (("c", eng), self.cnt[eng])
        self._commit(ev, reads, writes)
        self._issue(eng, waits, fn, (("c", eng), 1))
        return ev

    def dma(self, q, fn, reads=(), writes=()):
        waits = self._deps(q, reads, writes)
        i = self.dnext[q]
        self.dnext[q] = (i + 1) % self.NDS
        k = ("d", q, i)
        prev = self.dcnt[(q, i)]
        if prev > 0 and self.known[q].get(k, 0) < prev:
            self.known[q][k] = prev
            waits.append((k, prev))
        self.dcnt[(q, i)] = prev + 16
        ev = (k, prev + 16)
        self._commit(ev, reads, writes)
        self._issue(q, waits, fn, (k, 16))
        return ev

    def _all(self):
        waits = [(("d", q, i), v) for (q, i), v in self.dcnt.items() if v > 0]
        waits += [(("c", e), self.cnt[e]) for e in self.CE if self.cnt[e] > 0]
        return waits

    def barrier(self):
        allw = self._all()
        for eng in self.eng:
            kn = self.known[eng]
            w = []
            for k, v in allw:
                if kn.get(k, 0) < v:
                    kn[k] = v
                    w.append((k, v))
            self._issue(eng, w, None, None)

    def finish(self):
        self.barrier()


def build(stop=None, dbg=()):
    nc = bass.Bass("TRN2", target_bir_lowering=False)
    din = lambda n, s, dt=F32: nc.dram_tensor(n, list(s), dt, kind="ExternalInput").ap()
    xT = din("xT", [D, NT])
    cvec = din("cvec", [128, 32])
    ada_w = din("ada_w", [D, 6 * D])
    smallv = din("smallv", [128, 96 + 48 + 8 + 2])
    w_in = din("w_in", [D, 5120])
    pool_w = din("pool_w", [4, 128, 128])
    dlam = din("dlam", [128, 256])
    subg = din("subg", [128, 128])
    w_out = din("w_out", [D, D])
    wq = din("wq", [D, D])
    keysT = din("keysT", [16, 128, 128])
    uT = din("uT", [D, NE])
    pv = din("pv", [NE, D])
    ropeC = din("ropeC", [128, 2048])
    ropeS = din("ropeS", [128, 2048])
    cmat = din("cmat", [128, 256])
    icnt = din("icnt", [128, 4096])
    emat = din("emat", [128, 2, 4096])
    outT = nc.dram_tensor("outT", [D, NQ], F32, kind="ExternalOutput").ap()
    kT_s = nc.dram_tensor("kT_s", [12, 128, NT], BF16, kind="Internal").ap()
    qT_s = nc.dram_tensor("qT_s", [12, 128, NQ], BF16, kind="Internal").ap()
    V_s = nc.dram_tensor("V_s", [NT, 1536], BF16, kind="Internal").ap()
    Gd = nc.dram_tensor("Gd", [128, 128, NQ], BF16, kind="Internal").ap()
    x1_s = nc.dram_tensor("x1_s", [128, 16, NQ], F32, kind="Internal").ap()
    dbg_out = {}

    st = ExitStack()
    P = Prog(nc, st)
    off = [16384]

    def T(name, shape, dt, at=None):
        n = 1
        for s in shape[1:]:
            n *= s
        nb = n * (2 if dt == BF16 else 4)
        nb = (nb + 63) // 64 * 64
        if at is None:
            at = off[0]
            off[0] = at + nb
        assert at + nb <= 229000, (name, at, nb)
        return nc.alloc_sbuf_tensor_at(name, list(shape), dt, offset=at)

    def dump(name, ap, buf, shape, dt=F32):
        d = nc.dram_tensor("dbg_" + name, list(shape), dt, kind="ExternalOutput").ap()
        dbg_out[name] = d
        P.dma("sp", lambda e: e.dma_start(out=d, in_=ap), reads=buf)

    psum = nc.alloc_psum_tensor("psum", [128, 8, 512], F32)
    Bps = [Buf("ps%d" % i) for i in range(8)]

    def end():
        P.finish()
        st.close()
        return nc, dbg_out

    smv = T("smv", [128, 154], F32)
    cm = T("cm", [128, 256], F32)
    cvs = T("cvs", [128, 32], F32)
    dl = T("dl", [128, 256], F32)
    sgb = T("sgb", [128, 128], F32)
    identb = T("identb", [128, 128], BF16)
    pmb = T("pmb", [128, 128], BF16)
    onesb = T("onesb", [128, 128], BF16)
    modT = T("modT", [128, 96, 2], F32)
    gs1 = T("gs1", [128, 16, 2], F32)
    gs2 = T("gs2", [128, 16], F32)
    lamt = T("lamt", [128, 8], F32)
    iota_i = T("iota_i", [128, 128], I32)
    iota_f = T("iota_f", [128, 128], F32)
    Bc = Buf("consts")
    for t, d in ((smv, smallv), (cm, cmat), (cvs, cvec), (dl, dlam), (sgb, subg)):
        P.dma("sp", lambda e, t=t, d=d: e.dma_start(out=t[:], in_=d), writes=[Bc])
    identf = cm[:, 0:128]
    P.op("dve", lambda e: e.tensor_copy(out=identb[:], in_=cm[:, 0:128]), reads=[Bc], writes=[Bc])
    P.op("dve", lambda e: e.tensor_copy(out=pmb[:], in_=cm[:, 128:256]), reads=[Bc], writes=[Bc])
    P.op("dve", lambda e: e.memset(onesb[:], 1.0), writes=[Bc])
    epsb = T("epsb", [128, 1], F32)
    P.op("dve", lambda e: e.memset(epsb[:], EPS), writes=[Bc])
    P.op("pool", lambda e: e.iota(iota_i[:], pattern=[[1, 128]], base=0, channel_multiplier=0), writes=[Bc])
    P.op("dve", lambda e: e.tensor_copy(out=iota_f[:], in_=iota_i[:]), reads=[Bc], writes=[Bc])
    P.op("dve", lambda e: e.tensor_scalar_mul(out=sgb[:], in0=sgb[:], scalar1=1.0 - LAM_INIT), writes=[Bc])
    dlv = dl[:].rearrange("p (a b c) -> p a b c", a=2, b=2)
    prod = T("prod", [128, 2, 64], F32)
    P.op("dve", lambda e: e.tensor_tensor(out=prod[:], in0=dlv[:, :, 0, :], in1=dlv[:, :, 1, :], op=ALU.mult), writes=[Bc])
    P.op("dve", lambda e: e.tensor_reduce(out=lamt[:, 0:2], in_=prod[:], axis=AX.X, op=ALU.add), writes=[Bc])
    P.op("act", lambda e: e.activation(out=lamt[:, 2:4], in_=lamt[:, 0:2], func=AF.Exp), writes=[Bc])
    P.op("dve", lambda e: e.tensor_tensor(out=lamt[:, 4:5], in0=lamt[:, 3:4], in1=lamt[:, 2:3], op=ALU.subtract), writes=[Bc])
    P.op("dve", lambda e: e.tensor_scalar_add(out=lamt[:, 4:5], in0=lamt[:, 4:5], scalar1=-LAM_INIT), writes=[Bc])
    nlam = lamt[:, 4:5]
    adab = smv[:, 0:96]
    n1g = smv[:, 96:112]
    n2g = smv[:, 112:128]
    fg = smv[:, 128:144]
    poolb = smv[:, 144:148]
    pools = smv[:, 148:152]
    halo = smv[:, 152:154]
    P.op("act", lambda e: e.activation(out=cvs[:], in_=cvs[:], func=AF.Silu), writes=[Bc])
    base0 = off[0]
    R0 = base0
    R1 = R0 + 72 * 1024
    R2 = R1 + 32 * 1024

    off[0] = R1
    aw = [T("aw%d" % i, [128, 16, 512], F32) for i in range(2)]
    Baw = [Buf(), Buf()]
    modrow = T("modrow", [2, 6 * D], F32)
    Bmr = Buf()
    awv = ada_w.rearrange("(kc p) n -> p kc n", p=128)
    csv = cvs[:].rearrange("p (k r) -> p k r", r=2)
    for nt in range(24):
        b = nt % 2
        bank = nt % 4
        P.dma("sp", lambda e, nt=nt, b=b: e.dma_start(out=aw[b][:], in_=awv[:, :, nt * 512:(nt + 1) * 512]), writes=[Baw[b]])
        for kc in range(16):
            P.op("pe", lambda e, kc=kc, b=b, bank=bank: e.matmul(
                psum[0:2, bank, :], lhsT=csv[:, kc, :], rhs=aw[b][:, kc, :],
                start=(kc == 0), stop=(kc == 15)), reads=[Baw[b], Bc], writes=[Bps[bank]])
        P.op("act", lambda e, nt=nt, bank=bank: e.activation(out=modrow[:, nt * 512:(nt + 1) * 512], in_=psum[0:2, bank, :], func=AF.Copy),
             writes=[Bps[bank], Bmr])
    for j in range(96):
        P.op("pe", lambda e, j=j: e.transpose(out=psum[:, 4, 2 * j:2 * j + 2], in_=modrow[:, j * 128:(j + 1) * 128], identity=cm[0:2, 0:2]),
             reads=[Bmr, Bc], writes=[Bps[4]])
    P.op("dve", lambda e: e.tensor_tensor(
        out=modT[:], in0=psum[:, 4, 0:192].rearrange("p (j r) -> p j r", r=2),
        in1=adab.unsqueeze(2).to_broadcast([128, 96, 2]), op=ALU.add), reads=[Bc], writes=[Bps[4], Bc])
    P.op("dve", lambda e: e.tensor_scalar_add(out=gs1[:], in0=modT[:, 16:32, :], scalar1=1.0), writes=[Bc])
    P.op("dve", lambda e: e.tensor_tensor(out=gs1[:], in0=gs1[:], in1=n1g.unsqueeze(2).to_broadcast([128, 16, 2]), op=ALU.mult), writes=[Bc])
    P.op("dve", lambda e: e.tensor_scalar_add(out=gs2[:], in0=modT[:, 64:80, 0], scalar1=1.0), writes=[Bc])
    P.op("dve", lambda e: e.tensor_tensor(out=gs2[:], in0=gs2[:], in1=n2g, op=ALU.mult), writes=[Bc])
    sh1 = modT[:, 0:16, :]
    g1 = modT[:, 32:48, 0]
    sh2 = modT[:, 48:64, 0]
    g2 = modT[:, 80:96, 0]
    if "mod" in dbg:
        dump("mod", modT[:], [Bc], [128, 96, 2])
        dump("lam", lamt[:], [Bc], [128, 8])
    P.barrier()
    if stop == "A":
        return end()

    off[0] = R0
    hT = T("hT", [128, 16, NT], BF16)
    BhT = [Buf() for _ in range(5)]
    off[0] = R1
    xt = [T("xt%d" % i, [128, 16, 512], F32) for i in range(2)]
    Bxt = [[Buf() for _ in range(16)] for _ in range(2)]
    sq = T("sq", [128, 16, 512], BF16)
    Bsq = Buf()
    rstd = T("rstd", [128, 1024], F32)
    Brstd = Buf()
    xTv = xT.rearrange("(kc p) t -> p kc t", p=128)

    def rms_stats(src, Bsrc, n, ps_bank, rs_ap):
        P.op("act", lambda e: e.activation(out=sq[:, :, 0:n], in_=src, func=AF.Square), reads=Bsrc, writes=[Bsq])
        for kc in range(16):
            P.op("pe", lambda e, kc=kc: e.matmul(psum[:, ps_bank, 0:n], lhsT=onesb[:], rhs=sq[:, kc, 0:n],
                                                 start=(kc == 0), stop=(kc == 15)), reads=[Bsq, Bc], writes=[Bps[ps_bank]])
        P.op("act", lambda e: e.activation(out=rs_ap, in_=psum[:, ps_bank, 0:n], func=AF.Sqrt, bias=epsb[:], scale=1.0 / D),
             reads=[Bc], writes=[Bps[ps_bank], Brstd])
        P.op("dve", lambda e: e.reciprocal(out=rs_ap, in_=rs_ap), writes=[Brstd])

    for g in range(5):
        b = g % 2
        n = 512 if g < 4 else 256
        t0 = g * 512
        r = 0 if g < 4 else 1
        P.dma("sp", lambda e, b=b, t0=t0, n=n: e.dma_start(out=xt[b][:, :, 0:n], in_=xTv[:, :, t0:t0 + n]), writes=Bxt[b])
        rms_stats(xt[b][:, :, 0:n], Bxt[b], n, g % 2, rstd[:, 0:n])
        for kc in range(16):
            P.op("dve", lambda e, b=b, kc=kc, n=n, r=r: e.scalar_tensor_tensor(
                out=xt[b][:, kc, 0:n], in0=xt[b][:, kc, 0:n], scalar=gs1[:, kc, r:r + 1], in1=rstd[:, 0:n],
                op0=ALU.mult, op1=ALU.mult), reads=[Brstd, Bc], writes=[Bxt[b][kc]])
            P.op("act", lambda e, b=b, kc=kc, n=n, r=r, t0=t0: e.activation(
                out=hT[:, kc, t0:t0 + n], in_=xt[b][:, kc, 0:n], func=AF.Identity, bias=sh1[:, kc, r:r + 1], scale=1.0),
                reads=[Bxt[b][kc], Bc], writes=[BhT[g]])
    if "h" in dbg:
        dump("h", hT[:, :, 0:512], BhT, [128, 16, 512], BF16)
    P.barrier()
    if stop == "B":
        return end()

    off[0] = R1
    mixT = T("mixT", [128, 16, NQ], BF16)
    BmixT = [Buf() for _ in range(16)]
    off[0] = R2
    wt = [T("wt%d" % i, [128, 16, 512], BF16) for i in range(2)]
    Bwt = [Buf(), Buf()]
    rC = T("rC", [128, 2048], F32)
    rS = T("rS", [128, 2048], F32)
    Brope = Buf()
    P.dma("sp", lambda e: e.dma_start(out=rC[:], in_=ropeC), writes=[Brope])
    P.dma("sp", lambda e: e.dma_start(out=rS[:], in_=ropeS), writes=[Brope])
    wv = w_in.rearrange("(kc p) n -> p kc n", p=128)
    wcnt = [0]

    worder = [3584, 4096, 4608] + [c for hg in range(3) for c in (512 + hg * 512, 2048 + hg * 512)] + [0]

    def issue_w(i):
        if i < len(worder):
            c0 = worder[i]
            P.dma("pool", lambda e: e.dma_start(out=wt[i % 2][:], in_=wv[:, :, c0:c0 + 512]), writes=[Bwt[i % 2]])

    issue_w(0)

    def load_w(c0):
        i = wcnt[0]
        assert worder[i] == c0
        wcnt[0] += 1
        issue_w(i + 1)
        return i % 2

    NSTG = 8
    stg = [T("stg%d" % i, [128, 512], BF16) for i in range(NSTG)]
    Bstg = [Buf() for _ in range(NSTG)]
    scnt = [0]
    pcnt = [0]
    V_sv = V_s.rearrange("(tt p) c -> p tt c", p=128)
    for vc in range(3):
        b = load_w(3584 + vc * 512)
        for tt in range(18):
            bank = pcnt[0] % 4
            pcnt[0] += 1
            for kc in range(16):
                P.op("pe", lambda e, kc=kc, tt=tt, bank=bank, b=b: e.matmul(
                    psum[:, bank, :], lhsT=hT[:, kc, tt * 128:(tt + 1) * 128], rhs=wt[b][:, kc, :],
                    start=(kc == 0), stop=(kc == 15)), reads=[Bwt[b]] + BhT, writes=[Bps[bank]])
            s = scnt[0] % NSTG
            scnt[0] += 1
            P.op("act", lambda e, s=s, bank=bank: e.activation(out=stg[s][:], in_=psum[:, bank, :], func=AF.Copy),
                 writes=[Bps[bank], Bstg[s]])
            P.dma("sp", lambda e, s=s, tt=tt, vc=vc: e.dma_start(out=V_sv[:, tt, vc * 512:(vc + 1) * 512], in_=stg[s][:]),
                  reads=[Bstg[s]])
    t1 = [T("t1_%d" % i, [128, 512], F32) for i in range(2)]
    t2 = [T("t2_%d" % i, [128, 512], F32) for i in range(2)]
    Bt1 = [Buf(), Buf()]
    Bt2 = [Buf(), Buf()]
    rcnt = [0]

    def c2_tail(s, bank, t0, n, dst):
        if t0 >= 2048:
            P.dma("sp", lambda e: e.dma_start(out=dst, in_=stg[s][:, 0:n]), reads=[Bstg[s]])
            return
        rb = 4 + (rcnt[0] % 2)
        ri = rcnt[0] % 2
        rcnt[0] += 1
        P.op("pe", lambda e: e.matmul(psum[:, rb, :], lhsT=pmb[:], rhs=stg[s][:], start=True, stop=True),
             reads=[Bstg[s], Bc], writes=[Bps[rb]])
        P.op("dve", lambda e: e.tensor_tensor(out=t1[ri][:], in0=psum[:, bank, :], in1=rC[:, t0:t0 + 512], op=ALU.mult),
             reads=[Brope], writes=[Bps[bank], Bt1[ri]])
        P.op("dve", lambda e: e.tensor_tensor(out=t2[ri][:], in0=psum[:, rb, :], in1=rS[:, t0:t0 + 512], op=ALU.mult),
             reads=[Brope], writes=[Bps[rb], Bt2[ri]])
        s2 = scnt[0] % NSTG
        scnt[0] += 1
        P.op("pool", lambda e: e.tensor_tensor(out=stg[s2][:], in0=t1[ri][:], in1=t2[ri][:], op=ALU.add),
             reads=[Bt1[ri], Bt2[ri]], writes=[Bstg[s2]])
        P.dma("sp", lambda e: e.dma_start(out=dst, in_=stg[s2][:]), reads=[Bstg[s2]])

    pending = None
    for hg in range(3):
        for which in range(2):
            b = load_w((512 if which == 0 else 2048) + hg * 512)
            for hh in range(4):
                h = hg * 4 + hh
                groups = [(0, 512), (512, 512)] if which == 0 else [(0, 512), (512, 512), (1024, 512), (1536, 512), (2048, 256)]
                for (t0, n) in groups:
                    bank = pcnt[0] % 4
                    pcnt[0] += 1
                    for kc in range(16):
                        P.op("pe", lambda e, kc=kc, bank=bank, b=b, hh=hh, t0=t0, n=n: e.matmul(
                            psum[:, bank, 0:n], lhsT=wt[b][:, kc, hh * 128:(hh + 1) * 128], rhs=hT[:, kc, t0:t0 + n],
                            start=(kc == 0), stop=(kc == 15)), reads=[Bwt[b]] + BhT, writes=[Bps[bank]])
                    s = scnt[0] % NSTG
                    scnt[0] += 1
                    dst = (qT_s if which == 0 else kT_s)[h, :, t0:t0 + n]
                    P.op("act", lambda e, s=s, bank=bank, n=n: e.activation(out=stg[s][:, 0:n], in_=psum[:, bank, 0:n], func=AF.Copy),
                         writes=[Bps[bank], Bstg[s]])
                    if pending is not None:
                        c2_tail(*pending)
                    pending = (s, bank, t0, n, dst)
    c2_tail(*pending)
    b = load_w(0)
    pwb = T("pwb", [128, 4, 128], BF16)
    Bpw = Buf()
    P.dma("pool", lambda e: e.dma_start(out=pwb[:], in_=pool_w.rearrange("g c d -> c g d")), writes=[Bpw])
    ic = T("ic", [128, 4096], F32)
    P.dma("sp", lambda e: e.dma_start(out=ic[:], in_=icnt), writes=[Bpw])
    zT = T("zT", [128, 1040], F32)
    sA = T("sA", [128, 1040], F32)
    sB = T("sB", [128, 1040], F32)
    ymix = T("ymix", [128, 1024], BF16)
    Bz = Buf()
    for g in range(4):
        for th in range(2):
            bank = pcnt[0] % 4
            pcnt[0] += 1
            for kc in range(16):
                P.op("pe", lambda e, kc=kc, bank=bank, th=th, g=g: e.matmul(
                    psum[:, bank, :], lhsT=wt[b][:, kc, g * 128:(g + 1) * 128], rhs=hT[:, kc, th * 512:(th + 1) * 512],
                    start=(kc == 0), stop=(kc == 15)), reads=[Bwt[b]] + BhT, writes=[Bps[bank]])
            P.op("act", lambda e, bank=bank, th=th: e.activation(out=zT[:, 8 + th * 512:8 + (th + 1) * 512], in_=psum[:, bank, :], func=AF.Copy),
                 writes=[Bps[bank], Bz])
        bank = pcnt[0] % 4
        pcnt[0] += 1
        for hi, tsrc in enumerate((2040, 1024)):
            for kc in range(16):
                P.op("pe", lambda e, kc=kc, bank=bank, hi=hi, tsrc=tsrc, g=g: e.matmul(
                    psum[:, bank, hi * 8:hi * 8 + 8], lhsT=wt[b][:, kc, g * 128:(g + 1) * 128], rhs=hT[:, kc, tsrc:tsrc + 8],
                    start=(kc == 0), stop=(kc == 15)), reads=[Bwt[b]] + BhT, writes=[Bps[bank]])
        P.op("dve", lambda e, bank=bank: e.tensor_scalar_mul(out=zT[:, 0:8], in0=psum[:, bank, 0:8], scalar1=halo[:, 0:1]), reads=[Bc], writes=[Bps[bank], Bz])
        P.op("dve", lambda e, bank=bank: e.tensor_scalar_mul(out=zT[:, 1032:1040], in0=psum[:, bank, 8:16], scalar1=halo[:, 1:2]), reads=[Bc], writes=[Bps[bank], Bz])
        w = 2 << g
        src = zT
        ln = 1040
        step = 1
        bufs = [sA, sB]
        bi = 0
        while step < w:
            dst_ = bufs[bi]
            bi ^= 1
            ln2 = ln - step
            P.op("dve", lambda e, src=src, dst_=dst_, ln2=ln2, step=step: e.tensor_tensor(
                out=dst_[:, 0:ln2], in0=src[:, 0:ln2], in1=src[:, step:step + ln2], op=ALU.add), writes=[Bz])
            src = dst_
            ln = ln2
            step *= 2
        o0 = 8 - w // 2
        dst_ = bufs[bi]
        P.op("dve", lambda e, src=src, dst_=dst_, o0=o0, g=g: e.tensor_tensor(
            out=dst_[:, 0:1024], in0=src[:, o0:o0 + 1024], in1=ic[:, g * 1024:(g + 1) * 1024], op=ALU.mult), reads=[Bpw], writes=[Bz])
        P.op("dve", lambda e, dst_=dst_: e.tensor_tensor(out=ymix[:], in0=dst_[:, 0:1024], in1=zT[:, 8:1032], op=ALU.subtract), writes=[Bz])
        for th in range(2):
            bank = pcnt[0] % 4
            pcnt[0] += 1
            P.op("pe", lambda e, bank=bank, th=th, g=g: e.matmul(psum[:, bank, :], lhsT=pwb[:, g, :], rhs=ymix[:, th * 512:(th + 1) * 512],
                                                             start=True, stop=True), reads=[Bpw, Bz], writes=[Bps[bank]])
            P.op("dve", lambda e, bank=bank, th=th, g=g: e.tensor_scalar(
                out=mixT[:, g, th * 512:(th + 1) * 512], in0=psum[:, bank, :], scalar1=poolb[:, g:g + 1], scalar2=pools[:, g:g + 1],
                op0=ALU.add, op1=ALU.mult), reads=[Bc], writes=[Bps[bank], BmixT[g]])
    if "pool" in dbg:
        dump("pool", mixT[:, 0:4, :], BmixT, [128, 4, NQ], BF16)
    P.barrier()
    if stop == "C":
        if "qkv" in dbg:
            off[0] = R2
            for nm, src_ in (("qT0", qT_s[0]), ("kT0", kT_s[0])):
                tmpb = T(nm, list(src_.shape), BF16)
                bb = Buf()
                P.dma("sp", lambda e, tmpb=tmpb, src_=src_: e.dma_start(out=tmpb[:], in_=src_), writes=[bb])
                dump(nm, tmpb[:], [bb], list(src_.shape), BF16)
            tmpv = T("v0", [128, 18, 128], BF16)
            bb = Buf()
            P.dma("sp", lambda e: e.dma_start(out=tmpv[:], in_=V_sv[:, :, 0:128]), writes=[bb])
            dump("v0", tmpv[:], [bb], [128, 18, 128], BF16)
        return end()

    off[0] = R2
    kTh = [T("kTh%d" % i, [128, NT], BF16) for i in range(2)]
    qTh = [T("qTh%d" % i, [128, NQ], BF16) for i in range(2)]
    Vaug = [T("Vaug%d" % i, [128, 18, 129], BF16) for i in range(2)]
    Bkq = [Buf(), Buf()]
    for i in range(2):
        P.op("dve", lambda e, i=i: e.memset(Vaug[i][:, :, 128:129], 1.0), writes=[Bkq[i]])
    NEB = 3
    Eb = [T("Eb%d" % i, [128, 1024], BF16) for i in range(NEB)]
    BEb = [Buf() for _ in range(NEB)]
    posb = [T("posb%d" % i, [128, 4, 385], F32) for i in range(2)]
    Bpo = [Buf(), Buf()]
    rz = T("rz", [128, 4, 2], F32)
    nl = T("nl", [128, 4], F32)
    oo = T("oo", [128, 4, 128], F32)
    o2 = T("o2", [128, 4, 128], F32)
    ss = T("ss", [128, 8], F32)
    mhalf = T("mhalf", [128, 4], F32)
    ysb = [T("ysb%d" % i, [128, 4, 128], BF16) for i in range(2)]
    Bys = [Buf(), Buf()]
    Bpp = Buf()
    P.op("dve", lambda e: e.memset(mhalf[:], -0.5), writes=[Bpp])
    psb = psum[:, 0, 0:256].bitcast(BF16)

    def load_head(h):
        i = h % 2
        P.dma("sp", lambda e: e.dma_start(out=kTh[i][:], in_=kT_s[h]), writes=[Bkq[i]])
        P.dma("sp", lambda e: e.dma_start(out=qTh[i][:], in_=qT_s[h]), writes=[Bkq[i]])
        P.dma("sp", lambda e: e.dma_start(out=Vaug[i][:, :, 0:128], in_=V_sv[:, :, h * 128:(h + 1) * 128]), writes=[Bkq[i]])

    its = [(h, qh, kt) for h in range(12) for qh in range(2) for kt in range(18)]

    def qk(idx):
        h, qh, kt = its[idx]
        i = h % 2
        pi = idx % 2
        for m in range(2):
            bank = 2 * pi + m
            P.op("pe", lambda e, m=m, bank=bank: e.matmul(psum[:, bank, :], lhsT=kTh[i][64 * m:64 * m + 64, kt * 128:(kt + 1) * 128],
                                                          rhs=qTh[i][64 * m:64 * m + 64, qh * 512:(qh + 1) * 512], start=True, stop=True),
                 reads=[Bkq[i]], writes=[Bps[bank]])
        eb = idx % NEB
        P.op("act", lambda e: e.activation(out=Eb[eb][:].rearrange("p (m q) -> p m q", m=2), in_=psum[:, 2 * pi:2 * pi + 2, :], func=AF.Exp, scale=0.125),
             writes=[Bps[2 * pi], Bps[2 * pi + 1], BEb[eb]])

    def pvmm(idx):
        h, qh, kt = its[idx]
        i = h % 2
        eb = idx % NEB
        for m in range(2):
            for qt in range(4):
                P.op("pe", lambda e, qt=qt, m=m: e.matmul(psum[:, 4 + qt, m * 256:m * 256 + 129], lhsT=Eb[eb][:, m * 512 + qt * 128:m * 512 + (qt + 1) * 128],
                                                          rhs=Vaug[i][:, kt, :], start=(kt == 0 and m == 0), stop=(kt == 17),
                                                          skip_group_check=True),
                     reads=[BEb[eb], Bkq[i]], writes=[Bps[4 + qt]])

    def post_a(h, qh):
        pb_ = (h * 2 + qh) % 2
        po = posb[pb_]
        for qt in range(4):
            P.op("dve", lambda e, qt=qt: e.tensor_copy(out=po[:, qt, :], in_=psum[:, 4 + qt, 0:385]), writes=[Bps[4 + qt], Bpo[pb_]])
        B2 = [Bpo[pb_]]
        P.op("dve", lambda e: e.reciprocal(out=rz[:], in_=po[:, :, 128:385:256]), reads=B2, writes=[Bpp])
        P.op("dve", lambda e: e.tensor_scalar_mul(out=nl[:], in0=rz[:, :, 1], scalar1=nlam), reads=[Bc], writes=[Bpp])
        P.op("dve", lambda e: e.tensor_tensor(out=oo[:], in0=po[:, :, 0:128], in1=rz[:, :, 0:1].to_broadcast([128, 4, 128]), op=ALU.mult), reads=B2, writes=[Bpp])
        P.op("dve", lambda e: e.tensor_tensor(out=o2[:], in0=po[:, :, 256:384], in1=nl[:].unsqueeze(2).to_broadcast([128, 4, 128]), op=ALU.mult), reads=B2, writes=[Bpp])
        P.op("dve", lambda e: e.tensor_tensor(out=oo[:], in0=oo[:], in1=o2[:], op=ALU.add), writes=[Bpp])
        P.op("dve", lambda e: e.tensor_tensor(out=o2[:], in0=oo[:], in1=oo[:], op=ALU.mult), writes=[Bpp])
        P.op("dve", lambda e: e.tensor_reduce(out=ss[:, 0:4], in_=o2[:], axis=AX.X, op=ALU.add), writes=[Bpp])
        P.op("dve", lambda e: e.tensor_scalar(out=ss[:, 0:4], in0=ss[:, 0:4], scalar1=1.0 / 128, scalar2=EPS, op0=ALU.mult, op1=ALU.add), writes=[Bpp])
        P.op("pool", lambda e: e.tensor_tensor(out=ss[:, 4:8], in0=ss[:, 0:4], in1=mhalf[:], op=ALU.pow), writes=[Bpp])
        P.op("dve", lambda e: e.tensor_tensor(out=oo[:], in0=oo[:], in1=ss[:, 4:8].unsqueeze(2).to_broadcast([128, 4, 128]), op=ALU.mult), writes=[Bpp])
        P.op("dve", lambda e: e.tensor_tensor(out=ysb[pb_][:], in0=oo[:], in1=sgb[:].unsqueeze(1).to_broadcast([128, 4, 128]), op=ALU.mult), reads=[Bc, Bpp], writes=[Bys[pb_]])

    def post_b(h, qh):
        pb_ = (h * 2 + qh) % 2
        for qt in range(4):
            P.op("pe", lambda e, qt=qt: e.transpose(out=psb[:, qt * 128:(qt + 1) * 128], in_=ysb[pb_][:, qt, :], identity=identb[:]),
                 reads=[Bys[pb_], Bc], writes=[Bps[0]])
        P.op("dve", lambda e: e.tensor_copy(out=mixT[:, 4 + h, qh * 512:(qh + 1) * 512], in_=psb), reads=[Bys[pb_]], writes=[Bps[0], BmixT[4 + h]])

    load_head(0)
    NI = len(its)
    qk(0)
    pend = []
    for idx in range(NI):
        h, qh, kt = its[idx]
        if kt == 0 and qh == 0 and h + 1 < 12:
            load_head(h + 1)
        if idx + 1 < NI:
            qk(idx + 1)
        pvmm(idx)
        if kt == 17:
            post_a(h, qh)
            pend.append((idx + 4, h, qh))
        if pend and pend[0][0] <= idx:
            _, h_, qh_ = pend.pop(0)
            post_b(h_, qh_)
    for _, h_, qh_ in pend:
        post_b(h_, qh_)
    if "attn" in dbg:
        dump("attn", mixT[:, 4:16, :], BmixT, [128, 12, NQ], BF16)
    P.barrier()
    if stop == "D":
        return end()

    off[0] = R0
    x1T = T("x1T", [128, 16, NQ], F32)
    Bx1 = [[Buf() for _ in range(2)] for _ in range(16)]
    allx1 = [b_ for r_ in Bx1 for b_ in r_]
    off[0] = R2
    P.dma("sp", lambda e: e.dma_start(out=x1T[:], in_=xTv[:, :, 0:NQ]), writes=allx1)
    wov = w_out.rearrange("(kc p) n -> p kc n", p=128)
    wt2 = [T("wo%d" % i, [128, 16, 512], BF16) for i in range(2)]
    Bwo = [Buf(), Buf()]
    def issue_wo(dg):
        if dg < 4:
            P.dma("pool", lambda e: e.dma_start(out=wt2[dg % 2][:], in_=wov[:, :, dg * 512:(dg + 1) * 512]), writes=[Bwo[dg % 2]])

    issue_wo(0)
    for dg in range(4):
        b = dg % 2
        issue_wo(dg + 1)
        for dci in range(4):
            dc = dg * 4 + dci
            for th in range(2):
                bank = pcnt[0] % 4
                pcnt[0] += 1
                for kc in range(16):
                    P.op("pe", lambda e, kc=kc, bank=bank, b=b, dci=dci, th=th: e.matmul(
                        psum[:, bank, :], lhsT=wt2[b][:, kc, dci * 128:(dci + 1) * 128], rhs=mixT[:, kc, th * 512:(th + 1) * 512],
                        start=(kc == 0), stop=(kc == 15)), reads=[Bwo[b]] + BmixT, writes=[Bps[bank]])
                P.op("dve", lambda e, bank=bank, dc=dc, th=th: e.scalar_tensor_tensor(
                    out=x1T[:, dc, th * 512:(th + 1) * 512], in0=psum[:, bank, :], scalar=g1[:, dc:dc + 1],
                    in1=x1T[:, dc, th * 512:(th + 1) * 512], op0=ALU.mult, op1=ALU.add), reads=[Bc], writes=[Bps[bank], Bx1[dc][th]])
    if "x1" in dbg:
        dump("x1", x1T[:], allx1, [128, 16, NQ])
    P.barrier()
    if stop == "E":
        return end()

    off[0] = R1
    fT = T("fT", [128, 16, NQ], BF16)
    BfT = [Buf(), Buf()]
    off[0] = R2
    sq = T("sq2", [128, 16, 512], BF16)
    rstd = T("rstd2", [128, 1024], F32)
    ftmp = [T("ftmp%d" % i, [128, 512], F32) for i in range(2)]
    Bft = [Buf(), Buf()]
    fc = 0
    for th in range(2):
        rms_stats(x1T[:, :, th * 512:(th + 1) * 512], allx1, 512, th, rstd[:, th * 512:(th + 1) * 512])
        for kc in range(16):
            fi = fc % 2
            fc += 1
            P.op("dve", lambda e, kc=kc, th=th, fi=fi: e.scalar_tensor_tensor(
                out=ftmp[fi][:], in0=x1T[:, kc, th * 512:(th + 1) * 512], scalar=gs2[:, kc:kc + 1], in1=rstd[:, th * 512:(th + 1) * 512],
                op0=ALU.mult, op1=ALU.mult), reads=[Brstd, Bc] + allx1, writes=[Bft[fi]])
            P.op("act", lambda e, kc=kc, th=th, fi=fi: e.activation(
                out=fT[:, kc, th * 512:(th + 1) * 512], in_=ftmp[fi][:], func=AF.Identity, bias=sh2[:, kc:kc + 1], scale=1.0),
                reads=[Bft[fi], Bc], writes=[BfT[th]])
    if "f" in dbg:
        dump("f", fT[:], BfT, [128, 16, NQ], BF16)
    P.dma("sp", lambda e: e.dma_start(out=x1_s, in_=x1T[:]), reads=allx1)
    P.barrier()
    off[0] = R2
    qpT = T("qpT", [128, 16, NQ], BF16)
    BqpT = Buf()
    kTb = T("kTb", [128, 16, 128], BF16)
    BkTb = Buf()
    baseF = off[0]
    wq2 = [T("wqt%d" % i, [128, 16, 512], BF16) for i in range(2)]
    Bwq = [Buf(), Buf()]
    P.dma("pool", lambda e: e.dma_start(out=kTb[:], in_=keysT.rearrange("h c k -> c h k")), writes=[BkTb])
    wqv = wq.rearrange("(kc p) n -> p kc n", p=128)
    def issue_wq(c4):
        if c4 < 4:
            P.dma("pool", lambda e: e.dma_start(out=wq2[c4 % 2][:], in_=wqv[:, :, c4 * 512:(c4 + 1) * 512]), writes=[Bwq[c4 % 2]])

    issue_wq(0)
    for c4 in range(4):
        b = c4 % 2
        issue_wq(c4 + 1)
        for j in range(4):
            hp = c4 * 4 + j
            for th in range(2):
                bank = pcnt[0] % 4
                pcnt[0] += 1
                for kc in range(16):
                    P.op("pe", lambda e, kc=kc, bank=bank, b=b, j=j, th=th: e.matmul(
                        psum[:, bank, :], lhsT=wq2[b][:, kc, j * 128:(j + 1) * 128], rhs=fT[:, kc, th * 512:(th + 1) * 512],
                        start=(kc == 0), stop=(kc == 15)), reads=[Bwq[b]] + BfT, writes=[Bps[bank]])
                P.op("act", lambda e, bank=bank, hp=hp, th=th: e.activation(out=qpT[:, hp, th * 512:(th + 1) * 512], in_=psum[:, bank, :], func=AF.Copy),
                     writes=[Bps[bank], BqpT])
    P.barrier()
    off[0] = baseF
    sc = [T("sc0", [128, 16, 128], F32)]
    m16 = T("m16", [128, 16, 16], F32)
    ix = T("ix", [128, 16, 16], U32)
    ixf = T("ixf", [128, 16, 16], F32)
    cand = T("cand", [128, 8, 256], F32)
    t16 = T("t16", [128, 8, 16], F32)
    pos = T("pos", [128, 8, 16], U32)
    posf = T("posf", [128, 8, 16], F32)
    jf = T("jf", [128, 8, 16], F32)
    iff = T("iff", [128, 8, 16], F32)
    ee = T("ee", [128, 8, 16], F32)
    zs = T("zs", [128, 8], F32)
    sel = [T("sel%d" % i, [128, 3, 128], F32) for i in range(2)]
    selb = [T("selb%d" % i, [128, 2, 128], BF16) for i in range(2)]
    ohs = [T("oh%d" % i, [128, 8, 16, 16], F32) for i in range(2)]
    gT = [T("gT%d" % i, [128, 128], F32) for i in range(2)]
    Em = T("Em", [128, 2, 4096], BF16)
    BEm = Buf()
    P.dma("pool", lambda e: e.dma_start(out=Em[:], in_=emat), writes=[BEm])
    SIb = [T("SIb%d" % i, [128, 4, 128], BF16) for i in range(2)]
    BSI = [Buf(), Buf()]
    for i in range(2):
        P.op("dve", lambda e, i=i: e.memset(SIb[i][:], 1.0), writes=[BSI[i]])
    off[0] = R0
    QT = 32
    PA = [T("PA%d" % i, [128, QT, 128], BF16) for i in range(2)]
    PB = [T("PB%d" % i, [128, QT, 128], BF16) for i in range(2)]
    Gs = T("Gs", [128, 128, 128], BF16)
    Bsc = [Buf()]
    Bseg = [Buf() for _ in range(16)]
    Bch = [Buf() for _ in range(8)]
    Bixf, Bmisc, BGs, Bgate = Buf(), Buf(), Buf(), Buf()
    Boh = [Buf(), Buf()]
    Bsel = [Buf(), Buf()]
    BgT = [Buf(), Buf()]
    BPA = [Buf(), Buf()]
    BPB = [Buf(), Buf()]
    m16v = m16[:].rearrange("p (h s) k -> p h s k", s=2)
    ixfv = ixf[:].rearrange("p (h s) k -> p h s k", s=2)
    candv = cand[:].rearrange("p h (i j) -> p h i j", j=16)
    iota16 = iota_f[:, 0:16]
    thr16 = T("thr16", [128, 16], F32)
    P.op("dve", lambda e: e.tensor_scalar_mul(out=thr16[:], in0=iota16, scalar1=16.0), reads=[Bc], writes=[Bc])
    Gdv = Gd.rearrange("a b t -> b a t")
    qcnt = [0]
    dcnt = [0]
    gcnt = [0]
    OH_SCALE = 1.125
    GATE_FIX = 1.0 / (OH_SCALE * OH_SCALE)

    def scores(tt):
        si = 0
        for c4 in range(4):
            for j in range(4):
                hp = c4 * 4 + j
                P.op("pe", lambda e, j=j, hp=hp: e.matmul(psum[:, 0, j * 128:(j + 1) * 128], lhsT=qpT[:, hp, tt * 128:(tt + 1) * 128],
                                                          rhs=kTb[:, hp, :], start=True, stop=True), reads=[BqpT, BkTb], writes=[Bps[0]])
            P.op("act", lambda e, c4=c4: e.activation(out=sc[si][:, c4 * 4:(c4 + 1) * 4, :], in_=psum[:, 0, :].rearrange("p (j k) -> p j k", k=128), func=AF.Copy),
                 writes=[Bps[0], Bsc[si]])

    def topk(tt):
        si = tt % 2
        scur = sc[0]
        scores(tt)
        if "sc" in dbg and tt == 0:
            dump("sc", scur[:], [Bsc[0]], [128, 16, 128])
        R16 = range(16)
        for hp in R16:
            P.op("dve", lambda e, hp=hp: e.max(out=m16[:, hp, 0:8], in_=scur[:, hp, :]), reads=[Bsc[0]], writes=[Bseg[hp]])
        for hp in R16:
            P.op("dve", lambda e, hp=hp: e.max_index(out=ix[:, hp, 0:8], in_max=m16[:, hp, 0:8], in_values=scur[:, hp, :]), reads=[Bsc[0]], writes=[Bseg[hp]])
        for hp in R16:
            P.op("dve", lambda e, hp=hp: e.match_replace(out=scur[:, hp, :], in_to_replace=m16[:, hp, 0:8], in_values=scur[:, hp, :], imm_value=-1e30), writes=[Bseg[hp], Bsc[0]])
        for hp in R16:
            P.op("dve", lambda e, hp=hp: e.max(out=m16[:, hp, 8:16], in_=scur[:, hp, :]), reads=[Bsc[0]], writes=[Bseg[hp]])
        for hp in R16:
            P.op("dve", lambda e, hp=hp: e.max_index(out=ix[:, hp, 8:16], in_max=m16[:, hp, 8:16], in_values=scur[:, hp, :]), reads=[Bsc[0]], writes=[Bseg[hp]])
        P.op("dve", lambda e: e.tensor_copy(out=ixf[:], in_=ix[:]), reads=Bseg, writes=[Bixf])
        P.op("dve", lambda e: e.tensor_tensor(out=candv, in0=m16v[:, :, 0, :].unsqueeze(3).to_broadcast([128, 8, 16, 16]),
                                              in1=m16v[:, :, 1, :].unsqueeze(2).to_broadcast([128, 8, 16, 16]), op=ALU.add), reads=Bseg, writes=Bch)
        R8 = range(8)
        for h in R8:
            P.op("dve", lambda e, h=h: e.max(out=t16[:, h, 0:8], in_=cand[:, h, :]), writes=[Bch[h]])
        for h in R8:
            P.op("dve", lambda e, h=h: e.max_index(out=pos[:, h, 0:8], in_max=t16[:, h, 0:8], in_values=cand[:, h, :]), writes=[Bch[h]])
        for h in R8:
            P.op("dve", lambda e, h=h: e.match_replace(out=cand[:, h, :], in_to_replace=t16[:, h, 0:8], in_values=cand[:, h, :], imm_value=-1e30), writes=[Bch[h]])
        for h in R8:
            P.op("dve", lambda e, h=h: e.max(out=t16[:, h, 8:16], in_=cand[:, h, :]), writes=[Bch[h]])
        for h in R8:
            P.op("dve", lambda e, h=h: e.max_index(out=pos[:, h, 8:16], in_max=t16[:, h, 8:16], in_values=cand[:, h, :]), writes=[Bch[h]])
        selc = sel[si]
        P.op("dve", lambda e: e.tensor_copy(out=posf[:], in_=pos[:]), reads=Bch, writes=[Bmisc])
        P.op("dve", lambda e: e.tensor_tensor(
            out=ohs[0][:], in0=posf[:].unsqueeze(3).to_broadcast([128, 8, 16, 16]),
            in1=thr16[:].unsqueeze(1).unsqueeze(1).to_broadcast([128, 8, 16, 16]), op=ALU.is_ge), reads=[Bc, Bmisc], writes=[Boh[0]])
        P.op("dve", lambda e: e.tensor_reduce(out=iff[:], in_=ohs[0][:], axis=AX.X, op=ALU.add), reads=[Boh[0]], writes=[Bmisc])
        P.op("dve", lambda e: e.tensor_scalar_add(out=iff[:], in0=iff[:], scalar1=-1.0), writes=[Bmisc])
        P.op("dve", lambda e: e.scalar_tensor_tensor(out=jf[:], in0=iff[:], scalar=-16.0, in1=posf[:], op0=ALU.mult, op1=ALU.add), writes=[Bmisc])
        sides = ((0, iff), (1, jf))
        for side, selidx in sides:
            P.op("dve", lambda e, side=side, selidx=selidx: e.tensor_tensor(
                out=ohs[side][:], in0=iota16.unsqueeze(1).unsqueeze(1).to_broadcast([128, 8, 16, 16]),
                in1=selidx[:].unsqueeze(3).to_broadcast([128, 8, 16, 16]), op=ALU.is_equal), reads=[Bc, Bmisc], writes=[Boh[side]])
        for side, selidx in sides:
            P.op("dve", lambda e, side=side: e.tensor_tensor(
                out=ohs[side][:], in0=ohs[side][:], in1=ixfv[:, :, side, :].unsqueeze(2).to_broadcast([128, 8, 16, 16]), op=ALU.mult),
                reads=[Bixf], writes=[Boh[side]])
        for side, selidx in sides:
            P.op("dve", lambda e, side=side: e.tensor_reduce(
                out=selc[:, side, :].rearrange("p (h k) -> p h k", k=16), in_=ohs[side][:], axis=AX.X, op=ALU.add),
                reads=[Boh[side]], writes=[Bsel[si]])
        if "sel" in dbg and tt == 0:
            dump("sel", selc[:], [Bsel[si]], [128, 3, 128])
        sb_ = selb[si]
        P.op("dve", lambda e: e.tensor_copy(out=sb_[:], in_=selc[:, 0:2, :]), writes=[Bsel[si]])
        for h2 in range(2):
            for side in range(2):
                P.dma("sp", lambda e, h2=h2, side=side: e.dma_start(out=SIb[si][0:128:2, h2 * 2 + side, :], in_=sb_[64 * h2:64 * h2 + 64, side, :]),
                      reads=[Bsel[si]], writes=[BSI[si]])

    def topk_tail(tt):
        si = tt % 2
        selc = sel[si]
        P.op("dve", lambda e: e.tensor_tensor(out=ee[:], in0=t16[:], in1=t16[:, :, 0:1].to_broadcast([128, 8, 16]), op=ALU.subtract), reads=Bch, writes=[Bgate])
        P.op("act", lambda e: e.activation(out=ee[:], in_=ee[:], func=AF.Exp), writes=[Bgate])
        P.op("dve", lambda e: e.tensor_reduce(out=zs[:], in_=ee[:], axis=AX.X, op=ALU.add), writes=[Bgate])
        P.op("dve", lambda e: e.reciprocal(out=zs[:], in_=zs[:]), writes=[Bgate])
        P.op("dve", lambda e: e.tensor_scalar_mul(out=zs[:], in0=zs[:], scalar1=GATE_FIX), writes=[Bgate])
        P.op("dve", lambda e: e.tensor_tensor(out=selc[:, 2, :].rearrange("p (h k) -> p h k", k=16), in0=ee[:],
                                              in1=zs[:].unsqueeze(2).to_broadcast([128, 8, 16]), op=ALU.mult), reads=[Bgate], writes=[Bsel[si]])
        P.op("pe", lambda e: e.transpose(out=psum[:, 0, 0:128], in_=selc[:, 2, :], identity=identf), reads=[Bsel[si], Bc], writes=[Bps[0]])
        gTc = gT[si]
        P.op("dve", lambda e: e.tensor_copy(out=gTc[:], in_=psum[:, 0, 0:128]), writes=[Bps[0], BgT[si]])

    def stages(tt):
        si = tt % 2
        gTc = gT[si]
        def dstage(q):
            tb = q * QT
            h2, qq = q // 2, q % 2
            pi = qcnt[0] % 2
            qcnt[0] += 1
            for side, PX, BPX in ((0, PA, BPA), (1, PB, BPB)):
                for p in range(4):
                    bk = 4 + 2 * (dcnt[0] % 2)
                    dcnt[0] += 1
                    for j in range(2):
                        c0 = p * 1024 + j * 512
                        P.op("pe", lambda e, side=side, bk=bk, j=j, c0=c0: e.matmul(
                            psum[:, bk + j, :], lhsT=SIb[si][:, h2 * 2 + side, :], rhs=Em[:, qq, c0:c0 + 512],
                            start=True, stop=True), reads=[BSI[si], BEm], writes=[Bps[bk + j]])
                    P.op("act", lambda e, PX=PX, p=p, bk=bk: e.activation(
                        out=PX[pi][:, p * 8:(p + 1) * 8, :].rearrange("p (j u) a -> p j (u a)", j=2), in_=psum[:, bk:bk + 2, :],
                        func=AF.Derivative_Erf, scale=4.0), writes=[Bps[bk], Bps[bk + 1], BPX[pi]])
            P.op("pool", lambda e: e.tensor_tensor(
                out=PA[pi][:], in0=PA[pi][:], in1=gTc[:, tb:tb + QT].unsqueeze(2).to_broadcast([128, QT, 128]), op=ALU.mult), reads=[BgT[si]], writes=[BPA[pi]])
            return pi

        def gstage(q, pi):
            tb = q * QT
            for t4 in range(QT // 4):
                bank = 1 + (gcnt[0] % 3)
                gcnt[0] += 1
                for u in range(4):
                    tl = t4 * 4 + u
                    P.op("pe", lambda e, bank=bank, u=u, tl=tl: e.matmul(psum[:, bank, u * 128:(u + 1) * 128], lhsT=PB[pi][:, tl, :], rhs=PA[pi][:, tl, :],
                                                                      start=True, stop=True), reads=[BPA[pi], BPB[pi]], writes=[Bps[bank]])
                tg = tb + t4 * 4
                P.op("act", lambda e, bank=bank, tg=tg: e.activation(
                    out=Gs[:, :, tg:tg + 4], in_=psum[:, bank, :].rearrange("p (t a) -> p a t", a=128), func=AF.Copy),
                    writes=[Bps[bank], BGs])

        NQ4 = 128 // QT
        pis = [dstage(0)]
        for q in range(NQ4):
            if q + 1 < NQ4:
                pis.append(dstage(q + 1))
            gstage(q, pis[q])
        for a4 in range(4):
            P.dma("sp", lambda e, a4=a4: e.dma_start(out=Gdv[:, a4 * 32:(a4 + 1) * 32, tt * 128:(tt + 1) * 128], in_=Gs[:, a4 * 32:(a4 + 1) * 32, :]),
                  reads=[BGs])

    topk(0)
    topk_tail(0)
    for tt in range(8):
        if tt + 1 < 8:
            topk(tt + 1)
        stages(tt)
        if tt + 1 < 8:
            topk_tail(tt + 1)
    P.barrier()
    if stop == "F":
        if "G" in dbg:
            tmpg = T("tmpg", [128, 2, NQ], BF16)
            bb = Buf()
            P.dma("sp", lambda e: e.dma_start(out=tmpg[:], in_=Gdv[:, 0:2, :]), writes=[bb])
            dump("G", tmpg[:], [bb], [128, 2, NQ], BF16)
        return end()

    P.dma("sp", lambda e: e.dma_start(out=x1T[:], in_=x1_s), writes=allx1)
    off[0] = R2
    ut = [T("ut%d" % i, [128, 16, 256], BF16) for i in range(3)]
    vt = [T("vt%d" % i, [128, 4, D], BF16) for i in range(2)]
    gt = [T("gt%d" % i, [128, NQ], BF16) for i in range(3)]
    ga = [T("ga%d" % i, [128, NQ], BF16) for i in range(2)]
    AT = [T("AT%d" % i, [128, 4, NQ], BF16) for i in range(2)]
    But, Bvt, Bgt, Bga = [Buf(), Buf(), Buf()], [Buf(), Buf()], [Buf(), Buf(), Buf()], [Buf(), Buf()]
    BAT = [[Buf() for _ in range(4)] for _ in range(2)]
    uTv = uT.rearrange("(kc p) e -> p kc e", p=128)
    pvv = pv.rearrange("(c p) d -> p c d", p=128)
    NS = NE // 512

    def load_ut(cp):
        if cp < NE // 256:
            P.dma("pool", lambda e: e.dma_start(out=ut[cp % 3][:], in_=uTv[:, :, cp * 256:(cp + 1) * 256]), writes=[But[cp % 3]])

    load_ut(0)
    load_ut(1)

    def p1(s):
        si = s % 2
        for c in range(4):
            a = 4 * s + c
            cp = a // 2
            ui = cp % 3
            if a % 2 == 0:
                load_ut(cp + 2)
            if c == 3:
                P.dma("pool", lambda e: e.dma_start(out=vt[si][:], in_=pvv[:, 4 * s:4 * s + 4, :]), writes=[Bvt[si]])
            gi = a % 3
            P.dma("sp", lambda e, a=a, gi=gi: e.dma_start(out=gt[gi][:], in_=Gd[a]), writes=[Bgt[gi]])
            for th in range(2):
                bank = (a % 2) * 2 + th
                for kc in range(16):
                    P.op("pe", lambda e, kc=kc, bank=bank, ui=ui, a=a, th=th: e.matmul(
                        psum[:, bank, :], lhsT=ut[ui][:, kc, (a % 2) * 128:(a % 2) * 128 + 128], rhs=fT[:, kc, th * 512:(th + 1) * 512],
                        start=(kc == 0), stop=(kc == 15)), reads=[But[ui]] + BfT, writes=[Bps[bank]])
            gi2 = a % 2
            for th in range(2):
                bank = (a % 2) * 2 + th
                P.op("act", lambda e, bank=bank, gi2=gi2, th=th: e.activation(out=ga[gi2][:, th * 512:(th + 1) * 512], in_=psum[:, bank, :], func=AF.Gelu),
                     writes=[Bps[bank], Bga[gi2]])
            P.op("dve", lambda e, gi=gi, gi2=gi2, c=c, si=si: e.tensor_tensor(out=AT[si][:, c, :], in0=ga[gi2][:], in1=gt[gi][:], op=ALU.mult),
                 reads=[Bga[gi2], Bgt[gi]], writes=[BAT[si][c]])

    acnt = [0]

    def p2(s):
        si = s % 2
        for th in range(2):
            for dp in range(8):
                pair = acnt[0] % 2
                acnt[0] += 1
                for c in range(4):
                    for u in range(2):
                        dc = dp * 2 + u
                        bank = 4 + pair * 2 + u
                        P.op("pe", lambda e, c=c, dc=dc, bank=bank, th=th: e.matmul(
                            psum[:, bank, :], lhsT=vt[si][:, c, dc * 128:(dc + 1) * 128], rhs=AT[si][:, c, th * 512:(th + 1) * 512],
                            start=(c == 0), stop=(c == 3)), reads=[Bvt[si], BAT[si][c]], writes=[Bps[bank]])
                for u in range(2):
                    dc = dp * 2 + u
                    bank = 4 + pair * 2 + u
                    P.op("dve", lambda e, dc=dc, bank=bank, th=th: e.scalar_tensor_tensor(
                        out=x1T[:, dc, th * 512:(th + 1) * 512], in0=psum[:, bank, :], scalar=g2[:, dc:dc + 1],
                        in1=x1T[:, dc, th * 512:(th + 1) * 512], op0=ALU.mult, op1=ALU.add), reads=[Bc], writes=[Bps[bank], Bx1[dc][th]])

    nsuper = NS if stop != "G1" else 2
    p1(0)
    for s in range(nsuper):
        if s + 1 < nsuper:
            p1(s + 1)
        p2(s)
    if "x2" in dbg:
        dump("x2", x1T[:], allx1, [128, 16, NQ])
    P.barrier()

    off[0] = R2
    sq = T("sq3", [128, 16, 512], BF16)
    rstd = T("rstd3", [128, 1024], F32)
    ob = [T("ob%d" % i, [128, 16, 512], F32) for i in range(1)]
    Bob = Buf()
    outTv = outT.rearrange("(kc p) t -> p kc t", p=128)
    for th in range(2):
        rms_stats(x1T[:, :, th * 512:(th + 1) * 512], allx1, 512, th, rstd[:, th * 512:(th + 1) * 512])
        for kc in range(16):
            P.op("dve", lambda e, kc=kc, th=th: e.scalar_tensor_tensor(
                out=ob[0][:, kc, :], in0=x1T[:, kc, th * 512:(th + 1) * 512], scalar=fg[:, kc:kc + 1], in1=rstd[:, th * 512:(th + 1) * 512],
                op0=ALU.mult, op1=ALU.mult), reads=[Brstd, Bc] + allx1, writes=[Bob])
        P.dma("sp", lambda e, th=th: e.dma_start(out=outTv[:, :, th * 512:(th + 1) * 512], in_=ob[0][:]), reads=[Bob])
    return end()


def _rope_tables(pos):
    inv = (10000.0 ** (-np.arange(16, dtype=np.float32) / 16)).astype(np.float32)
    row = (pos // 64).astype(np.float32)
    col = (pos % 64).astype(np.float32)
    C = np.zeros((128, 2048), np.float32)
    S = np.zeros((128, 2048), np.float32)
    for p in range(128):
        j = p % 64
        axis, r = j // 32, j % 32
        hf, i = r // 16, r % 16
        ang = (row if axis == 0 else col) * inv[i]
        C[p] = np.cos(ang)
        S[p] = np.sin(ang) * (-1.0 if hf == 0 else 1.0)
    return C, S


def _emat():
    e = np.zeros((64, 2, 2, 32, 128), np.float32)
    bb = np.arange(128, dtype=np.float32)
    for tp in range(64):
        q, t = tp // 32, tp % 32
        e[tp, 0, q, t, :] = 1.0
        e[tp, 1, q, t, :] = -bb
    return np.ascontiguousarray(e.reshape(128, 2, 4096))


def _core_inputs(k, I, shared):
    b, half = k // 2, k % 2
    own = slice(half * 1024, half * 1024 + 1024)
    oth = slice((1 - half) * 1024, (1 - half) * 1024 + 1024)
    x = I["x"][b]
    xT = np.ascontiguousarray(np.concatenate([x[own], x[oth], I["ctx"][b]], 0).T)
    cv = np.stack([I["c"][b].reshape(16, 128).T, I["c_ctx"].reshape(16, 128).T], -1).reshape(128, 32)
    pos = np.concatenate([np.arange(2048)[own], np.arange(2048)[oth]])
    C, S = _rope_tables(pos)
    t = np.arange(2048)
    ic = np.zeros((4, 1024), np.float32)
    for g, w in enumerate((2, 4, 8, 16)):
        lo = np.clip(t - w // 2, 0, 2048)
        hi = np.clip(t + w // 2, 0, 2048)
        ic[g] = (1.0 / (hi - lo).astype(np.float32))[own]
    halo = np.array([1.0 if half == 1 else 0.0, 1.0 if half == 0 else 0.0], np.float32)
    smallv = np.concatenate([shared["smallv"], np.broadcast_to(halo, (128, 2))], 1)
    d = dict(shared["common"])
    d.update(xT=xT, cvec=np.ascontiguousarray(cv, np.float32), smallv=np.ascontiguousarray(smallv, np.float32),
             ropeC=C, ropeS=S, icnt=np.ascontiguousarray(np.broadcast_to(ic.reshape(1, 4096), (128, 4096))))
    return d


def _shared(I):
    f = lambda a: np.ascontiguousarray(a, dtype=np.float32)
    pc = lambda v, n: v.reshape(n, 128).T
    smallv = np.concatenate([pc(I["ada_b"][0], 96), pc(I["norm1_g"][0], 16), pc(I["norm2_g"][0], 16), pc(I["final_g"], 16),
                             I["pool_b"][0].T, pc(I["pool_scale"][0], 4)], 1)
    pm = np.zeros((128, 128), np.float32)
    for m in range(128):
        r = (m % 64) % 32
        pm[m + 16 if r < 16 else m - 16, m] = 1.0
    cmat = np.concatenate([np.eye(128, dtype=np.float32), pm], 1)
    common = dict(
        ada_w=f(I["ada_w"][0]), w_in=f(I["w_in"][0]), pool_w=f(I["pool_w"][0]),
        dlam=f(np.broadcast_to(I["diff_lambda"][0].reshape(1, 256), (128, 256))),
        subg=f(np.broadcast_to(I["subln_g"][0].reshape(1, 128), (128, 128))),
        w_out=f(I["w_out"][0]), wq=f(I["peer_wq"][0]),
        keysT=f(I["peer_keys"][0].reshape(16, 128, 128).transpose(0, 2, 1)),
        uT=f(I["peer_u"][0].T), pv=f(I["peer_v"][0]), cmat=f(cmat), emat=_emat())
    return dict(smallv=f(smallv), common=common)


_NC = None


def kernel(**inputs):
    global _NC
    I = {k: np.asarray(v) for k, v in inputs.items()}
    if _NC is None:
        _NC = build()[0]
    shared = _shared(I)
    in_maps = [_core_inputs(k, I, shared) for k in range(8)]
    res = run_bass_kernel_spmd(_NC, in_maps, core_ids=list(range(8)))
    out = np.empty((4, 2048, 2048), np.float32)
    for k in range(8):
        b, half = k // 2, k % 2
        out[b, half * 1024:(half + 1) * 1024, :] = res.results[k]["outT"].T
    return out
```

```python
import math
import numpy as np
import concourse.bass as bass
import concourse.mybir as mybir
from concourse.bass_utils import run_bass_kernel_spmd
from contextlib import ExitStack

F32 = mybir.dt.float32
BF16 = mybir.dt.bfloat16
I32 = mybir.dt.int32
U32 = mybir.dt.uint32
AF = mybir.ActivationFunctionType
ALU = mybir.AluOpType
AX = mybir.AxisListType

D = 2048
NT = 2304
NQ = 1024
NE = 16384
EPS = 1e-6
LAM_INIT = 0.8 - 0.6 * math.exp(0.0)


class Buf:
    __slots__ = ("name", "w", "r")

    def __init__(self, name=""):
        self.name = name
        self.w = None
        self.r = []


class Prog:
    CE = ("act", "dve", "pool", "pe")
    NDS = 12

    def __init__(self, nc, stack):
        self.nc = nc
        self.eng = {"sp": nc.sync, "act": nc.scalar, "dve": nc.vector, "pool": nc.gpsimd, "pe": nc.tensor}
        self.cnt = {e: 0 for e in self.CE}
        self.sem = {("c", e): stack.enter_context(nc.semaphore("c_" + e)) for e in self.CE}
        self.dcnt = {}
        self.dnext = {}
        for q in ("sp", "act", "pool"):
            for i in range(self.NDS):
                self.sem[("d", q, i)] = stack.enter_context(nc.semaphore("d_%s%d" % (q, i)))
                self.dcnt[(q, i)] = 0
            self.dnext[q] = 0
        self.known = {e: {} for e in self.eng}
        self.nops = 0

    def _deps(self, eng, reads, writes):
        deps = {}

        def add(ev):
            if ev is None:
                return
            k, v = ev
            if eng == "pe" and k == ("c", "pe"):
                return
            if deps.get(k, 0) < v:
                deps[k] = v
        for b in reads:
            add(b.w)
        for b in writes:
            add(b.w)
            for ev in b.r:
                add(ev)
        out = []
        kn = self.known[eng]
        for k, v in deps.items():
            if kn.get(k, 0) < v:
                kn[k] = v
                out.append((k, v))
        return out

    def _commit(self, ev, reads, writes):
        for b in reads:
            b.r.append(ev)
        for b in writes:
            b.w = ev
            b.r = []

    def _issue(self, eng, waits, fn, inc):
        e = self.eng[eng]
        for k, v in waits:
            e.wait_ge(self.sem[k], v)
        if fn is not None:
            fn(e).then_inc(self.sem[inc[0]], inc[1])
        self.nops += 1

    def op(self, eng, fn, reads=(), writes=()):
        waits = self._deps(eng, reads, writes)
        self.cnt[eng] += 1
        ev = (("c", eng), self.cnt[eng])
        self._commit(ev, reads, writes)
        self._issue(eng, waits, fn, (("c", eng), 1))
        return ev

    def dma(self, q, fn, reads=(), writes=()):
        waits = self._deps(q, reads, writes)
        i = self.dnext[q]
        self.dnext[q] = (i + 1) % self.NDS
        k = ("d", q, i)
        prev = self.dcnt[(q, i)]
        if prev > 0 and self.known[q].get(k, 0) < prev:
            self.known[q][k] = prev
            waits.append((k, prev))
        self.dcnt[(q, i)] = prev + 16
        ev = (k, prev + 16)
        self._commit(ev, reads, writes)
        self._issue(q, waits, fn, (k, 16))
        return ev

    def _all(self):
        waits = [(("d", q, i), v) for (q, i), v in self.dcnt.items() if v > 0]
        waits += [(("c", e), self.cnt[e]) for e in self.CE if self.cnt[e] > 0]
        return waits

    def barrier(self):
        allw = self._all()
        for eng in self.eng:
            kn = self.known[eng]
            w = []
            for k, v in allw:
                if kn.get(k, 0) < v:
                    kn[k] = v
                    w.append((k, v))
            self._issue(eng, w, None, None)

    def finish(self):
        self.barrier()


def build(stop=None, dbg=()):
    nc = bass.Bass("TRN2", target_bir_lowering=False)
    din = lambda n, s, dt=F32: nc.dram_tensor(n, list(s), dt, kind="ExternalInput").ap()
    xT = din("xT", [D, NT])
    cvec = din("cvec", [128, 32])
    ada_w = din("ada_w", [D, 6 * D])
    smallv = din("smallv", [128, 96 + 48 + 8 + 2])
    w_in = din("w_in", [D, 5120])
    pool_w = din("pool_w", [4, 128, 128])
    dlam = din("dlam", [128, 256])
    subg = din("subg", [128, 128])
    w_out = din("w_out", [D, D])
    wq = din("wq", [D, D])
    keysT = din("keysT", [16, 128, 128])
    uT = din("uT", [D, NE])
    pv = din("pv", [NE, D])
    ropeC = din("ropeC", [128, 2048])
    ropeS = din("ropeS", [128, 2048])
    cmat = din("cmat", [128, 256])
    icnt = din("icnt", [128, 4096])
    emat = din("emat", [128, 2, 4096])
    outT = nc.dram_tensor("outT", [D, NQ], F32, kind="ExternalOutput").ap()
    kT_s = nc.dram_tensor("kT_s", [12, 128, NT], BF16, kind="Internal").ap()
    qT_s = nc.dram_tensor("qT_s", [12, 128, NQ], BF16, kind="Internal").ap()
    V_s = nc.dram_tensor("V_s", [NT, 1536], BF16, kind="Internal").ap()
    Gd = nc.dram_tensor("Gd", [128, 128, NQ], BF16, kind="Internal").ap()
    x1_s = nc.dram_tensor("x1_s", [128, 16, NQ], F32, kind="Internal").ap()
    dbg_out = {}

    st = ExitStack()
    P = Prog(nc, st)
    off = [16384]

    def T(name, shape, dt, at=None):
        n = 1
        for s in shape[1:]:
            n *= s
        nb = n * (2 if dt == BF16 else 4)
        nb = (nb + 63) // 64 * 64
        if at is None:
            at = off[0]
            off[0] = at + nb
        assert at + nb <= 229000, (name, at, nb)
        return nc.alloc_sbuf_tensor_at(name, list(shape), dt, offset=at)

    def dump(name, ap, buf, shape, dt=F32):
        d = nc.dram_tensor("dbg_" + name, list(shape), dt, kind="ExternalOutput").ap()
        dbg_out[name] = d
        P.dma("sp", lambda e: e.dma_start(out=d, in_=ap), reads=buf)

    psum = nc.alloc_psum_tensor("psum", [128, 8, 512], F32)
    Bps = [Buf("ps%d" % i) for i in range(8)]

    def end():
        P.finish()
        st.close()
        return nc, dbg_out

    smv = T("smv", [128, 154], F32)
    cm = T("cm", [128, 256], F32)
    cvs = T("cvs", [128, 32], F32)
    dl = T("dl", [128, 256], F32)
    sgb = T("sgb", [128, 128], F32)
    identb = T("identb", [128, 128], BF16)
    pmb = T("pmb", [128, 128], BF16)
    onesb = T("onesb", [128, 128], BF16)
    modT = T("modT", [128, 96, 2], F32)
    gs1 = T("gs1", [128, 16, 2], F32)
    gs2 = T("gs2", [128, 16], F32)
    lamt = T("lamt", [128, 8], F32)
    iota_i = T("iota_i", [128, 128], I32)
    iota_f = T("iota_f", [128, 128], F32)
    Bc = Buf("consts")
    for t, d in ((smv, smallv), (cm, cmat), (cvs, cvec), (dl, dlam), (sgb, subg)):
        P.dma("sp", lambda e, t=t, d=d: e.dma_start(out=t[:], in_=d), writes=[Bc])
    identf = cm[:, 0:128]
    P.op("dve", lambda e: e.tensor_copy(out=identb[:], in_=cm[:, 0:128]), reads=[Bc], writes=[Bc])
    P.op("dve", lambda e: e.tensor_copy(out=pmb[:], in_=cm[:, 128:256]), reads=[Bc], writes=[Bc])
    P.op("dve", lambda e: e.memset(onesb[:], 1.0), writes=[Bc])
    epsb = T("epsb", [128, 1], F32)
    P.op("dve", lambda e: e.memset(epsb[:], EPS), writes=[Bc])
    P.op("pool", lambda e: e.iota(iota_i[:], pattern=[[1, 128]], base=0, channel_multiplier=0), writes=[Bc])
    P.op("dve", lambda e: e.tensor_copy(out=iota_f[:], in_=iota_i[:]), reads=[Bc], writes=[Bc])
    P.op("dve", lambda e: e.tensor_scalar_mul(out=sgb[:], in0=sgb[:], scalar1=1.0 - LAM_INIT), writes=[Bc])
    dlv = dl[:].rearrange("p (a b c) -> p a b c", a=2, b=2)
    prod = T("prod", [128, 2, 64], F32)
    P.op("dve", lambda e: e.tensor_tensor(out=prod[:], in0=dlv[:, :, 0, :], in1=dlv[:, :, 1, :], op=ALU.mult), writes=[Bc])
    P.op("dve", lambda e: e.tensor_reduce(out=lamt[:, 0:2], in_=prod[:], axis=AX.X, op=ALU.add), writes=[Bc])
    P.op("act", lambda e: e.activation(out=lamt[:, 2:4], in_=lamt[:, 0:2], func=AF.Exp), writes=[Bc])
    P.op("dve", lambda e: e.tensor_tensor(out=lamt[:, 4:5], in0=lamt[:, 3:4], in1=lamt[:, 2:3], op=ALU.subtract), writes=[Bc])
    P.op("dve", lambda e: e.tensor_scalar_add(out=lamt[:, 4:5], in0=lamt[:, 4:5], scalar1=-LAM_INIT), writes=[Bc])
    nlam = lamt[:, 4:5]
    adab = smv[:, 0:96]
    n1g = smv[:, 96:112]
    n2g = smv[:, 112:128]
    fg = smv[:, 128:144]
    poolb = smv[:, 144:148]
    pools = smv[:, 148:152]
    halo = smv[:, 152:154]
    P.op("act", lambda e: e.activation(out=cvs[:], in_=cvs[:], func=AF.Silu), writes=[Bc])
    base0 = off[0]
    R0 = base0
    R1 = R0 + 72 * 1024
    R2 = R1 + 32 * 1024

    off[0] = R1
    aw = [T("aw%d" % i, [128, 16, 512], F32) for i in range(2)]
    Baw = [Buf(), Buf()]
    modrow = T("modrow", [2, 6 * D], F32)
    Bmr = Buf()
    awv = ada_w.rearrange("(kc p) n -> p kc n", p=128)
    csv = cvs[:].rearrange("p (k r) -> p k r", r=2)
    for nt in range(24):
        b = nt % 2
        bank = nt % 4
        P.dma("sp", lambda e, nt=nt, b=b: e.dma_start(out=aw[b][:], in_=awv[:, :, nt * 512:(nt + 1) * 512]), writes=[Baw[b]])
        for kc in range(16):
            P.op("pe", lambda e, kc=kc, b=b, bank=bank: e.matmul(
                psum[0:2, bank, :], lhsT=csv[:, kc, :], rhs=aw[b][:, kc, :],
                start=(kc == 0), stop=(kc == 15)), reads=[Baw[b], Bc], writes=[Bps[bank]])
        P.op("act", lambda e, nt=nt, bank=bank: e.activation(out=modrow[:, nt * 512:(nt + 1) * 512], in_=psum[0:2, bank, :], func=AF.Copy),
             writes=[Bps[bank], Bmr])
    for j in range(96):
        P.op("pe", lambda e, j=j: e.transpose(out=psum[:, 4, 2 * j:2 * j + 2], in_=modrow[:, j * 128:(j + 1) * 128], identity=cm[0:2, 0:2]),
             reads=[Bmr, Bc], writes=[Bps[4]])
    P.op("dve", lambda e: e.tensor_tensor(
        out=modT[:], in0=psum[:, 4, 0:192].rearrange("p (j r) -> p j r", r=2),
        in1=adab.unsqueeze(2).to_broadcast([128, 96, 2]), op=ALU.add), reads=[Bc], writes=[Bps[4], Bc])
    P.op("dve", lambda e: e.tensor_scalar_add(out=gs1[:], in0=modT[:, 16:32, :], scalar1=1.0), writes=[Bc])
    P.op("dve", lambda e: e.tensor_tensor(out=gs1[:], in0=gs1[:], in1=n1g.unsqueeze(2).to_broadcast([128, 16, 2]), op=ALU.mult), writes=[Bc])
    P.op("dve", lambda e: e.tensor_scalar_add(out=gs2[:], in0=modT[:, 64:80, 0], scalar1=1.0), writes=[Bc])
    P.op("dve", lambda e: e.tensor_tensor(out=gs2[:], in0=gs2[:], in1=n2g, op=ALU.mult), writes=[Bc])
    sh1 = modT[:, 0:16, :]
    g1 = modT[:, 32:48, 0]
    sh2 = modT[:, 48:64, 0]
    g2 = modT[:, 80:96, 0]
    if "mod" in dbg:
        dump("mod", modT[:], [Bc], [128, 96, 2])
        dump("lam", lamt[:], [Bc], [128, 8])
    P.barrier()
    if stop == "A":
        return end()

    off[0] = R0
    hT = T("hT", [128, 16, NT], BF16)
    BhT = [Buf() for _ in range(5)]
    off[0] = R1
    xt = [T("xt%d" % i, [128, 16, 512], F32) for i in range(2)]
    Bxt = [[Buf() for _ in range(16)] for _ in range(2)]
    sq = T("sq", [128, 16, 512], BF16)
    Bsq = Buf()
    rstd = T("rstd", [128, 1024], F32)
    Brstd = Buf()
    xTv = xT.rearrange("(kc p) t -> p kc t", p=128)

    def rms_stats(src, Bsrc, n, ps_bank, rs_ap, sq_on_dve=False):
        if sq_on_dve:
            P.op("dve", lambda e: e.tensor_tensor(out=sq[:, :, 0:n], in0=src, in1=src, op=ALU.mult), reads=Bsrc, writes=[Bsq])
        else:
            P.op("act", lambda e: e.activation(out=sq[:, :, 0:n], in_=src, func=AF.Square), reads=Bsrc, writes=[Bsq])
        for kc in range(16):
            P.op("pe", lambda e, kc=kc: e.matmul(psum[:, ps_bank, 0:n], lhsT=onesb[:], rhs=sq[:, kc, 0:n],
                                                 start=(kc == 0), stop=(kc == 15)), reads=[Bsq, Bc], writes=[Bps[ps_bank]])
        P.op("act", lambda e: e.activation(out=rs_ap, in_=psum[:, ps_bank, 0:n], func=AF.Sqrt, bias=epsb[:], scale=1.0 / D),
             reads=[Bc], writes=[Bps[ps_bank], Brstd])
        P.op("dve", lambda e: e.reciprocal(out=rs_ap, in_=rs_ap), writes=[Brstd])

    for g in range(5):
        b = g % 2
        n = 512 if g < 4 else 256
        t0 = g * 512
        r = 0 if g < 4 else 1
        P.dma("sp", lambda e, b=b, t0=t0, n=n: e.dma_start(out=xt[b][:, :, 0:n], in_=xTv[:, :, t0:t0 + n]), writes=Bxt[b])
        rms_stats(xt[b][:, :, 0:n], Bxt[b], n, g % 2, rstd[:, 0:n], sq_on_dve=True)
        for kc in range(16):
            P.op("dve", lambda e, b=b, kc=kc, n=n, r=r: e.scalar_tensor_tensor(
                out=xt[b][:, kc, 0:n], in0=xt[b][:, kc, 0:n], scalar=gs1[:, kc, r:r + 1], in1=rstd[:, 0:n],
                op0=ALU.mult, op1=ALU.mult), reads=[Brstd, Bc], writes=[Bxt[b][kc]])
            P.op("act", lambda e, b=b, kc=kc, n=n, r=r, t0=t0: e.activation(
                out=hT[:, kc, t0:t0 + n], in_=xt[b][:, kc, 0:n], func=AF.Identity, bias=sh1[:, kc, r:r + 1], scale=1.0),
                reads=[Bxt[b][kc], Bc], writes=[BhT[g]])
    if "h" in dbg:
        dump("h", hT[:, :, 0:512], BhT, [128, 16, 512], BF16)
    P.barrier()
    if stop == "B":
        return end()

    off[0] = R1
    mixT = T("mixT", [128, 16, NQ], BF16)
    BmixT = [Buf() for _ in range(16)]
    off[0] = R2
    wt = [T("wt%d" % i, [128, 16, 512], BF16) for i in range(2)]
    Bwt = [Buf(), Buf()]
    rC = T("rC", [128, 2048], F32)
    rS = T("rS", [128, 2048], F32)
    Brope = Buf()
    P.dma("sp", lambda e: e.dma_start(out=rC[:], in_=ropeC), writes=[Brope])
    P.dma("sp", lambda e: e.dma_start(out=rS[:], in_=ropeS), writes=[Brope])
    wv = w_in.rearrange("(kc p) n -> p kc n", p=128)
    wcnt = [0]

    worder = [3584, 4096, 4608] + [c for hg in range(3) for c in (512 + hg * 512, 2048 + hg * 512)] + [0]

    def issue_w(i):
        if i < len(worder):
            c0 = worder[i]
            P.dma("pool", lambda e: e.dma_start(out=wt[i % 2][:], in_=wv[:, :, c0:c0 + 512]), writes=[Bwt[i % 2]])

    issue_w(0)

    def load_w(c0):
        i = wcnt[0]
        assert worder[i] == c0
        wcnt[0] += 1
        issue_w(i + 1)
        return i % 2

    NSTG = 8
    stg = [T("stg%d" % i, [128, 512], BF16) for i in range(NSTG)]
    Bstg = [Buf() for _ in range(NSTG)]
    scnt = [0]
    pcnt = [0]
    V_sv = V_s.rearrange("(tt p) c -> p tt c", p=128)
    for vc in range(3):
        b = load_w(3584 + vc * 512)
        for tt in range(18):
            bank = pcnt[0] % 4
            pcnt[0] += 1
            for kc in range(16):
                P.op("pe", lambda e, kc=kc, tt=tt, bank=bank, b=b: e.matmul(
                    psum[:, bank, :], lhsT=hT[:, kc, tt * 128:(tt + 1) * 128], rhs=wt[b][:, kc, :],
                    start=(kc == 0), stop=(kc == 15)), reads=[Bwt[b]] + BhT, writes=[Bps[bank]])
            s = scnt[0] % NSTG
            scnt[0] += 1
            P.op("act", lambda e, s=s, bank=bank: e.activation(out=stg[s][:], in_=psum[:, bank, :], func=AF.Copy),
                 writes=[Bps[bank], Bstg[s]])
            P.dma("sp", lambda e, s=s, tt=tt, vc=vc: e.dma_start(out=V_sv[:, tt, vc * 512:(vc + 1) * 512], in_=stg[s][:]),
                  reads=[Bstg[s]])
    t1 = [T("t1_%d" % i, [128, 512], F32) for i in range(2)]
    t2 = [T("t2_%d" % i, [128, 512], F32) for i in range(2)]
    Bt1 = [Buf(), Buf()]
    Bt2 = [Buf(), Buf()]
    rcnt = [0]

    def c2_tail(s, bank, t0, n, dst):
        if t0 >= 2048:
            P.dma("sp", lambda e: e.dma_start(out=dst, in_=stg[s][:, 0:n]), reads=[Bstg[s]])
            return
        rb = 4 + (rcnt[0] % 2)
        ri = rcnt[0] % 2
        rcnt[0] += 1
        P.op("pe", lambda e: e.matmul(psum[:, rb, :], lhsT=pmb[:], rhs=stg[s][:], start=True, stop=True),
             reads=[Bstg[s], Bc], writes=[Bps[rb]])
        P.op("dve", lambda e: e.tensor_tensor(out=t1[ri][:], in0=psum[:, bank, :], in1=rC[:, t0:t0 + 512], op=ALU.mult),
             reads=[Brope], writes=[Bps[bank], Bt1[ri]])
        P.op("dve", lambda e: e.tensor_tensor(out=t2[ri][:], in0=psum[:, rb, :], in1=rS[:, t0:t0 + 512], op=ALU.mult),
             reads=[Brope], writes=[Bps[rb], Bt2[ri]])
        s2 = scnt[0] % NSTG
        scnt[0] += 1
        P.op("pool", lambda e: e.tensor_tensor(out=stg[s2][:], in0=t1[ri][:], in1=t2[ri][:], op=ALU.add),
             reads=[Bt1[ri], Bt2[ri]], writes=[Bstg[s2]])
        P.dma("sp", lambda e: e.dma_start(out=dst, in_=stg[s2][:]), reads=[Bstg[s2]])

    pending = None
    for hg in range(3):
        for which in range(2):
            b = load_w((512 if which == 0 else 2048) + hg * 512)
            for hh in range(4):
                h = hg * 4 + hh
                groups = [(0, 512), (512, 512)] if which == 0 else [(0, 512), (512, 512), (1024, 512), (1536, 512), (2048, 256)]
                for (t0, n) in groups:
                    bank = pcnt[0] % 4
                    pcnt[0] += 1
                    for kc in range(16):
                        P.op("pe", lambda e, kc=kc, bank=bank, b=b, hh=hh, t0=t0, n=n: e.matmul(
                            psum[:, bank, 0:n], lhsT=wt[b][:, kc, hh * 128:(hh + 1) * 128], rhs=hT[:, kc, t0:t0 + n],
                            start=(kc == 0), stop=(kc == 15)), reads=[Bwt[b]] + BhT, writes=[Bps[bank]])
                    s = scnt[0] % NSTG
                    scnt[0] += 1
                    dst = (qT_s if which == 0 else kT_s)[h, :, t0:t0 + n]
                    P.op("act", lambda e, s=s, bank=bank, n=n: e.activation(out=stg[s][:, 0:n], in_=psum[:, bank, 0:n], func=AF.Copy),
                         writes=[Bps[bank], Bstg[s]])
                    if pending is not None:
                        c2_tail(*pending)
                    pending = (s, bank, t0, n, dst)
    c2_tail(*pending)
    b = load_w(0)
    pwb = T("pwb", [128, 4, 128], BF16)
    Bpw = Buf()
    P.dma("pool", lambda e: e.dma_start(out=pwb[:], in_=pool_w.rearrange("g c d -> c g d")), writes=[Bpw])
    ic = T("ic", [128, 4096], F32)
    P.dma("sp", lambda e: e.dma_start(out=ic[:], in_=icnt), writes=[Bpw])
    zT = T("zT", [128, 1040], F32)
    sA = T("sA", [128, 1040], F32)
    sB = T("sB", [128, 1040], F32)
    ymix = T("ymix", [128, 1024], BF16)
    Bz = Buf()
    for g in range(4):
        for th in range(2):
            bank = pcnt[0] % 4
            pcnt[0] += 1
            for kc in range(16):
                P.op("pe", lambda e, kc=kc, bank=bank, th=th, g=g: e.matmul(
                    psum[:, bank, :], lhsT=wt[b][:, kc, g * 128:(g + 1) * 128], rhs=hT[:, kc, th * 512:(th + 1) * 512],
                    start=(kc == 0), stop=(kc == 15)), reads=[Bwt[b]] + BhT, writes=[Bps[bank]])
            P.op("act", lambda e, bank=bank, th=th: e.activation(out=zT[:, 8 + th * 512:8 + (th + 1) * 512], in_=psum[:, bank, :], func=AF.Copy),
                 writes=[Bps[bank], Bz])
        bank = pcnt[0] % 4
        pcnt[0] += 1
        for hi, tsrc in enumerate((2040, 1024)):
            for kc in range(16):
                P.op("pe", lambda e, kc=kc, bank=bank, hi=hi, tsrc=tsrc, g=g: e.matmul(
                    psum[:, bank, hi * 8:hi * 8 + 8], lhsT=wt[b][:, kc, g * 128:(g + 1) * 128], rhs=hT[:, kc, tsrc:tsrc + 8],
                    start=(kc == 0), stop=(kc == 15)), reads=[Bwt[b]] + BhT, writes=[Bps[bank]])
        P.op("dve", lambda e, bank=bank: e.tensor_scalar_mul(out=zT[:, 0:8], in0=psum[:, bank, 0:8], scalar1=halo[:, 0:1]), reads=[Bc], writes=[Bps[bank], Bz])
        P.op("dve", lambda e, bank=bank: e.tensor_scalar_mul(out=zT[:, 1032:1040], in0=psum[:, bank, 8:16], scalar1=halo[:, 1:2]), reads=[Bc], writes=[Bps[bank], Bz])
        w = 2 << g
        src = zT
        ln = 1040
        step = 1
        bufs = [sA, sB]
        bi = 0
        while step < w:
            dst_ = bufs[bi]
            bi ^= 1
            ln2 = ln - step
            P.op("dve", lambda e, src=src, dst_=dst_, ln2=ln2, step=step: e.tensor_tensor(
                out=dst_[:, 0:ln2], in0=src[:, 0:ln2], in1=src[:, step:step + ln2], op=ALU.add), writes=[Bz])
            src = dst_
            ln = ln2
            step *= 2
        o0 = 8 - w // 2
        dst_ = bufs[bi]
        P.op("dve", lambda e, src=src, dst_=dst_, o0=o0, g=g: e.tensor_tensor(
            out=dst_[:, 0:1024], in0=src[:, o0:o0 + 1024], in1=ic[:, g * 1024:(g + 1) * 1024], op=ALU.mult), reads=[Bpw], writes=[Bz])
        P.op("dve", lambda e, dst_=dst_: e.tensor_tensor(out=ymix[:], in0=dst_[:, 0:1024], in1=zT[:, 8:1032], op=ALU.subtract), writes=[Bz])
        for th in range(2):
            bank = pcnt[0] % 4
            pcnt[0] += 1
            P.op("pe", lambda e, bank=bank, th=th, g=g: e.matmul(psum[:, bank, :], lhsT=pwb[:, g, :], rhs=ymix[:, th * 512:(th + 1) * 512],
                                                             start=True, stop=True), reads=[Bpw, Bz], writes=[Bps[bank]])
            P.op("dve", lambda e, bank=bank, th=th, g=g: e.tensor_scalar(
                out=mixT[:, g, th * 512:(th + 1) * 512], in0=psum[:, bank, :], scalar1=poolb[:, g:g + 1], scalar2=pools[:, g:g + 1],
                op0=ALU.add, op1=ALU.mult), reads=[Bc], writes=[Bps[bank], BmixT[g]])
    if "pool" in dbg:
        dump("pool", mixT[:, 0:4, :], BmixT, [128, 4, NQ], BF16)
    P.barrier()
    if stop == "C":
        if "qkv" in dbg:
            off[0] = R2
            for nm, src_ in (("qT0", qT_s[0]), ("kT0", kT_s[0])):
                tmpb = T(nm, list(src_.shape), BF16)
                bb = Buf()
                P.dma("sp", lambda e, tmpb=tmpb, src_=src_: e.dma_start(out=tmpb[:], in_=src_), writes=[bb])
                dump(nm, tmpb[:], [bb], list(src_.shape), BF16)
            tmpv = T("v0", [128, 18, 128], BF16)
            bb = Buf()
            P.dma("sp", lambda e: e.dma_start(out=tmpv[:], in_=V_sv[:, :, 0:128]), writes=[bb])
            dump("v0", tmpv[:], [bb], [128, 18, 128], BF16)
        return end()

    off[0] = R2
    kTh = [T("kTh%d" % i, [128, NT], BF16) for i in range(2)]
    qTh = [T("qTh%d" % i, [128, NQ], BF16) for i in range(2)]
    Vaug = [T("Vaug%d" % i, [128, 18, 129], BF16) for i in range(2)]
    Bkq = [Buf(), Buf()]
    for i in range(2):
        P.op("dve", lambda e, i=i: e.memset(Vaug[i][:, :, 128:129], 1.0), writes=[Bkq[i]])
    NEB = 3
    Eb = [T("Eb%d" % i, [128, 1024], BF16) for i in range(NEB)]
    BEb = [Buf() for _ in range(NEB)]
    posb = [T("posb%d" % i, [128, 8, 129], F32) for i in range(2)]
    Bpo = [Buf(), Buf()]
    rz = T("rz", [128, 4, 2], F32)
    nl = T("nl", [128, 4], F32)
    oo = T("oo", [128, 4, 128], F32)
    o2 = T("o2", [128, 4, 128], F32)
    ss = T("ss", [128, 8], F32)
    mhalf = T("mhalf", [128, 4], F32)
    ysb = [T("ysb%d" % i, [128, 4, 128], BF16) for i in range(2)]
    Bys = [Buf(), Buf()]
    Bpp = Buf()
    P.op("dve", lambda e: e.memset(mhalf[:], -0.5), writes=[Bpp])
    psb = psum[:, 7, 0:256].bitcast(BF16)

    def load_head(h):
        i = h % 2
        P.dma("sp", lambda e: e.dma_start(out=kTh[i][:], in_=kT_s[h]), writes=[Bkq[i]])
        P.dma("sp", lambda e: e.dma_start(out=qTh[i][:], in_=qT_s[h]), writes=[Bkq[i]])
        P.dma("sp", lambda e: e.dma_start(out=Vaug[i][:, :, 0:128], in_=V_sv[:, :, h * 128:(h + 1) * 128]), writes=[Bkq[i]])

    its = [(h, qh, kt) for h in range(12) for qh in range(2) for kt in range(18)]

    def qk(idx):
        h, qh, kt = its[idx]
        i = h % 2
        pi = idx % 2
        for m in range(2):
            bank = 2 * pi + m
            P.op("pe", lambda e, m=m, bank=bank: e.matmul(psum[:, bank, :], lhsT=kTh[i][64 * m:64 * m + 64, kt * 128:(kt + 1) * 128],
                                                          rhs=qTh[i][64 * m:64 * m + 64, qh * 512:(qh + 1) * 512], start=True, stop=True),
                 reads=[Bkq[i]], writes=[Bps[bank]])
        eb = idx % NEB
        P.op("act", lambda e: e.activation(out=Eb[eb][:].rearrange("p (m q) -> p m q", m=2), in_=psum[:, 2 * pi:2 * pi + 2, :], func=AF.Exp, scale=0.125),
             writes=[Bps[2 * pi], Bps[2 * pi + 1], BEb[eb]])

    def pvmm(idx):
        h, qh, kt = its[idx]
        i = h % 2
        eb = idx % NEB
        for m in range(2):
            for qt in range(4):
                a_ = m * 4 + qt
                bank, c0 = 4 + a_ // 3, (a_ % 3) * 129
                P.op("pe", lambda e, qt=qt, m=m, bank=bank, c0=c0, a_=a_: e.matmul(
                    psum[:, bank, c0:c0 + 129], lhsT=Eb[eb][:, m * 512 + qt * 128:m * 512 + (qt + 1) * 128],
                    rhs=Vaug[i][:, kt, :], start=(kt == 0 and a_ % 3 == 0), stop=(kt == 17), skip_group_check=True),
                    reads=[BEb[eb], Bkq[i]], writes=[Bps[bank]])

    def post_a(h, qh):
        pb_ = (h * 2 + qh) % 2
        po = posb[pb_]
        for j, na in ((0, 3), (1, 3), (2, 2)):
            P.op("dve", lambda e, j=j, na=na: e.tensor_copy(out=po[:, 3 * j:3 * j + na, :], in_=psum[:, 4 + j, 0:na * 129].rearrange("p (a c) -> p a c", c=129)),
                 writes=[Bps[4 + j], Bpo[pb_]])
        B2 = [Bpo[pb_]]
        P.op("dve", lambda e: e.reciprocal(out=rz[:], in_=po[:, :, 128].rearrange("p (m q) -> p q m", m=2)), reads=B2, writes=[Bpp])
        P.op("dve", lambda e: e.tensor_scalar_mul(out=nl[:], in0=rz[:, :, 1], scalar1=nlam), reads=[Bc], writes=[Bpp])
        P.op("dve", lambda e: e.tensor_tensor(out=oo[:], in0=po[:, 0:4, 0:128], in1=rz[:, :, 0:1].to_broadcast([128, 4, 128]), op=ALU.mult), reads=B2, writes=[Bpp])
        P.op("dve", lambda e: e.tensor_tensor(out=o2[:], in0=po[:, 4:8, 0:128], in1=nl[:].unsqueeze(2).to_broadcast([128, 4, 128]), op=ALU.mult), reads=B2, writes=[Bpp])
        P.op("dve", lambda e: e.tensor_tensor(out=oo[:], in0=oo[:], in1=o2[:], op=ALU.add), writes=[Bpp])
        P.op("dve", lambda e: e.tensor_tensor(out=o2[:], in0=oo[:], in1=oo[:], op=ALU.mult), writes=[Bpp])
        P.op("dve", lambda e: e.tensor_reduce(out=ss[:, 0:4], in_=o2[:], axis=AX.X, op=ALU.add), writes=[Bpp])
        P.op("dve", lambda e: e.tensor_scalar(out=ss[:, 0:4], in0=ss[:, 0:4], scalar1=1.0 / 128, scalar2=EPS, op0=ALU.mult, op1=ALU.add), writes=[Bpp])
        P.op("pool", lambda e: e.tensor_tensor(out=ss[:, 4:8], in0=ss[:, 0:4], in1=mhalf[:], op=ALU.pow), writes=[Bpp])
        P.op("dve", lambda e: e.tensor_tensor(out=oo[:], in0=oo[:], in1=ss[:, 4:8].unsqueeze(2).to_broadcast([128, 4, 128]), op=ALU.mult), writes=[Bpp])
        P.op("dve", lambda e: e.tensor_tensor(out=ysb[pb_][:], in0=oo[:], in1=sgb[:].unsqueeze(1).to_broadcast([128, 4, 128]), op=ALU.mult), reads=[Bc, Bpp], writes=[Bys[pb_]])

    def post_b(h, qh):
        pb_ = (h * 2 + qh) % 2
        for qt in range(4):
            P.op("pe", lambda e, qt=qt: e.transpose(out=psb[:, qt * 128:(qt + 1) * 128], in_=ysb[pb_][:, qt, :], identity=identb[:]),
                 reads=[Bys[pb_], Bc], writes=[Bps[7]])
        P.op("dve", lambda e: e.tensor_copy(out=mixT[:, 4 + h, qh * 512:(qh + 1) * 512], in_=psb), reads=[Bys[pb_]], writes=[Bps[7], BmixT[4 + h]])

    load_head(0)
    NI = len(its)
    qk(0)
    qk(1)
    pend = []
    for idx in range(NI):
        h, qh, kt = its[idx]
        if kt == 0 and qh == 0 and h + 1 < 12:
            load_head(h + 1)
        if idx + 2 < NI:
            qk(idx + 2)
        pvmm(idx)
        if kt == 17:
            post_a(h, qh)
            pend.append((idx + 11, h, qh))
        if pend and pend[0][0] <= idx:
            _, h_, qh_ = pend.pop(0)
            post_b(h_, qh_)
    for _, h_, qh_ in pend:
        post_b(h_, qh_)
    if "attn" in dbg:
        dump("attn", mixT[:, 4:16, :], BmixT, [128, 12, NQ], BF16)
    P.barrier()
    if stop == "D":
        return end()

    off[0] = R0
    x1T = T("x1T", [128, 16, NQ], F32)
    Bx1 = [[Buf() for _ in range(2)] for _ in range(16)]
    allx1 = [b_ for r_ in Bx1 for b_ in r_]
    off[0] = R2
    P.dma("sp", lambda e: e.dma_start(out=x1T[:], in_=xTv[:, :, 0:NQ]), writes=allx1)
    wov = w_out.rearrange("(kc p) n -> p kc n", p=128)
    wt2 = [T("wo%d" % i, [128, 16, 512], BF16) for i in range(2)]
    Bwo = [Buf(), Buf()]
    def issue_wo(dg):
        if dg < 4:
            P.dma("pool", lambda e: e.dma_start(out=wt2[dg % 2][:], in_=wov[:, :, dg * 512:(dg + 1) * 512]), writes=[Bwo[dg % 2]])

    issue_wo(0)
    for dg in range(4):
        b = dg % 2
        issue_wo(dg + 1)
        for dci in range(4):
            dc = dg * 4 + dci
            for th in range(2):
                bank = pcnt[0] % 4
                pcnt[0] += 1
                for kc in range(16):
                    P.op("pe", lambda e, kc=kc, bank=bank, b=b, dci=dci, th=th: e.matmul(
                        psum[:, bank, :], lhsT=wt2[b][:, kc, dci * 128:(dci + 1) * 128], rhs=mixT[:, kc, th * 512:(th + 1) * 512],
                        start=(kc == 0), stop=(kc == 15)), reads=[Bwo[b]] + BmixT, writes=[Bps[bank]])
                P.op("dve", lambda e, bank=bank, dc=dc, th=th: e.scalar_tensor_tensor(
                    out=x1T[:, dc, th * 512:(th + 1) * 512], in0=psum[:, bank, :], scalar=g1[:, dc:dc + 1],
                    in1=x1T[:, dc, th * 512:(th + 1) * 512], op0=ALU.mult, op1=ALU.add), reads=[Bc], writes=[Bps[bank], Bx1[dc][th]])
    if "x1" in dbg:
        dump("x1", x1T[:], allx1, [128, 16, NQ])
    P.barrier()
    if stop == "E":
        return end()

    off[0] = R1
    fT = T("fT", [128, 16, NQ], BF16)
    BfT = [Buf(), Buf()]
    off[0] = R2
    sq = T("sq2", [128, 16, 512], BF16)
    rstd = T("rstd2", [128, 1024], F32)
    ftmp = [T("ftmp%d" % i, [128, 512], F32) for i in range(2)]
    Bft = [Buf(), Buf()]
    fc = 0
    for th in range(2):
        rms_stats(x1T[:, :, th * 512:(th + 1) * 512], allx1, 512, th, rstd[:, th * 512:(th + 1) * 512])
        for kc in range(16):
            fi = fc % 2
            fc += 1
            P.op("dve", lambda e, kc=kc, th=th, fi=fi: e.scalar_tensor_tensor(
                out=ftmp[fi][:], in0=x1T[:, kc, th * 512:(th + 1) * 512], scalar=gs2[:, kc:kc + 1], in1=rstd[:, th * 512:(th + 1) * 512],
                op0=ALU.mult, op1=ALU.mult), reads=[Brstd, Bc] + allx1, writes=[Bft[fi]])
            P.op("act", lambda e, kc=kc, th=th, fi=fi: e.activation(
                out=fT[:, kc, th * 512:(th + 1) * 512], in_=ftmp[fi][:], func=AF.Identity, bias=sh2[:, kc:kc + 1], scale=1.0),
                reads=[Bft[fi], Bc], writes=[BfT[th]])
    if "f" in dbg:
        dump("f", fT[:], BfT, [128, 16, NQ], BF16)
    P.dma("sp", lambda e: e.dma_start(out=x1_s, in_=x1T[:]), reads=allx1)
    P.barrier()
    off[0] = R2
    qpT = T("qpT", [128, 16, NQ], BF16)
    BqpT = Buf()
    kTb = T("kTb", [128, 16, 128], BF16)
    BkTb = Buf()
    baseF = off[0]
    wq2 = [T("wqt%d" % i, [128, 16, 512], BF16) for i in range(2)]
    Bwq = [Buf(), Buf()]
    P.dma("pool", lambda e: e.dma_start(out=kTb[:], in_=keysT.rearrange("h c k -> c h k")), writes=[BkTb])
    wqv = wq.rearrange("(kc p) n -> p kc n", p=128)
    def issue_wq(c4):
        if c4 < 4:
            P.dma("pool", lambda e: e.dma_start(out=wq2[c4 % 2][:], in_=wqv[:, :, c4 * 512:(c4 + 1) * 512]), writes=[Bwq[c4 % 2]])

    issue_wq(0)
    for c4 in range(4):
        b = c4 % 2
        issue_wq(c4 + 1)
        for j in range(4):
            hp = c4 * 4 + j
            for th in range(2):
                bank = pcnt[0] % 4
                pcnt[0] += 1
                for kc in range(16):
                    P.op("pe", lambda e, kc=kc, bank=bank, b=b, j=j, th=th: e.matmul(
                        psum[:, bank, :], lhsT=wq2[b][:, kc, j * 128:(j + 1) * 128], rhs=fT[:, kc, th * 512:(th + 1) * 512],
                        start=(kc == 0), stop=(kc == 15)), reads=[Bwq[b]] + BfT, writes=[Bps[bank]])
                P.op("act", lambda e, bank=bank, hp=hp, th=th: e.activation(out=qpT[:, hp, th * 512:(th + 1) * 512], in_=psum[:, bank, :], func=AF.Copy),
                     writes=[Bps[bank], BqpT])
    P.barrier()
    off[0] = baseF
    sc = [T("sc0", [128, 16, 128], F32)]
    m16 = T("m16", [128, 16, 16], F32)
    ix = T("ix", [128, 16, 16], U32)
    ixf = T("ixf", [128, 16, 16], F32)
    cand = T("cand", [128, 8, 256], F32)
    t16 = T("t16", [128, 8, 16], F32)
    pos = T("pos", [128, 8, 16], U32)
    posf = T("posf", [128, 8, 16], F32)
    jf = T("jf", [128, 8, 16], F32)
    iff = T("iff", [128, 8, 16], F32)
    ee = T("ee", [128, 8, 16], F32)
    zs = T("zs", [128, 8], F32)
    sel = [T("sel%d" % i, [128, 3, 128], F32) for i in range(2)]
    selb = [T("selb%d" % i, [128, 2, 128], BF16) for i in range(2)]
    ohs = [T("oh%d" % i, [128, 8, 16, 16], F32) for i in range(2)]
    gT = [T("gT%d" % i, [128, 128], F32) for i in range(2)]
    Em = T("Em", [128, 2, 4096], BF16)
    BEm = Buf()
    P.dma("pool", lambda e: e.dma_start(out=Em[:], in_=emat), writes=[BEm])
    SIb = [T("SIb%d" % i, [128, 4, 128], BF16) for i in range(2)]
    BSI = [Buf(), Buf()]
    for i in range(2):
        P.op("dve", lambda e, i=i: e.memset(SIb[i][:], 1.0), writes=[BSI[i]])
    off[0] = R0
    QT = 32
    PA = [T("PA%d" % i, [128, QT, 128], BF16) for i in range(2)]
    PB = [T("PB%d" % i, [128, QT, 128], BF16) for i in range(2)]
    Gs = T("Gs", [128, 128, 128], BF16)
    Bsc = [Buf()]
    Bseg = [Buf() for _ in range(16)]
    Bch = [Buf() for _ in range(8)]
    Bixf, Bmisc, BGs, Bgate = Buf(), Buf(), Buf(), Buf()
    Boh = [Buf(), Buf()]
    Bsel = [Buf(), Buf()]
    BgT = [Buf(), Buf()]
    BPA = [Buf(), Buf()]
    BPB = [Buf(), Buf()]
    m16v = m16[:].rearrange("p (h s) k -> p h s k", s=2)
    ixfv = ixf[:].rearrange("p (h s) k -> p h s k", s=2)
    candv = cand[:].rearrange("p h (i j) -> p h i j", j=16)
    iota16 = iota_f[:, 0:16]
    thr16 = T("thr16", [128, 16], F32)
    P.op("dve", lambda e: e.tensor_scalar_mul(out=thr16[:], in0=iota16, scalar1=16.0), reads=[Bc], writes=[Bc])
    Gdv = Gd.rearrange("a b t -> b a t")
    qcnt = [0]
    dcnt = [0]
    gcnt = [0]
    OH_SCALE = 1.125
    GATE_FIX = 1.0 / (OH_SCALE * OH_SCALE)

    def scores(tt):
        si = 0
        for c4 in range(4):
            for j in range(4):
                hp = c4 * 4 + j
                P.op("pe", lambda e, j=j, hp=hp: e.matmul(psum[:, 0, j * 128:(j + 1) * 128], lhsT=qpT[:, hp, tt * 128:(tt + 1) * 128],
                                                          rhs=kTb[:, hp, :], start=True, stop=True), reads=[BqpT, BkTb], writes=[Bps[0]])
            P.op("act", lambda e, c4=c4: e.activation(out=sc[si][:, c4 * 4:(c4 + 1) * 4, :], in_=psum[:, 0, :].rearrange("p (j k) -> p j k", k=128), func=AF.Copy),
                 writes=[Bps[0], Bsc[si]])

    def topk(tt):
        si = tt % 2
        scur = sc[0]
        scores(tt)
        if "sc" in dbg and tt == 0:
            dump("sc", scur[:], [Bsc[0]], [128, 16, 128])
        R16 = range(16)
        for hp in R16:
            P.op("dve", lambda e, hp=hp: e.max(out=m16[:, hp, 0:8], in_=scur[:, hp, :]), reads=[Bsc[0]], writes=[Bseg[hp]])
        for hp in R16:
            P.op("dve", lambda e, hp=hp: e.max_index(out=ix[:, hp, 0:8], in_max=m16[:, hp, 0:8], in_values=scur[:, hp, :]), reads=[Bsc[0]], writes=[Bseg[hp]])
        for hp in R16:
            P.op("dve", lambda e, hp=hp: e.match_replace(out=scur[:, hp, :], in_to_replace=m16[:, hp, 0:8], in_values=scur[:, hp, :], imm_value=-1e30), writes=[Bseg[hp], Bsc[0]])
        for hp in R16:
            P.op("dve", lambda e, hp=hp: e.max(out=m16[:, hp, 8:16], in_=scur[:, hp, :]), reads=[Bsc[0]], writes=[Bseg[hp]])
        for hp in R16:
            P.op("dve", lambda e, hp=hp: e.max_index(out=ix[:, hp, 8:16], in_max=m16[:, hp, 8:16], in_values=scur[:, hp, :]), reads=[Bsc[0]], writes=[Bseg[hp]])
        P.op("dve", lambda e: e.tensor_copy(out=ixf[:], in_=ix[:]), reads=Bseg, writes=[Bixf])
        P.op("dve", lambda e: e.tensor_tensor(out=candv, in0=m16v[:, :, 0, :].unsqueeze(3).to_broadcast([128, 8, 16, 16]),
                                              in1=m16v[:, :, 1, :].unsqueeze(2).to_broadcast([128, 8, 16, 16]), op=ALU.add), reads=Bseg, writes=Bch)
        R8 = range(8)
        for h in R8:
            P.op("dve", lambda e, h=h: e.max(out=t16[:, h, 0:8], in_=cand[:, h, :]), writes=[Bch[h]])
        for h in R8:
            P.op("dve", lambda e, h=h: e.max_index(out=pos[:, h, 0:8], in_max=t16[:, h, 0:8], in_values=cand[:, h, :]), writes=[Bch[h]])
        for h in R8:
            P.op("dve", lambda e, h=h: e.match_replace(out=cand[:, h, :], in_to_replace=t16[:, h, 0:8], in_values=cand[:, h, :], imm_value=-1e30), writes=[Bch[h]])
        for h in R8:
            P.op("dve", lambda e, h=h: e.max(out=t16[:, h, 8:16], in_=cand[:, h, :]), writes=[Bch[h]])
        for h in R8:
            P.op("dve", lambda e, h=h: e.max_index(out=pos[:, h, 8:16], in_max=t16[:, h, 8:16], in_values=cand[:, h, :]), writes=[Bch[h]])
        selc = sel[si]
        P.op("dve", lambda e: e.tensor_copy(out=posf[:], in_=pos[:]), reads=Bch, writes=[Bmisc])
        P.op("dve", lambda e: e.tensor_tensor(
            out=ohs[0][:], in0=posf[:].unsqueeze(3).to_broadcast([128, 8, 16, 16]),
            in1=thr16[:].unsqueeze(1).unsqueeze(1).to_broadcast([128, 8, 16, 16]), op=ALU.is_ge), reads=[Bc, Bmisc], writes=[Boh[0]])
        P.op("dve", lambda e: e.tensor_reduce(out=iff[:], in_=ohs[0][:], axis=AX.X, op=ALU.add), reads=[Boh[0]], writes=[Bmisc])
        P.op("dve", lambda e: e.tensor_scalar_add(out=iff[:], in0=iff[:], scalar1=-1.0), writes=[Bmisc])
        P.op("dve", lambda e: e.scalar_tensor_tensor(out=jf[:], in0=iff[:], scalar=-16.0, in1=posf[:], op0=ALU.mult, op1=ALU.add), writes=[Bmisc])
        sides = ((0, iff), (1, jf))
        for side, selidx in sides:
            P.op("dve", lambda e, side=side, selidx=selidx: e.tensor_tensor(
                out=ohs[side][:], in0=iota16.unsqueeze(1).unsqueeze(1).to_broadcast([128, 8, 16, 16]),
                in1=selidx[:].unsqueeze(3).to_broadcast([128, 8, 16, 16]), op=ALU.is_equal), reads=[Bc, Bmisc], writes=[Boh[side]])
        for side, selidx in sides:
            P.op("dve", lambda e, side=side: e.tensor_tensor(
                out=ohs[side][:], in0=ohs[side][:], in1=ixfv[:, :, side, :].unsqueeze(2).to_broadcast([128, 8, 16, 16]), op=ALU.mult),
                reads=[Bixf], writes=[Boh[side]])
        for side, selidx in sides:
            P.op("dve", lambda e, side=side: e.tensor_reduce(
                out=selc[:, side, :].rearrange("p (h k) -> p h k", k=16), in_=ohs[side][:], axis=AX.X, op=ALU.add),
                reads=[Boh[side]], writes=[Bsel[si]])
        if "sel" in dbg and tt == 0:
            dump("sel", selc[:], [Bsel[si]], [128, 3, 128])
        sb_ = selb[si]
        P.op("dve", lambda e: e.tensor_copy(out=sb_[:], in_=selc[:, 0:2, :]), writes=[Bsel[si]])
        for h2 in range(2):
            for side in range(2):
                P.dma("sp", lambda e, h2=h2, side=side: e.dma_start(out=SIb[si][0:128:2, h2 * 2 + side, :], in_=sb_[64 * h2:64 * h2 + 64, side, :]),
                      reads=[Bsel[si]], writes=[BSI[si]])

    def topk_tail(tt):
        si = tt % 2
        selc = sel[si]
        P.op("dve", lambda e: e.tensor_tensor(out=ee[:], in0=t16[:], in1=t16[:, :, 0:1].to_broadcast([128, 8, 16]), op=ALU.subtract), reads=Bch, writes=[Bgate])
        P.op("act", lambda e: e.activation(out=ee[:], in_=ee[:], func=AF.Exp), writes=[Bgate])
        P.op("dve", lambda e: e.tensor_reduce(out=zs[:], in_=ee[:], axis=AX.X, op=ALU.add), writes=[Bgate])
        P.op("dve", lambda e: e.reciprocal(out=zs[:], in_=zs[:]), writes=[Bgate])
        P.op("dve", lambda e: e.tensor_scalar_mul(out=zs[:], in0=zs[:], scalar1=GATE_FIX), writes=[Bgate])
        P.op("dve", lambda e: e.tensor_tensor(out=selc[:, 2, :].rearrange("p (h k) -> p h k", k=16), in0=ee[:],
                                              in1=zs[:].unsqueeze(2).to_broadcast([128, 8, 16]), op=ALU.mult), reads=[Bgate], writes=[Bsel[si]])
        P.op("pe", lambda e: e.transpose(out=psum[:, 0, 0:128], in_=selc[:, 2, :], identity=identf), reads=[Bsel[si], Bc], writes=[Bps[0]])
        gTc = gT[si]
        P.op("dve", lambda e: e.tensor_copy(out=gTc[:], in_=psum[:, 0, 0:128]), writes=[Bps[0], BgT[si]])

    def stages(tt):
        si = tt % 2
        gTc = gT[si]
        def dstage(q):
            tb = q * QT
            h2, qq = q // 2, q % 2
            pi = qcnt[0] % 2
            qcnt[0] += 1
            for side, PX, BPX in ((0, PA, BPA), (1, PB, BPB)):
                for p in range(4):
                    bk = 4 + 2 * (dcnt[0] % 2)
                    dcnt[0] += 1
                    for j in range(2):
                        c0 = p * 1024 + j * 512
                        P.op("pe", lambda e, side=side, bk=bk, j=j, c0=c0: e.matmul(
                            psum[:, bk + j, :], lhsT=SIb[si][:, h2 * 2 + side, :], rhs=Em[:, qq, c0:c0 + 512],
                            start=True, stop=True), reads=[BSI[si], BEm], writes=[Bps[bk + j]])
                    P.op("act", lambda e, PX=PX, p=p, bk=bk: e.activation(
                        out=PX[pi][:, p * 8:(p + 1) * 8, :].rearrange("p (j u) a -> p j (u a)", j=2), in_=psum[:, bk:bk + 2, :],
                        func=AF.Derivative_Erf, scale=4.0), writes=[Bps[bk], Bps[bk + 1], BPX[pi]])
            P.op("pool", lambda e: e.tensor_tensor(
                out=PA[pi][:], in0=PA[pi][:], in1=gTc[:, tb:tb + QT].unsqueeze(2).to_broadcast([128, QT, 128]), op=ALU.mult), reads=[BgT[si]], writes=[BPA[pi]])
            return pi

        def gstage(q, pi):
            tb = q * QT
            for t4 in range(QT // 4):
                bank = 1 + (gcnt[0] % 3)
                gcnt[0] += 1
                for u in range(4):
                    tl = t4 * 4 + u
                    P.op("pe", lambda e, bank=bank, u=u, tl=tl: e.matmul(psum[:, bank, u * 128:(u + 1) * 128], lhsT=PB[pi][:, tl, :], rhs=PA[pi][:, tl, :],
                                                                      start=True, stop=True), reads=[BPA[pi], BPB[pi]], writes=[Bps[bank]])
                tg = tb + t4 * 4
                P.op("act", lambda e, bank=bank, tg=tg: e.activation(
                    out=Gs[:, :, tg:tg + 4], in_=psum[:, bank, :].rearrange("p (t a) -> p a t", a=128), func=AF.Copy),
                    writes=[Bps[bank], BGs])

        NQ4 = 128 // QT
        pis = [dstage(0)]
        for q in range(NQ4):
            if q + 1 < NQ4:
                pis.append(dstage(q + 1))
            gstage(q, pis[q])
        for a4 in range(4):
            P.dma("sp", lambda e, a4=a4: e.dma_start(out=Gdv[:, a4 * 32:(a4 + 1) * 32, tt * 128:(tt + 1) * 128], in_=Gs[:, a4 * 32:(a4 + 1) * 32, :]),
                  reads=[BGs])

    topk(0)
    topk_tail(0)
    for tt in range(8):
        if tt + 1 < 8:
            topk(tt + 1)
        stages(tt)
        if tt + 1 < 8:
            topk_tail(tt + 1)
    P.barrier()
    if stop == "F":
        if "G" in dbg:
            tmpg = T("tmpg", [128, 2, NQ], BF16)
            bb = Buf()
            P.dma("sp", lambda e: e.dma_start(out=tmpg[:], in_=Gdv[:, 0:2, :]), writes=[bb])
            dump("G", tmpg[:], [bb], [128, 2, NQ], BF16)
        return end()

    P.dma("sp", lambda e: e.dma_start(out=x1T[:], in_=x1_s), writes=allx1)
    off[0] = R2
    ut = [T("ut%d" % i, [128, 16, 256], BF16) for i in range(3)]
    vt = [T("vt%d" % i, [128, 4, D], BF16) for i in range(2)]
    gt = [T("gt%d" % i, [128, NQ], BF16) for i in range(3)]
    ga = [T("ga%d" % i, [128, NQ], BF16) for i in range(2)]
    AT = [T("AT%d" % i, [128, 4, NQ], BF16) for i in range(2)]
    But, Bvt, Bgt, Bga = [Buf(), Buf(), Buf()], [Buf(), Buf()], [Buf(), Buf(), Buf()], [Buf(), Buf()]
    BAT = [[Buf() for _ in range(4)] for _ in range(2)]
    uTv = uT.rearrange("(kc p) e -> p kc e", p=128)
    pvv = pv.rearrange("(c p) d -> p c d", p=128)
    NS = NE // 512

    def load_ut(cp):
        if cp < NE // 256:
            P.dma("pool", lambda e: e.dma_start(out=ut[cp % 3][:], in_=uTv[:, :, cp * 256:(cp + 1) * 256]), writes=[But[cp % 3]])

    load_ut(0)
    load_ut(1)

    def p1(s):
        si = s % 2
        for c in range(4):
            a = 4 * s + c
            cp = a // 2
            ui = cp % 3
            if a % 2 == 0:
                load_ut(cp + 2)
            if c == 3:
                P.dma("pool", lambda e: e.dma_start(out=vt[si][:], in_=pvv[:, 4 * s:4 * s + 4, :]), writes=[Bvt[si]])
            gi = a % 3
            P.dma("sp", lambda e, a=a, gi=gi: e.dma_start(out=gt[gi][:], in_=Gd[a]), writes=[Bgt[gi]])
            for th in range(2):
                bank = (a % 2) * 2 + th
                for kc in range(16):
                    P.op("pe", lambda e, kc=kc, bank=bank, ui=ui, a=a, th=th: e.matmul(
                        psum[:, bank, :], lhsT=ut[ui][:, kc, (a % 2) * 128:(a % 2) * 128 + 128], rhs=fT[:, kc, th * 512:(th + 1) * 512],
                        start=(kc == 0), stop=(kc == 15)), reads=[But[ui]] + BfT, writes=[Bps[bank]])
            gi2 = a % 2
            for th in range(2):
                bank = (a % 2) * 2 + th
                P.op("act", lambda e, bank=bank, gi2=gi2, th=th: e.activation(out=ga[gi2][:, th * 512:(th + 1) * 512], in_=psum[:, bank, :], func=AF.Gelu),
                     writes=[Bps[bank], Bga[gi2]])
            P.op("dve", lambda e, gi=gi, gi2=gi2, c=c, si=si: e.tensor_tensor(out=AT[si][:, c, :], in0=ga[gi2][:], in1=gt[gi][:], op=ALU.mult),
                 reads=[Bga[gi2], Bgt[gi]], writes=[BAT[si][c]])

    acnt = [0]

    def p2(s):
        si = s % 2
        for th in range(2):
            for dp in range(8):
                pair = acnt[0] % 2
                acnt[0] += 1
                for c in range(4):
                    for u in range(2):
                        dc = dp * 2 + u
                        bank = 4 + pair * 2 + u
                        P.op("pe", lambda e, c=c, dc=dc, bank=bank, th=th: e.matmul(
                            psum[:, bank, :], lhsT=vt[si][:, c, dc * 128:(dc + 1) * 128], rhs=AT[si][:, c, th * 512:(th + 1) * 512],
                            start=(c == 0), stop=(c == 3)), reads=[Bvt[si], BAT[si][c]], writes=[Bps[bank]])
                for u in range(2):
                    dc = dp * 2 + u
                    bank = 4 + pair * 2 + u
                    P.op("dve", lambda e, dc=dc, bank=bank, th=th: e.scalar_tensor_tensor(
                        out=x1T[:, dc, th * 512:(th + 1) * 512], in0=psum[:, bank, :], scalar=g2[:, dc:dc + 1],
                        in1=x1T[:, dc, th * 512:(th + 1) * 512], op0=ALU.mult, op1=ALU.add), reads=[Bc], writes=[Bps[bank], Bx1[dc][th]])

    nsuper = NS if stop != "G1" else 2
    p1(0)
    for s in range(nsuper):
        if s + 1 < nsuper:
            p1(s + 1)
        p2(s)
    if "x2" in dbg:
        dump("x2", x1T[:], allx1, [128, 16, NQ])
    P.barrier()

    off[0] = R2
    sq = T("sq3", [128, 16, 512], BF16)
    rstd = T("rstd3", [128, 1024], F32)
    ob = [T("ob%d" % i, [128, 16, 512], F32) for i in range(1)]
    Bob = Buf()
    outTv = outT.rearrange("(kc p) t -> p kc t", p=128)
    for th in range(2):
        rms_stats(x1T[:, :, th * 512:(th + 1) * 512], allx1, 512, th, rstd[:, th * 512:(th + 1) * 512])
        for kc in range(16):
            P.op("dve", lambda e, kc=kc, th=th: e.scalar_tensor_tensor(
                out=ob[0][:, kc, :], in0=x1T[:, kc, th * 512:(th + 1) * 512], scalar=fg[:, kc:kc + 1], in1=rstd[:, th * 512:(th + 1) * 512],
                op0=ALU.mult, op1=ALU.mult), reads=[Brstd, Bc] + allx1, writes=[Bob])
        P.dma("sp", lambda e, th=th: e.dma_start(out=outTv[:, :, th * 512:(th + 1) * 512], in_=ob[0][:]), reads=[Bob])
    return end()


def _rope_tables(pos):
    inv = (10000.0 ** (-np.arange(16, dtype=np.float32) / 16)).astype(np.float32)
    row = (pos // 64).astype(np.float32)
    col = (pos % 64).astype(np.float32)
    C = np.zeros((128, 2048), np.float32)
    S = np.zeros((128, 2048), np.float32)
    for p in range(128):
        j = p % 64
        axis, r = j // 32, j % 32
        hf, i = r // 16, r % 16
        ang = (row if axis == 0 else col) * inv[i]
        C[p] = np.cos(ang)
        S[p] = np.sin(ang) * (-1.0 if hf == 0 else 1.0)
    return C, S


def _emat():
    e = np.zeros((64, 2, 2, 32, 128), np.float32)
    bb = np.arange(128, dtype=np.float32)
    for tp in range(64):
        q, t = tp // 32, tp % 32
        e[tp, 0, q, t, :] = 1.0
        e[tp, 1, q, t, :] = -bb
    return np.ascontiguousarray(e.reshape(128, 2, 4096))


def _core_inputs(k, I, shared):
    b, half = k // 2, k % 2
    own = slice(half * 1024, half * 1024 + 1024)
    oth = slice((1 - half) * 1024, (1 - half) * 1024 + 1024)
    x = I["x"][b]
    xT = np.ascontiguousarray(np.concatenate([x[own], x[oth], I["ctx"][b]], 0).T)
    cv = np.stack([I["c"][b].reshape(16, 128).T, I["c_ctx"].reshape(16, 128).T], -1).reshape(128, 32)
    pos = np.concatenate([np.arange(2048)[own], np.arange(2048)[oth]])
    C, S = _rope_tables(pos)
    t = np.arange(2048)
    ic = np.zeros((4, 1024), np.float32)
    for g, w in enumerate((2, 4, 8, 16)):
        lo = np.clip(t - w // 2, 0, 2048)
        hi = np.clip(t + w // 2, 0, 2048)
        ic[g] = (1.0 / (hi - lo).astype(np.float32))[own]
    halo = np.array([1.0 if half == 1 else 0.0, 1.0 if half == 0 else 0.0], np.float32)
    smallv = np.concatenate([shared["smallv"], np.broadcast_to(halo, (128, 2))], 1)
    d = dict(shared["common"])
    d.update(xT=xT, cvec=np.ascontiguousarray(cv, np.float32), smallv=np.ascontiguousarray(smallv, np.float32),
             ropeC=C, ropeS=S, icnt=np.ascontiguousarray(np.broadcast_to(ic.reshape(1, 4096), (128, 4096))))
    return d


def _shared(I):
    f = lambda a: np.ascontiguousarray(a, dtype=np.float32)
    pc = lambda v, n: v.reshape(n, 128).T
    smallv = np.concatenate([pc(I["ada_b"][0], 96), pc(I["norm1_g"][0], 16), pc(I["norm2_g"][0], 16), pc(I["final_g"], 16),
                             I["pool_b"][0].T, pc(I["pool_scale"][0], 4)], 1)
    pm = np.zeros((128, 128), np.float32)
    for m in range(128):
        r = (m % 64) % 32
        pm[m + 16 if r < 16 else m - 16, m] = 1.0
    cmat = np.concatenate([np.eye(128, dtype=np.float32), pm], 1)
    common = dict(
        ada_w=f(I["ada_w"][0]), w_in=f(I["w_in"][0]), pool_w=f(I["pool_w"][0]),
        dlam=f(np.broadcast_to(I["diff_lambda"][0].reshape(1, 256), (128, 256))),
        subg=f(np.broadcast_to(I["subln_g"][0].reshape(1, 128), (128, 128))),
        w_out=f(I["w_out"][0]), wq=f(I["peer_wq"][0]),
        keysT=f(I["peer_keys"][0].reshape(16, 128, 128).transpose(0, 2, 1)),
        uT=f(I["peer_u"][0].T), pv=f(I["peer_v"][0]), cmat=f(cmat), emat=_emat())
    return dict(smallv=f(smallv), common=common)


_NC = None


def kernel(**inputs):
    global _NC
    I = {k: np.asarray(v) for k, v in inputs.items()}
    if _NC is None:
        _NC = build()[0]
    shared = _shared(I)
    in_maps = [_core_inputs(k, I, shared) for k in range(8)]
    res = run_bass_kernel_spmd(_NC, in_maps, core_ids=list(range(8)))
    out = np.empty((4, 2048, 2048), np.float32)
    for k in range(8):
        b, half = k // 2, k % 2
        out[b, half * 1024:(half + 1) * 1024, :] = res.results[k]["outT"].T
    return out
```

```python
import math
import numpy as np
import concourse.bass as bass
import concourse.mybir as mybir
from concourse.bass_utils import run_bass_kernel_spmd
from contextlib import ExitStack

F32 = mybir.dt.float32
BF16 = mybir.dt.bfloat16
I32 = mybir.dt.int32
U32 = mybir.dt.uint32
AF = mybir.ActivationFunctionType
ALU = mybir.AluOpType
AX = mybir.AxisListType

D = 2048
NT = 2304
NQ = 1024
NE = 16384
EPS = 1e-6
LAM_INIT = 0.8 - 0.6 * math.exp(0.0)


class Buf:
    __slots__ = ("name", "w", "r")

    def __init__(self, name=""):
        self.name = name
        self.w = None
        self.r = []


class Prog:
    CE = ("act", "dve", "pool", "pe")
    NDS = 12

    def __init__(self, nc, stack):
        self.nc = nc
        self.eng = {"sp": nc.sync, "act": nc.scalar, "dve": nc.vector, "pool": nc.gpsimd, "pe": nc.tensor}
        self.cnt = {e: 0 for e in self.CE}
        self.sem = {("c", e): stack.enter_context(nc.semaphore("c_" + e)) for e in self.CE}
        self.dcnt = {}
        self.dnext = {}
        for q in ("sp", "act", "pool"):
            for i in range(self.NDS):
                self.sem[("d", q, i)] = stack.enter_context(nc.semaphore("d_%s%d" % (q, i)))
                self.dcnt[(q, i)] = 0
            self.dnext[q] = 0
        self.known = {e: {} for e in self.eng}
        self.nops = 0

    def _deps(self, eng, reads, writes):
        deps = {}

        def add(ev):
            if ev is None:
                return
            k, v = ev
            if eng == "pe" and k == ("c", "pe"):
                return
            if deps.get(k, 0) < v:
                deps[k] = v
        for b in reads:
            add(b.w)
        for b in writes:
            add(b.w)
            for ev in b.r:
                add(ev)
        out = []
        kn = self.known[eng]
        for k, v in deps.items():
            if kn.get(k, 0) < v:
                kn[k] = v
                out.append((k, v))
        return out

    def _commit(self, ev, reads, writes):
        for b in reads:
            b.r.append(ev)
        for b in writes:
            b.w = ev
            b.r = []

    def _issue(self, eng, waits, fn, inc):
        e = self.eng[eng]
        for k, v in waits:
            e.wait_ge(self.sem[k], v)
        if fn is not None:
            fn(e).then_inc(self.sem[inc[0]], inc[1])
        self.nops += 1

    def op(self, eng, fn, reads=(), writes=()):
        waits = self._deps(eng, reads, writes)
        self.cnt[eng] += 1
        ev = (("c", eng), self.cnt[eng])
        self._commit(ev, reads, writes)
        self._issue(eng, waits, fn, (("c", eng), 1))
        return ev

    def dma(self, q, fn, reads=(), writes=()):
        waits = self._deps(q, reads, writes)
        i = self.dnext[q]
        self.dnext[q] = (i + 1) % self.NDS
        k = ("d", q, i)
        prev = self.dcnt[(q, i)]
        if prev > 0 and self.known[q].get(k, 0) < prev:
            self.known[q][k] = prev
            waits.append((k, prev))
        self.dcnt[(q, i)] = prev + 16
        ev = (k, prev + 16)
        self._commit(ev, reads, writes)
        self._issue(q, waits, fn, (k, 16))
        return ev

    def _all(self):
        waits = [(("d", q, i), v) for (q, i), v in self.dcnt.items() if v > 0]
        waits += [(("c", e), self.cnt[e]) for e in self.CE if self.cnt[e] > 0]
        return waits

    def barrier(self):
        allw = self._all()
        for eng in self.eng:
            kn = self.known[eng]
            w = []
            for k, v in allw:
                if kn.get(k, 0) < v:
                    kn[k] = v
                    w.append((k, v))
            self._issue(eng, w, None, None)

    def finish(self):
        self.barrier()


def build(stop=None, dbg=()):
    nc = bass.Bass("TRN2", target_bir_lowering=False)
    din = lambda n, s, dt=F32: nc.dram_tensor(n, list(s), dt, kind="ExternalInput").ap()
    xT = din("xT", [D, NT])
    cvec = din("cvec", [128, 32])
    ada_w = din("ada_w", [D, 6 * D])
    smallv = din("smallv", [128, 96 + 48 + 8 + 2])
    w_in = din("w_in", [D, 5120])
    pool_w = din("pool_w", [4, 128, 128])
    dlam = din("dlam", [128, 256])
    subg = din("subg", [128, 128])
    w_out = din("w_out", [D, D])
    wq = din("wq", [D, D])
    keysT = din("keysT", [16, 128, 128])
    uT = din("uT", [D, NE])
    pv = din("pv", [NE, D])
    ropeC = din("ropeC", [128, 2048])
    ropeS = din("ropeS", [128, 2048])
    cmat = din("cmat", [128, 256])
    icnt = din("icnt", [128, 4096])
    emat = din("emat", [128, 2, 4096])
    outT = nc.dram_tensor("outT", [D, NQ], F32, kind="ExternalOutput").ap()
    kT_s = nc.dram_tensor("kT_s", [12, 128, NT], BF16, kind="Internal").ap()
    qT_s = nc.dram_tensor("qT_s", [12, 128, NQ], BF16, kind="Internal").ap()
    V_s = nc.dram_tensor("V_s", [NT, 1536], BF16, kind="Internal").ap()
    Gd = nc.dram_tensor("Gd", [128, 128, NQ], BF16, kind="Internal").ap()
    x1_s = nc.dram_tensor("x1_s", [128, 16, NQ], F32, kind="Internal").ap()
    dbg_out = {}

    st = ExitStack()
    P = Prog(nc, st)
    off = [16384]

    def T(name, shape, dt, at=None):
        n = 1
        for s in shape[1:]:
            n *= s
        nb = n * (2 if dt == BF16 else 4)
        nb = (nb + 63) // 64 * 64
        if at is None:
            at = off[0]
            off[0] = at + nb
        assert at + nb <= 229000, (name, at, nb)
        return nc.alloc_sbuf_tensor_at(name, list(shape), dt, offset=at)

    def dump(name, ap, buf, shape, dt=F32):
        d = nc.dram_tensor("dbg_" + name, list(shape), dt, kind="ExternalOutput").ap()
        dbg_out[name] = d
        P.dma("sp", lambda e: e.dma_start(out=d, in_=ap), reads=buf)

    psum = nc.alloc_psum_tensor("psum", [128, 8, 512], F32)
    Bps = [Buf("ps%d" % i) for i in range(8)]

    def end():
        P.finish()
        st.close()
        return nc, dbg_out

    smv = T("smv", [128, 154], F32)
    cm = T("cm", [128, 256], F32)
    cvs = T("cvs", [128, 32], F32)
    dl = T("dl", [128, 256], F32)
    sgb = T("sgb", [128, 128], F32)
    identb = T("identb", [128, 128], BF16)
    pmb = T("pmb", [128, 128], BF16)
    onesb = T("onesb", [128, 128], BF16)
    modT = T("modT", [128, 96, 2], F32)
    gs1 = T("gs1", [128, 16, 2], F32)
    gs2 = T("gs2", [128, 16], F32)
    lamt = T("lamt", [128, 8], F32)
    iota_i = T("iota_i", [128, 128], I32)
    iota_f = T("iota_f", [128, 128], F32)
    Bc = Buf("consts")
    for t, d in ((smv, smallv), (cm, cmat), (cvs, cvec), (dl, dlam), (sgb, subg)):
        P.dma("sp", lambda e, t=t, d=d: e.dma_start(out=t[:], in_=d), writes=[Bc])
    identf = cm[:, 0:128]
    P.op("dve", lambda e: e.tensor_copy(out=identb[:], in_=cm[:, 0:128]), reads=[Bc], writes=[Bc])
    P.op("dve", lambda e: e.tensor_copy(out=pmb[:], in_=cm[:, 128:256]), reads=[Bc], writes=[Bc])
    P.op("dve", lambda e: e.memset(onesb[:], 1.0), writes=[Bc])
    epsb = T("epsb", [128, 1], F32)
    P.op("dve", lambda e: e.memset(epsb[:], EPS), writes=[Bc])
    P.op("pool", lambda e: e.iota(iota_i[:], pattern=[[1, 128]], base=0, channel_multiplier=0), writes=[Bc])
    P.op("dve", lambda e: e.tensor_copy(out=iota_f[:], in_=iota_i[:]), reads=[Bc], writes=[Bc])
    P.op("dve", lambda e: e.tensor_scalar_mul(out=sgb[:], in0=sgb[:], scalar1=1.0 - LAM_INIT), writes=[Bc])
    dlv = dl[:].rearrange("p (a b c) -> p a b c", a=2, b=2)
    prod = T("prod", [128, 2, 64], F32)
    P.op("dve", lambda e: e.tensor_tensor(out=prod[:], in0=dlv[:, :, 0, :], in1=dlv[:, :, 1, :], op=ALU.mult), writes=[Bc])
    P.op("dve", lambda e: e.tensor_reduce(out=lamt[:, 0:2], in_=prod[:], axis=AX.X, op=ALU.add), writes=[Bc])
    P.op("act", lambda e: e.activation(out=lamt[:, 2:4], in_=lamt[:, 0:2], func=AF.Exp), writes=[Bc])
    P.op("dve", lambda e: e.tensor_tensor(out=lamt[:, 4:5], in0=lamt[:, 3:4], in1=lamt[:, 2:3], op=ALU.subtract), writes=[Bc])
    P.op("dve", lambda e: e.tensor_scalar_add(out=lamt[:, 4:5], in0=lamt[:, 4:5], scalar1=-LAM_INIT), writes=[Bc])
    nlam = lamt[:, 4:5]
    adab = smv[:, 0:96]
    n1g = smv[:, 96:112]
    n2g = smv[:, 112:128]
    fg = smv[:, 128:144]
    poolb = smv[:, 144:148]
    pools = smv[:, 148:152]
    halo = smv[:, 152:154]
    P.op("act", lambda e: e.activation(out=cvs[:], in_=cvs[:], func=AF.Silu), writes=[Bc])
    base0 = off[0]
    R0 = base0
    R1 = R0 + 72 * 1024
    R2 = R1 + 32 * 1024

    off[0] = R1
    aw = [T("aw%d" % i, [128, 16, 512], F32) for i in range(2)]
    Baw = [Buf(), Buf()]
    modrow = T("modrow", [2, 6 * D], F32)
    Bmr = Buf()
    awv = ada_w.rearrange("(kc p) n -> p kc n", p=128)
    csv = cvs[:].rearrange("p (k r) -> p k r", r=2)
    for nt in range(24):
        b = nt % 2
        bank = nt % 4
        P.dma("sp", lambda e, nt=nt, b=b: e.dma_start(out=aw[b][:], in_=awv[:, :, nt * 512:(nt + 1) * 512]), writes=[Baw[b]])
        for kc in range(16):
            P.op("pe", lambda e, kc=kc, b=b, bank=bank: e.matmul(
                psum[0:2, bank, :], lhsT=csv[:, kc, :], rhs=aw[b][:, kc, :],
                start=(kc == 0), stop=(kc == 15)), reads=[Baw[b], Bc], writes=[Bps[bank]])
        P.op("act", lambda e, nt=nt, bank=bank: e.activation(out=modrow[:, nt * 512:(nt + 1) * 512], in_=psum[0:2, bank, :], func=AF.Copy),
             writes=[Bps[bank], Bmr])
    for j in range(96):
        P.op("pe", lambda e, j=j: e.transpose(out=psum[:, 4, 2 * j:2 * j + 2], in_=modrow[:, j * 128:(j + 1) * 128], identity=cm[0:2, 0:2]),
             reads=[Bmr, Bc], writes=[Bps[4]])
    P.op("dve", lambda e: e.tensor_tensor(
        out=modT[:], in0=psum[:, 4, 0:192].rearrange("p (j r) -> p j r", r=2),
        in1=adab.unsqueeze(2).to_broadcast([128, 96, 2]), op=ALU.add), reads=[Bc], writes=[Bps[4], Bc])
    P.op("dve", lambda e: e.tensor_scalar_add(out=gs1[:], in0=modT[:, 16:32, :], scalar1=1.0), writes=[Bc])
    P.op("dve", lambda e: e.tensor_tensor(out=gs1[:], in0=gs1[:], in1=n1g.unsqueeze(2).to_broadcast([128, 16, 2]), op=ALU.mult), writes=[Bc])
    P.op("dve", lambda e: e.tensor_scalar_add(out=gs2[:], in0=modT[:, 64:80, 0], scalar1=1.0), writes=[Bc])
    P.op("dve", lambda e: e.tensor_tensor(out=gs2[:], in0=gs2[:], in1=n2g, op=ALU.mult), writes=[Bc])
    sh1 = modT[:, 0:16, :]
    g1 = modT[:, 32:48, 0]
    sh2 = modT[:, 48:64, 0]
    g2 = modT[:, 80:96, 0]
    if "mod" in dbg:
        dump("mod", modT[:], [Bc], [128, 96, 2])
        dump("lam", lamt[:], [Bc], [128, 8])
    P.barrier()
    if stop == "A":
        return end()

    off[0] = R0
    hT = T("hT", [128, 16, NT], BF16)
    BhT = [Buf() for _ in range(5)]
    off[0] = R1
    xt = [T("xt%d" % i, [128, 16, 512], F32) for i in range(2)]
    Bxt = [[Buf() for _ in range(16)] for _ in range(2)]
    sq = T("sq", [128, 16, 512], BF16)
    Bsq = Buf()
    rstd = T("rstd", [128, 1024], F32)
    Brstd = Buf()
    xTv = xT.rearrange("(kc p) t -> p kc t", p=128)

    def rms_stats(src, Bsrc, n, ps_bank, rs_ap, sq_on_dve=False):
        if sq_on_dve:
            P.op("dve", lambda e: e.tensor_tensor(out=sq[:, :, 0:n], in0=src, in1=src, op=ALU.mult), reads=Bsrc, writes=[Bsq])
        else:
            P.op("act", lambda e: e.activation(out=sq[:, :, 0:n], in_=src, func=AF.Square), reads=Bsrc, writes=[Bsq])
        for kc in range(16):
            P.op("pe", lambda e, kc=kc: e.matmul(psum[:, ps_bank, 0:n], lhsT=onesb[:], rhs=sq[:, kc, 0:n],
                                                 start=(kc == 0), stop=(kc == 15)), reads=[Bsq, Bc], writes=[Bps[ps_bank]])
        P.op("act", lambda e: e.activation(out=rs_ap, in_=psum[:, ps_bank, 0:n], func=AF.Sqrt, bias=epsb[:], scale=1.0 / D),
             reads=[Bc], writes=[Bps[ps_bank], Brstd])
        P.op("dve", lambda e: e.reciprocal(out=rs_ap, in_=rs_ap), writes=[Brstd])

    for g in range(5):
        b = g % 2
        n = 512 if g < 4 else 256
        t0 = g * 512
        r = 0 if g < 4 else 1
        P.dma("sp", lambda e, b=b, t0=t0, n=n: e.dma_start(out=xt[b][:, :, 0:n], in_=xTv[:, :, t0:t0 + n]), writes=Bxt[b])
        rms_stats(xt[b][:, :, 0:n], Bxt[b], n, g % 2, rstd[:, 0:n], sq_on_dve=True)
        for kc in range(16):
            P.op("dve", lambda e, b=b, kc=kc, n=n, r=r: e.scalar_tensor_tensor(
                out=xt[b][:, kc, 0:n], in0=xt[b][:, kc, 0:n], scalar=gs1[:, kc, r:r + 1], in1=rstd[:, 0:n],
                op0=ALU.mult, op1=ALU.mult), reads=[Brstd, Bc], writes=[Bxt[b][kc]])
            P.op("act", lambda e, b=b, kc=kc, n=n, r=r, t0=t0: e.activation(
                out=hT[:, kc, t0:t0 + n], in_=xt[b][:, kc, 0:n], func=AF.Identity, bias=sh1[:, kc, r:r + 1], scale=1.0),
                reads=[Bxt[b][kc], Bc], writes=[BhT[g]])
    if "h" in dbg:
        dump("h", hT[:, :, 0:512], BhT, [128, 16, 512], BF16)
    P.barrier()
    if stop == "B":
        return end()

    off[0] = R1
    mixT = T("mixT", [128, 16, NQ], BF16)
    BmixT = [Buf() for _ in range(16)]
    off[0] = R2
    wt = [T("wt%d" % i, [128, 16, 512], BF16) for i in range(2)]
    Bwt = [Buf(), Buf()]
    rC = T("rC", [128, 2048], F32)
    rS = T("rS", [128, 2048], F32)
    Brope = Buf()
    P.dma("sp", lambda e: e.dma_start(out=rC[:], in_=ropeC), writes=[Brope])
    P.dma("sp", lambda e: e.dma_start(out=rS[:], in_=ropeS), writes=[Brope])
    wv = w_in.rearrange("(kc p) n -> p kc n", p=128)
    wcnt = [0]

    worder = [3584, 4096, 4608] + [c for hg in range(3) for c in (512 + hg * 512, 2048 + hg * 512)] + [0]

    def issue_w(i):
        if i < len(worder):
            c0 = worder[i]
            P.dma("pool", lambda e: e.dma_start(out=wt[i % 2][:], in_=wv[:, :, c0:c0 + 512]), writes=[Bwt[i % 2]])

    issue_w(0)

    def load_w(c0):
        i = wcnt[0]
        assert worder[i] == c0
        wcnt[0] += 1
        issue_w(i + 1)
        return i % 2

    NSTG = 8
    stg = [T("stg%d" % i, [128, 512], BF16) for i in range(NSTG)]
    Bstg = [Buf() for _ in range(NSTG)]
    scnt = [0]
    pcnt = [0]
    V_sv = V_s.rearrange("(tt p) c -> p tt c", p=128)
    for vc in range(3):
        b = load_w(3584 + vc * 512)
        for tt in range(18):
            bank = pcnt[0] % 4
            pcnt[0] += 1
            for kc in range(16):
                P.op("pe", lambda e, kc=kc, tt=tt, bank=bank, b=b: e.matmul(
                    psum[:, bank, :], lhsT=hT[:, kc, tt * 128:(tt + 1) * 128], rhs=wt[b][:, kc, :],
                    start=(kc == 0), stop=(kc == 15)), reads=[Bwt[b]] + BhT, writes=[Bps[bank]])
            s = scnt[0] % NSTG
            scnt[0] += 1
            P.op("act", lambda e, s=s, bank=bank: e.activation(out=stg[s][:], in_=psum[:, bank, :], func=AF.Copy),
                 writes=[Bps[bank], Bstg[s]])
            P.dma("sp", lambda e, s=s, tt=tt, vc=vc: e.dma_start(out=V_sv[:, tt, vc * 512:(vc + 1) * 512], in_=stg[s][:]),
                  reads=[Bstg[s]])
    t1 = [T("t1_%d" % i, [128, 512], F32) for i in range(2)]
    t2 = [T("t2_%d" % i, [128, 512], F32) for i in range(2)]
    Bt1 = [Buf(), Buf()]
    Bt2 = [Buf(), Buf()]
    rcnt = [0]

    def c2_tail(s, bank, t0, n, dst):
        if t0 >= 2048:
            P.dma("sp", lambda e: e.dma_start(out=dst, in_=stg[s][:, 0:n]), reads=[Bstg[s]])
            return
        rb = 4 + (rcnt[0] % 2)
        ri = rcnt[0] % 2
        rcnt[0] += 1
        P.op("pe", lambda e: e.matmul(psum[:, rb, :], lhsT=pmb[:], rhs=stg[s][:], start=True, stop=True),
             reads=[Bstg[s], Bc], writes=[Bps[rb]])
        P.op("dve", lambda e: e.tensor_tensor(out=t1[ri][:], in0=psum[:, bank, :], in1=rC[:, t0:t0 + 512], op=ALU.mult),
             reads=[Brope], writes=[Bps[bank], Bt1[ri]])
        P.op("dve", lambda e: e.tensor_tensor(out=t2[ri][:], in0=psum[:, rb, :], in1=rS[:, t0:t0 + 512], op=ALU.mult),
             reads=[Brope], writes=[Bps[rb], Bt2[ri]])
        s2 = scnt[0] % NSTG
        scnt[0] += 1
        P.op("pool", lambda e: e.tensor_tensor(out=stg[s2][:], in0=t1[ri][:], in1=t2[ri][:], op=ALU.add),
             reads=[Bt1[ri], Bt2[ri]], writes=[Bstg[s2]])
        P.dma("sp", lambda e: e.dma_start(out=dst, in_=stg[s2][:]), reads=[Bstg[s2]])

    pending = None
    for hg in range(3):
        for which in range(2):
            b = load_w((512 if which == 0 else 2048) + hg * 512)
            for hh in range(4):
                h = hg * 4 + hh
                groups = [(0, 512), (512, 512)] if which == 0 else [(0, 512), (512, 512), (1024, 512), (1536, 512), (2048, 256)]
                for (t0, n) in groups:
                    bank = pcnt[0] % 4
                    pcnt[0] += 1
                    for kc in range(16):
                        P.op("pe", lambda e, kc=kc, bank=bank, b=b, hh=hh, t0=t0, n=n: e.matmul(
                            psum[:, bank, 0:n], lhsT=wt[b][:, kc, hh * 128:(hh + 1) * 128], rhs=hT[:, kc, t0:t0 + n],
                            start=(kc == 0), stop=(kc == 15)), reads=[Bwt[b]] + BhT, writes=[Bps[bank]])
                    s = scnt[0] % NSTG
                    scnt[0] += 1
                    dst = (qT_s if which == 0 else kT_s)[h, :, t0:t0 + n]
                    P.op("act", lambda e, s=s, bank=bank, n=n: e.activation(out=stg[s][:, 0:n], in_=psum[:, bank, 0:n], func=AF.Copy),
                         writes=[Bps[bank], Bstg[s]])
                    if pending is not None:
                        c2_tail(*pending)
                    pending = (s, bank, t0, n, dst)
    c2_tail(*pending)
    b = load_w(0)
    pwb = T("pwb", [128, 4, 128], BF16)
    Bpw = Buf()
    P.dma("pool", lambda e: e.dma_start(out=pwb[:], in_=pool_w.rearrange("g c d -> c g d")), writes=[Bpw])
    ic = T("ic", [128, 4096], F32)
    P.dma("sp", lambda e: e.dma_start(out=ic[:], in_=icnt), writes=[Bpw])
    zT = T("zT", [128, 1040], F32)
    sA = T("sA", [128, 1040], F32)
    sB = T("sB", [128, 1040], F32)
    ymix = T("ymix", [128, 1024], BF16)
    Bz = Buf()
    for g in range(4):
        for th in range(2):
            bank = pcnt[0] % 4
            pcnt[0] += 1
            for kc in range(16):
                P.op("pe", lambda e, kc=kc, bank=bank, th=th, g=g: e.matmul(
                    psum[:, bank, :], lhsT=wt[b][:, kc, g * 128:(g + 1) * 128], rhs=hT[:, kc, th * 512:(th + 1) * 512],
                    start=(kc == 0), stop=(kc == 15)), reads=[Bwt[b]] + BhT, writes=[Bps[bank]])
            P.op("act", lambda e, bank=bank, th=th: e.activation(out=zT[:, 8 + th * 512:8 + (th + 1) * 512], in_=psum[:, bank, :], func=AF.Copy),
                 writes=[Bps[bank], Bz])
        bank = pcnt[0] % 4
        pcnt[0] += 1
        for hi, tsrc in enumerate((2040, 1024)):
            for kc in range(16):
                P.op("pe", lambda e, kc=kc, bank=bank, hi=hi, tsrc=tsrc, g=g: e.matmul(
                    psum[:, bank, hi * 8:hi * 8 + 8], lhsT=wt[b][:, kc, g * 128:(g + 1) * 128], rhs=hT[:, kc, tsrc:tsrc + 8],
                    start=(kc == 0), stop=(kc == 15)), reads=[Bwt[b]] + BhT, writes=[Bps[bank]])
        P.op("dve", lambda e, bank=bank: e.tensor_scalar_mul(out=zT[:, 0:8], in0=psum[:, bank, 0:8], scalar1=halo[:, 0:1]), reads=[Bc], writes=[Bps[bank], Bz])
        P.op("dve", lambda e, bank=bank: e.tensor_scalar_mul(out=zT[:, 1032:1040], in0=psum[:, bank, 8:16], scalar1=halo[:, 1:2]), reads=[Bc], writes=[Bps[bank], Bz])
        w = 2 << g
        src = zT
        ln = 1040
        step = 1
        bufs = [sA, sB]
        bi = 0
        while step < w:
            dst_ = bufs[bi]
            bi ^= 1
            ln2 = ln - step
            P.op("dve", lambda e, src=src, dst_=dst_, ln2=ln2, step=step: e.tensor_tensor(
                out=dst_[:, 0:ln2], in0=src[:, 0:ln2], in1=src[:, step:step + ln2], op=ALU.add), writes=[Bz])
            src = dst_
            ln = ln2
            step *= 2
        o0 = 8 - w // 2
        dst_ = bufs[bi]
        P.op("dve", lambda e, src=src, dst_=dst_, o0=o0, g=g: e.tensor_tensor(
            out=dst_[:, 0:1024], in0=src[:, o0:o0 + 1024], in1=ic[:, g * 1024:(g + 1) * 1024], op=ALU.mult), reads=[Bpw], writes=[Bz])
        P.op("dve", lambda e, dst_=dst_: e.tensor_tensor(out=ymix[:], in0=dst_[:, 0:1024], in1=zT[:, 8:1032], op=ALU.subtract), writes=[Bz])
        for th in range(2):
            bank = pcnt[0] % 4
            pcnt[0] += 1
            P.op("pe", lambda e, bank=bank, th=th, g=g: e.matmul(psum[:, bank, :], lhsT=pwb[:, g, :], rhs=ymix[:, th * 512:(th + 1) * 512],
                                                             start=True, stop=True), reads=[Bpw, Bz], writes=[Bps[bank]])
            P.op("dve", lambda e, bank=bank, th=th, g=g: e.tensor_scalar(
                out=mixT[:, g, th * 512:(th + 1) * 512], in0=psum[:, bank, :], scalar1=poolb[:, g:g + 1], scalar2=pools[:, g:g + 1],
                op0=ALU.add, op1=ALU.mult), reads=[Bc], writes=[Bps[bank], BmixT[g]])
    if "pool" in dbg:
        dump("pool", mixT[:, 0:4, :], BmixT, [128, 4, NQ], BF16)
    P.barrier()
    if stop == "C":
        if "qkv" in dbg:
            off[0] = R2
            for nm, src_ in (("qT0", qT_s[0]), ("kT0", kT_s[0])):
                tmpb = T(nm, list(src_.shape), BF16)
                bb = Buf()
                P.dma("sp", lambda e, tmpb=tmpb, src_=src_: e.dma_start(out=tmpb[:], in_=src_), writes=[bb])
                dump(nm, tmpb[:], [bb], list(src_.shape), BF16)
            tmpv = T("v0", [128, 18, 128], BF16)
            bb = Buf()
            P.dma("sp", lambda e: e.dma_start(out=tmpv[:], in_=V_sv[:, :, 0:128]), writes=[bb])
            dump("v0", tmpv[:], [bb], [128, 18, 128], BF16)
        return end()

    off[0] = R2
    kTh = [T("kTh%d" % i, [128, NT], BF16) for i in range(2)]
    qTh = [T("qTh%d" % i, [128, NQ], BF16) for i in range(2)]
    Vaug = [T("Vaug%d" % i, [128, 18, 129], BF16) for i in range(2)]
    Bkq = [Buf(), Buf()]
    for i in range(2):
        P.op("dve", lambda e, i=i: e.memset(Vaug[i][:, :, 128:129], 1.0), writes=[Bkq[i]])
    NEB = 3
    Eb = [T("Eb%d" % i, [128, 1024], BF16) for i in range(NEB)]
    BEb = [Buf() for _ in range(NEB)]
    posb = [T("posb%d" % i, [128, 8, 129], F32) for i in range(2)]
    Bpo = [Buf(), Buf()]
    rz = T("rz", [128, 4, 2], F32)
    nl = T("nl", [128, 4], F32)
    oo = T("oo", [128, 4, 128], F32)
    o2 = T("o2", [128, 4, 128], F32)
    ss = T("ss", [128, 8], F32)
    mhalf = T("mhalf", [128, 4], F32)
    ysb = [T("ysb%d" % i, [128, 4, 128], BF16) for i in range(2)]
    Bys = [Buf(), Buf()]
    Bpp = Buf()
    P.op("dve", lambda e: e.memset(mhalf[:], -0.5), writes=[Bpp])
    psb = psum[:, 7, 0:256].bitcast(BF16)

    def load_head(h):
        i = h % 2
        P.dma("sp", lambda e: e.dma_start(out=kTh[i][:], in_=kT_s[h]), writes=[Bkq[i]])
        P.dma("sp", lambda e: e.dma_start(out=qTh[i][:], in_=qT_s[h]), writes=[Bkq[i]])
        P.dma("sp", lambda e: e.dma_start(out=Vaug[i][:, :, 0:128], in_=V_sv[:, :, h * 128:(h + 1) * 128]), writes=[Bkq[i]])

    its = [(h, qh, kt) for h in range(12) for qh in range(2) for kt in range(18)]

    def qk(idx):
        h, qh, kt = its[idx]
        i = h % 2
        pi = idx % 2
        for m in range(2):
            bank = 2 * pi + m
            P.op("pe", lambda e, m=m, bank=bank: e.matmul(psum[:, bank, :], lhsT=kTh[i][64 * m:64 * m + 64, kt * 128:(kt + 1) * 128],
                                                          rhs=qTh[i][64 * m:64 * m + 64, qh * 512:(qh + 1) * 512], start=True, stop=True),
                 reads=[Bkq[i]], writes=[Bps[bank]])
        eb = idx % NEB
        P.op("act", lambda e: e.activation(out=Eb[eb][:].rearrange("p (m q) -> p m q", m=2), in_=psum[:, 2 * pi:2 * pi + 2, :], func=AF.Exp, scale=0.125),
             writes=[Bps[2 * pi], Bps[2 * pi + 1], BEb[eb]])

    def pvmm(idx):
        h, qh, kt = its[idx]
        i = h % 2
        eb = idx % NEB
        for m in range(2):
            for qt in range(4):
                a_ = m * 4 + qt
                bank, c0 = 4 + a_ // 3, (a_ % 3) * 129
                P.op("pe", lambda e, qt=qt, m=m, bank=bank, c0=c0, a_=a_: e.matmul(
                    psum[:, bank, c0:c0 + 129], lhsT=Eb[eb][:, m * 512 + qt * 128:m * 512 + (qt + 1) * 128],
                    rhs=Vaug[i][:, kt, :], start=(kt == 0 and a_ % 3 == 0), stop=(kt == 17), skip_group_check=True),
                    reads=[BEb[eb], Bkq[i]], writes=[Bps[bank]])

    def post_a(h, qh):
        pb_ = (h * 2 + qh) % 2
        po = posb[pb_]
        for j, na in ((0, 3), (1, 3), (2, 2)):
            P.op("dve", lambda e, j=j, na=na: e.tensor_copy(out=po[:, 3 * j:3 * j + na, :], in_=psum[:, 4 + j, 0:na * 129].rearrange("p (a c) -> p a c", c=129)),
                 writes=[Bps[4 + j], Bpo[pb_]])
        B2 = [Bpo[pb_]]
        P.op("dve", lambda e: e.reciprocal(out=rz[:], in_=po[:, :, 128].rearrange("p (m q) -> p q m", m=2)), reads=B2, writes=[Bpp])
        P.op("dve", lambda e: e.tensor_scalar_mul(out=nl[:], in0=rz[:, :, 1], scalar1=nlam), reads=[Bc], writes=[Bpp])
        P.op("dve", lambda e: e.tensor_tensor(out=oo[:], in0=po[:, 0:4, 0:128], in1=rz[:, :, 0:1].to_broadcast([128, 4, 128]), op=ALU.mult), reads=B2, writes=[Bpp])
        P.op("dve", lambda e: e.tensor_tensor(out=o2[:], in0=po[:, 4:8, 0:128], in1=nl[:].unsqueeze(2).to_broadcast([128, 4, 128]), op=ALU.mult), reads=B2, writes=[Bpp])
        P.op("dve", lambda e: e.tensor_tensor(out=oo[:], in0=oo[:], in1=o2[:], op=ALU.add), writes=[Bpp])
        P.op("dve", lambda e: e.tensor_tensor(out=o2[:], in0=oo[:], in1=oo[:], op=ALU.mult), writes=[Bpp])
        P.op("dve", lambda e: e.tensor_reduce(out=ss[:, 0:4], in_=o2[:], axis=AX.X, op=ALU.add), writes=[Bpp])
        P.op("dve", lambda e: e.tensor_scalar(out=ss[:, 0:4], in0=ss[:, 0:4], scalar1=1.0 / 128, scalar2=EPS, op0=ALU.mult, op1=ALU.add), writes=[Bpp])
        P.op("pool", lambda e: e.tensor_tensor(out=ss[:, 4:8], in0=ss[:, 0:4], in1=mhalf[:], op=ALU.pow), writes=[Bpp])
        P.op("dve", lambda e: e.tensor_tensor(out=oo[:], in0=oo[:], in1=ss[:, 4:8].unsqueeze(2).to_broadcast([128, 4, 128]), op=ALU.mult), writes=[Bpp])
        P.op("dve", lambda e: e.tensor_tensor(out=ysb[pb_][:], in0=oo[:], in1=sgb[:].unsqueeze(1).to_broadcast([128, 4, 128]), op=ALU.mult), reads=[Bc, Bpp], writes=[Bys[pb_]])

    def post_b(h, qh):
        pb_ = (h * 2 + qh) % 2
        for qt in range(4):
            P.op("pe", lambda e, qt=qt: e.transpose(out=psb[:, qt * 128:(qt + 1) * 128], in_=ysb[pb_][:, qt, :], identity=identb[:]),
                 reads=[Bys[pb_], Bc], writes=[Bps[7]])
        P.op("dve", lambda e: e.tensor_copy(out=mixT[:, 4 + h, qh * 512:(qh + 1) * 512], in_=psb), reads=[Bys[pb_]], writes=[Bps[7], BmixT[4 + h]])

    load_head(0)
    NI = len(its)
    qk(0)
    qk(1)
    pend = []
    for idx in range(NI):
        h, qh, kt = its[idx]
        if kt == 0 and qh == 0 and h + 1 < 12:
            load_head(h + 1)
        if idx + 2 < NI:
            qk(idx + 2)
        pvmm(idx)
        if kt == 17:
            post_a(h, qh)
            pend.append((idx + 11, h, qh))
        if pend and pend[0][0] <= idx:
            _, h_, qh_ = pend.pop(0)
            post_b(h_, qh_)
    for _, h_, qh_ in pend:
        post_b(h_, qh_)
    if "attn" in dbg:
        dump("attn", mixT[:, 4:16, :], BmixT, [128, 12, NQ], BF16)
    P.barrier()
    if stop == "D":
        return end()

    off[0] = R0
    x1T = T("x1T", [128, 16, NQ], F32)
    Bx1 = [[Buf() for _ in range(2)] for _ in range(16)]
    allx1 = [b_ for r_ in Bx1 for b_ in r_]
    off[0] = R2
    P.dma("sp", lambda e: e.dma_start(out=x1T[:], in_=xTv[:, :, 0:NQ]), writes=allx1)
    wov = w_out.rearrange("(kc p) n -> p kc n", p=128)
    wt2 = [T("wo%d" % i, [128, 16, 512], BF16) for i in range(2)]
    Bwo = [Buf(), Buf()]
    def issue_wo(dg):
        if dg < 4:
            P.dma("pool", lambda e: e.dma_start(out=wt2[dg % 2][:], in_=wov[:, :, dg * 512:(dg + 1) * 512]), writes=[Bwo[dg % 2]])

    issue_wo(0)
    for dg in range(4):
        b = dg % 2
        issue_wo(dg + 1)
        for dci in range(4):
            dc = dg * 4 + dci
            for th in range(2):
                bank = pcnt[0] % 4
                pcnt[0] += 1
                for kc in range(16):
                    P.op("pe", lambda e, kc=kc, bank=bank, b=b, dci=dci, th=th: e.matmul(
                        psum[:, bank, :], lhsT=wt2[b][:, kc, dci * 128:(dci + 1) * 128], rhs=mixT[:, kc, th * 512:(th + 1) * 512],
                        start=(kc == 0), stop=(kc == 15)), reads=[Bwo[b]] + BmixT, writes=[Bps[bank]])
                P.op("dve", lambda e, bank=bank, dc=dc, th=th: e.scalar_tensor_tensor(
                    out=x1T[:, dc, th * 512:(th + 1) * 512], in0=psum[:, bank, :], scalar=g1[:, dc:dc + 1],
                    in1=x1T[:, dc, th * 512:(th + 1) * 512], op0=ALU.mult, op1=ALU.add), reads=[Bc], writes=[Bps[bank], Bx1[dc][th]])
    if "x1" in dbg:
        dump("x1", x1T[:], allx1, [128, 16, NQ])
    P.barrier()
    if stop == "E":
        return end()

    off[0] = R1
    fT = T("fT", [128, 16, NQ], BF16)
    BfT = [Buf(), Buf()]
    off[0] = R2
    sq = T("sq2", [128, 16, 512], BF16)
    rstd = T("rstd2", [128, 1024], F32)
    ftmp = [T("ftmp%d" % i, [128, 512], F32) for i in range(2)]
    Bft = [Buf(), Buf()]
    fc = 0
    for th in range(2):
        rms_stats(x1T[:, :, th * 512:(th + 1) * 512], allx1, 512, th, rstd[:, th * 512:(th + 1) * 512])
        for kc in range(16):
            fi = fc % 2
            fc += 1
            P.op("dve", lambda e, kc=kc, th=th, fi=fi: e.scalar_tensor_tensor(
                out=ftmp[fi][:], in0=x1T[:, kc, th * 512:(th + 1) * 512], scalar=gs2[:, kc:kc + 1], in1=rstd[:, th * 512:(th + 1) * 512],
                op0=ALU.mult, op1=ALU.mult), reads=[Brstd, Bc] + allx1, writes=[Bft[fi]])
            P.op("act", lambda e, kc=kc, th=th, fi=fi: e.activation(
                out=fT[:, kc, th * 512:(th + 1) * 512], in_=ftmp[fi][:], func=AF.Identity, bias=sh2[:, kc:kc + 1], scale=1.0),
                reads=[Bft[fi], Bc], writes=[BfT[th]])
    if "f" in dbg:
        dump("f", fT[:], BfT, [128, 16, NQ], BF16)
    P.dma("sp", lambda e: e.dma_start(out=x1_s, in_=x1T[:]), reads=allx1)
    P.barrier()
    off[0] = R2
    qpT = T("qpT", [128, 16, NQ], BF16)
    BqpT = Buf()
    kTb = T("kTb", [128, 16, 128], BF16)
    BkTb = Buf()
    baseF = off[0]
    wq2 = [T("wqt%d" % i, [128, 16, 512], BF16) for i in range(2)]
    Bwq = [Buf(), Buf()]
    P.dma("pool", lambda e: e.dma_start(out=kTb[:], in_=keysT.rearrange("h c k -> c h k")), writes=[BkTb])
    wqv = wq.rearrange("(kc p) n -> p kc n", p=128)
    def issue_wq(c4):
        if c4 < 4:
            P.dma("pool", lambda e: e.dma_start(out=wq2[c4 % 2][:], in_=wqv[:, :, c4 * 512:(c4 + 1) * 512]), writes=[Bwq[c4 % 2]])

    issue_wq(0)
    for c4 in range(4):
        b = c4 % 2
        issue_wq(c4 + 1)
        for j in range(4):
            hp = c4 * 4 + j
            for th in range(2):
                bank = pcnt[0] % 4
                pcnt[0] += 1
                for kc in range(16):
                    P.op("pe", lambda e, kc=kc, bank=bank, b=b, j=j, th=th: e.matmul(
                        psum[:, bank, :], lhsT=wq2[b][:, kc, j * 128:(j + 1) * 128], rhs=fT[:, kc, th * 512:(th + 1) * 512],
                        start=(kc == 0), stop=(kc == 15)), reads=[Bwq[b]] + BfT, writes=[Bps[bank]])
                P.op("act", lambda e, bank=bank, hp=hp, th=th: e.activation(out=qpT[:, hp, th * 512:(th + 1) * 512], in_=psum[:, bank, :], func=AF.Copy),
                     writes=[Bps[bank], BqpT])
    P.barrier()
    off[0] = baseF
    sc = [T("sc0", [128, 16, 128], F32)]
    m16 = T("m16", [128, 16, 16], F32)
    ix = T("ix", [128, 16, 16], U32)
    ixf = T("ixf", [128, 16, 16], F32)
    cand = T("cand", [128, 8, 256], F32)
    t16 = T("t16", [128, 8, 16], F32)
    pos = T("pos", [128, 8, 16], U32)
    posf = T("posf", [128, 8, 16], F32)
    jf = T("jf", [128, 8, 16], F32)
    iff = T("iff", [128, 8, 16], F32)
    ee = T("ee", [128, 8, 16], F32)
    zs = T("zs", [128, 8], F32)
    sel = [T("sel%d" % i, [128, 3, 128], F32) for i in range(2)]
    selb = [T("selb%d" % i, [128, 2, 128], BF16) for i in range(2)]
    ohs = [T("oh%d" % i, [128, 8, 16, 16], F32) for i in range(2)]
    gT = [T("gT%d" % i, [128, 128], F32) for i in range(2)]
    Em = T("Em", [128, 2, 4096], BF16)
    BEm = Buf()
    P.dma("pool", lambda e: e.dma_start(out=Em[:], in_=emat), writes=[BEm])
    SIb = [T("SIb%d" % i, [128, 4, 128], BF16) for i in range(2)]
    BSI = [Buf(), Buf()]
    for i in range(2):
        P.op("dve", lambda e, i=i: e.memset(SIb[i][:], 1.0), writes=[BSI[i]])
    off[0] = R0
    QT = 32
    PA = [T("PA%d" % i, [128, QT, 128], BF16) for i in range(2)]
    PB = [T("PB%d" % i, [128, QT, 128], BF16) for i in range(2)]
    Gs = T("Gs", [128, 128, 128], BF16)
    Bsc = [Buf()]
    Bseg = [Buf() for _ in range(16)]
    Bch = [Buf() for _ in range(8)]
    Bixf, Bmisc, BGs, Bgate = Buf(), Buf(), Buf(), Buf()
    Boh = [Buf(), Buf()]
    Bsel = [Buf(), Buf()]
    BgT = [Buf(), Buf()]
    BPA = [Buf(), Buf()]
    BPB = [Buf(), Buf()]
    m16v = m16[:].rearrange("p (h s) k -> p h s k", s=2)
    ixfv = ixf[:].rearrange("p (h s) k -> p h s k", s=2)
    candv = cand[:].rearrange("p h (i j) -> p h i j", j=16)
    iota16 = iota_f[:, 0:16]
    thr16 = T("thr16", [128, 16], F32)
    P.op("dve", lambda e: e.tensor_scalar_mul(out=thr16[:], in0=iota16, scalar1=16.0), reads=[Bc], writes=[Bc])
    Gdv = Gd.rearrange("a b t -> b a t")
    qcnt = [0]
    dcnt = [0]
    gcnt = [0]
    OH_SCALE = 1.125
    GATE_FIX = 1.0 / (OH_SCALE * OH_SCALE)

    def scores(tt):
        si = 0
        for c4 in range(4):
            for j in range(4):
                hp = c4 * 4 + j
                P.op("pe", lambda e, j=j, hp=hp: e.matmul(psum[:, 0, j * 128:(j + 1) * 128], lhsT=qpT[:, hp, tt * 128:(tt + 1) * 128],
                                                          rhs=kTb[:, hp, :], start=True, stop=True), reads=[BqpT, BkTb], writes=[Bps[0]])
            P.op("act", lambda e, c4=c4: e.activation(out=sc[si][:, c4 * 4:(c4 + 1) * 4, :], in_=psum[:, 0, :].rearrange("p (j k) -> p j k", k=128), func=AF.Copy),
                 writes=[Bps[0], Bsc[si]])

    def topk(tt):
        si = tt % 2
        scur = sc[0]
        if "sc" in dbg and tt == 0:
            dump("sc", scur[:], [Bsc[0]], [128, 16, 128])
        R16 = range(16)
        for hp in R16:
            P.op("dve", lambda e, hp=hp: e.max(out=m16[:, hp, 0:8], in_=scur[:, hp, :]), reads=[Bsc[0]], writes=[Bseg[hp]])
        for hp in R16:
            P.op("dve", lambda e, hp=hp: e.max_index(out=ix[:, hp, 0:8], in_max=m16[:, hp, 0:8], in_values=scur[:, hp, :]), reads=[Bsc[0]], writes=[Bseg[hp]])
        for hp in R16:
            P.op("dve", lambda e, hp=hp: e.match_replace(out=scur[:, hp, :], in_to_replace=m16[:, hp, 0:8], in_values=scur[:, hp, :], imm_value=-1e30), writes=[Bseg[hp], Bsc[0]])
        for hp in R16:
            P.op("dve", lambda e, hp=hp: e.max(out=m16[:, hp, 8:16], in_=scur[:, hp, :]), reads=[Bsc[0]], writes=[Bseg[hp]])
        for hp in R16:
            P.op("dve", lambda e, hp=hp: e.max_index(out=ix[:, hp, 8:16], in_max=m16[:, hp, 8:16], in_values=scur[:, hp, :]), reads=[Bsc[0]], writes=[Bseg[hp]])
        P.op("dve", lambda e: e.tensor_copy(out=ixf[:], in_=ix[:]), reads=Bseg, writes=[Bixf])
        P.op("dve", lambda e: e.tensor_tensor(out=candv, in0=m16v[:, :, 0, :].unsqueeze(3).to_broadcast([128, 8, 16, 16]),
                                              in1=m16v[:, :, 1, :].unsqueeze(2).to_broadcast([128, 8, 16, 16]), op=ALU.add), reads=Bseg, writes=Bch)
        R8 = range(8)
        for h in R8:
            P.op("dve", lambda e, h=h: e.max(out=t16[:, h, 0:8], in_=cand[:, h, :]), writes=[Bch[h]])
        for h in R8:
            P.op("dve", lambda e, h=h: e.max_index(out=pos[:, h, 0:8], in_max=t16[:, h, 0:8], in_values=cand[:, h, :]), writes=[Bch[h]])
        for h in R8:
            P.op("dve", lambda e, h=h: e.match_replace(out=cand[:, h, :], in_to_replace=t16[:, h, 0:8], in_values=cand[:, h, :], imm_value=-1e30), writes=[Bch[h]])
        for h in R8:
            P.op("dve", lambda e, h=h: e.max(out=t16[:, h, 8:16], in_=cand[:, h, :]), writes=[Bch[h]])
        for h in R8:
            P.op("dve", lambda e, h=h: e.max_index(out=pos[:, h, 8:16], in_max=t16[:, h, 8:16], in_values=cand[:, h, :]), writes=[Bch[h]])
        selc = sel[si]
        P.op("dve", lambda e: e.tensor_copy(out=posf[:], in_=pos[:]), reads=Bch, writes=[Bmisc])
        P.op("dve", lambda e: e.tensor_tensor(
            out=ohs[0][:], in0=posf[:].unsqueeze(3).to_broadcast([128, 8, 16, 16]),
            in1=thr16[:].unsqueeze(1).unsqueeze(1).to_broadcast([128, 8, 16, 16]), op=ALU.is_ge), reads=[Bc, Bmisc], writes=[Boh[0]])
        P.op("dve", lambda e: e.tensor_reduce(out=iff[:], in_=ohs[0][:], axis=AX.X, op=ALU.add), reads=[Boh[0]], writes=[Bmisc])
        P.op("dve", lambda e: e.tensor_scalar_add(out=iff[:], in0=iff[:], scalar1=-1.0), writes=[Bmisc])
        P.op("dve", lambda e: e.scalar_tensor_tensor(out=jf[:], in0=iff[:], scalar=-16.0, in1=posf[:], op0=ALU.mult, op1=ALU.add), writes=[Bmisc])
        sides = ((0, iff), (1, jf))
        for side, selidx in sides:
            P.op("dve", lambda e, side=side, selidx=selidx: e.tensor_tensor(
                out=ohs[side][:], in0=iota16.unsqueeze(1).unsqueeze(1).to_broadcast([128, 8, 16, 16]),
                in1=selidx[:].unsqueeze(3).to_broadcast([128, 8, 16, 16]), op=ALU.is_equal), reads=[Bc, Bmisc], writes=[Boh[side]])
        for side, selidx in sides:
            P.op("dve", lambda e, side=side: e.tensor_tensor(
                out=ohs[side][:], in0=ohs[side][:], in1=ixfv[:, :, side, :].unsqueeze(2).to_broadcast([128, 8, 16, 16]), op=ALU.mult),
                reads=[Bixf], writes=[Boh[side]])
        for side, selidx in sides:
            P.op("dve", lambda e, side=side: e.tensor_reduce(
                out=selc[:, side, :].rearrange("p (h k) -> p h k", k=16), in_=ohs[side][:], axis=AX.X, op=ALU.add),
                reads=[Boh[side]], writes=[Bsel[si]])
        if "sel" in dbg and tt == 0:
            dump("sel", selc[:], [Bsel[si]], [128, 3, 128])
        sb_ = selb[si]
        P.op("dve", lambda e: e.tensor_copy(out=sb_[:], in_=selc[:, 0:2, :]), writes=[Bsel[si]])
        for h2 in range(2):
            for side in range(2):
                P.dma("sp", lambda e, h2=h2, side=side: e.dma_start(out=SIb[si][0:128:2, h2 * 2 + side, :], in_=sb_[64 * h2:64 * h2 + 64, side, :]),
                      reads=[Bsel[si]], writes=[BSI[si]])

    def topk_tail(tt):
        si = tt % 2
        selc = sel[si]
        P.op("dve", lambda e: e.tensor_tensor(out=ee[:], in0=t16[:], in1=t16[:, :, 0:1].to_broadcast([128, 8, 16]), op=ALU.subtract), reads=Bch, writes=[Bgate])
        P.op("act", lambda e: e.activation(out=ee[:], in_=ee[:], func=AF.Exp), writes=[Bgate])
        P.op("dve", lambda e: e.tensor_reduce(out=zs[:], in_=ee[:], axis=AX.X, op=ALU.add), writes=[Bgate])
        P.op("dve", lambda e: e.reciprocal(out=zs[:], in_=zs[:]), writes=[Bgate])
        P.op("dve", lambda e: e.tensor_scalar_mul(out=zs[:], in0=zs[:], scalar1=GATE_FIX), writes=[Bgate])
        P.op("dve", lambda e: e.tensor_tensor(out=selc[:, 2, :].rearrange("p (h k) -> p h k", k=16), in0=ee[:],
                                              in1=zs[:].unsqueeze(2).to_broadcast([128, 8, 16]), op=ALU.mult), reads=[Bgate], writes=[Bsel[si]])
        P.op("pe", lambda e: e.transpose(out=psum[:, 0, 0:128], in_=selc[:, 2, :], identity=identf), reads=[Bsel[si], Bc], writes=[Bps[0]])
        gTc = gT[si]
        P.op("dve", lambda e: e.tensor_copy(out=gTc[:], in_=psum[:, 0, 0:128]), writes=[Bps[0], BgT[si]])

    def stages(tt):
        si = tt % 2
        gTc = gT[si]
        def dstage(q):
            tb = q * QT
            h2, qq = q // 2, q % 2
            pi = qcnt[0] % 2
            qcnt[0] += 1
            for side, PX, BPX in ((0, PA, BPA), (1, PB, BPB)):
                for p in range(4):
                    bk = 4 + 2 * (dcnt[0] % 2)
                    dcnt[0] += 1
                    for j in range(2):
                        c0 = p * 1024 + j * 512
                        P.op("pe", lambda e, side=side, bk=bk, j=j, c0=c0: e.matmul(
                            psum[:, bk + j, :], lhsT=SIb[si][:, h2 * 2 + side, :], rhs=Em[:, qq, c0:c0 + 512],
                            start=True, stop=True), reads=[BSI[si], BEm], writes=[Bps[bk + j]])
                    P.op("act", lambda e, PX=PX, p=p, bk=bk: e.activation(
                        out=PX[pi][:, p * 8:(p + 1) * 8, :].rearrange("p (j u) a -> p j (u a)", j=2), in_=psum[:, bk:bk + 2, :],
                        func=AF.Derivative_Erf, scale=4.0), writes=[Bps[bk], Bps[bk + 1], BPX[pi]])
            P.op("pool", lambda e: e.tensor_tensor(
                out=PA[pi][:], in0=PA[pi][:], in1=gTc[:, tb:tb + QT].unsqueeze(2).to_broadcast([128, QT, 128]), op=ALU.mult), reads=[BgT[si]], writes=[BPA[pi]])
            return pi

        def gstage(q, pi):
            tb = q * QT
            for t4 in range(QT // 4):
                bank = 1 + (gcnt[0] % 3)
                gcnt[0] += 1
                for u in range(4):
                    tl = t4 * 4 + u
                    P.op("pe", lambda e, bank=bank, u=u, tl=tl: e.matmul(psum[:, bank, u * 128:(u + 1) * 128], lhsT=PB[pi][:, tl, :], rhs=PA[pi][:, tl, :],
                                                                      start=True, stop=True), reads=[BPA[pi], BPB[pi]], writes=[Bps[bank]])
                tg = tb + t4 * 4
                P.op("act", lambda e, bank=bank, tg=tg: e.activation(
                    out=Gs[:, :, tg:tg + 4], in_=psum[:, bank, :].rearrange("p (t a) -> p a t", a=128), func=AF.Copy),
                    writes=[Bps[bank], BGs])

        NQ4 = 128 // QT
        pis = [dstage(0)]
        for q in range(NQ4):
            if q + 1 < NQ4:
                pis.append(dstage(q + 1))
            gstage(q, pis[q])
        for a4 in range(4):
            P.dma("sp", lambda e, a4=a4: e.dma_start(out=Gdv[:, a4 * 32:(a4 + 1) * 32, tt * 128:(tt + 1) * 128], in_=Gs[:, a4 * 32:(a4 + 1) * 32, :]),
                  reads=[BGs])

    scores(0)
    topk(0)
    topk_tail(0)
    scores(1)
    topk(1)
    for tt in range(8):
        stages(tt)
        if tt + 2 < 8:
            scores(tt + 2)
        if tt + 1 < 8:
            topk_tail(tt + 1)
        if tt + 2 < 8:
            topk(tt + 2)
    P.barrier()
    if stop == "F":
        if "G" in dbg:
            tmpg = T("tmpg", [128, 2, NQ], BF16)
            bb = Buf()
            P.dma("sp", lambda e: e.dma_start(out=tmpg[:], in_=Gdv[:, 0:2, :]), writes=[bb])
            dump("G", tmpg[:], [bb], [128, 2, NQ], BF16)
        return end()

    P.dma("sp", lambda e: e.dma_start(out=x1T[:], in_=x1_s), writes=allx1)
    off[0] = R2
    ut = [T("ut%d" % i, [128, 16, 256], BF16) for i in range(3)]
    vt = [T("vt%d" % i, [128, 4, D], BF16) for i in range(2)]
    gt = [T("gt%d" % i, [128, NQ], BF16) for i in range(3)]
    ga = [T("ga%d" % i, [128, NQ], BF16) for i in range(2)]
    AT = [T("AT%d" % i, [128, 4, NQ], BF16) for i in range(2)]
    But, Bvt, Bgt, Bga = [Buf(), Buf(), Buf()], [Buf(), Buf()], [Buf(), Buf(), Buf()], [Buf(), Buf()]
    BAT = [[Buf() for _ in range(4)] for _ in range(2)]
    uTv = uT.rearrange("(kc p) e -> p kc e", p=128)
    pvv = pv.rearrange("(c p) d -> p c d", p=128)
    NS = NE // 512

    def load_ut(cp):
        if cp < NE // 256:
            P.dma("pool", lambda e: e.dma_start(out=ut[cp % 3][:], in_=uTv[:, :, cp * 256:(cp + 1) * 256]), writes=[But[cp % 3]])

    load_ut(0)
    load_ut(1)

    def p1(s):
        si = s % 2
        for c in range(4):
            a = 4 * s + c
            cp = a // 2
            ui = cp % 3
            if a % 2 == 0:
                load_ut(cp + 2)
            if c == 3:
                P.dma("pool", lambda e: e.dma_start(out=vt[si][:], in_=pvv[:, 4 * s:4 * s + 4, :]), writes=[Bvt[si]])
            gi = a % 3
            P.dma("sp", lambda e, a=a, gi=gi: e.dma_start(out=gt[gi][:], in_=Gd[a]), writes=[Bgt[gi]])
            for th in range(2):
                bank = (a % 2) * 2 + th
                for kc in range(16):
                    P.op("pe", lambda e, kc=kc, bank=bank, ui=ui, a=a, th=th: e.matmul(
                        psum[:, bank, :], lhsT=ut[ui][:, kc, (a % 2) * 128:(a % 2) * 128 + 128], rhs=fT[:, kc, th * 512:(th + 1) * 512],
                        start=(kc == 0), stop=(kc == 15)), reads=[But[ui]] + BfT, writes=[Bps[bank]])
            gi2 = a % 2
            for th in range(2):
                bank = (a % 2) * 2 + th
                P.op("act", lambda e, bank=bank, gi2=gi2, th=th: e.activation(out=ga[gi2][:, th * 512:(th + 1) * 512], in_=psum[:, bank, :], func=AF.Gelu),
                     writes=[Bps[bank], Bga[gi2]])
            P.op("dve", lambda e, gi=gi, gi2=gi2, c=c, si=si: e.tensor_tensor(out=AT[si][:, c, :], in0=ga[gi2][:], in1=gt[gi][:], op=ALU.mult),
                 reads=[Bga[gi2], Bgt[gi]], writes=[BAT[si][c]])

    acnt = [0]

    def p2(s):
        si = s % 2
        for th in range(2):
            for dp in range(8):
                pair = acnt[0] % 2
                acnt[0] += 1
                for c in range(4):
                    for u in range(2):
                        dc = dp * 2 + u
                        bank = 4 + pair * 2 + u
                        P.op("pe", lambda e, c=c, dc=dc, bank=bank, th=th: e.matmul(
                            psum[:, bank, :], lhsT=vt[si][:, c, dc * 128:(dc + 1) * 128], rhs=AT[si][:, c, th * 512:(th + 1) * 512],
                            start=(c == 0), stop=(c == 3)), reads=[Bvt[si], BAT[si][c]], writes=[Bps[bank]])
                for u in range(2):
                    dc = dp * 2 + u
                    bank = 4 + pair * 2 + u
                    P.op("dve", lambda e, dc=dc, bank=bank, th=th: e.scalar_tensor_tensor(
                        out=x1T[:, dc, th * 512:(th + 1) * 512], in0=psum[:, bank, :], scalar=g2[:, dc:dc + 1],
                        in1=x1T[:, dc, th * 512:(th + 1) * 512], op0=ALU.mult, op1=ALU.add), reads=[Bc], writes=[Bps[bank], Bx1[dc][th]])

    nsuper = NS if stop != "G1" else 2
    p1(0)
    for s in range(nsuper):
        if s + 1 < nsuper:
            p1(s + 1)
        p2(s)
    if "x2" in dbg:
        dump("x2", x1T[:], allx1, [128, 16, NQ])
    P.barrier()

    off[0] = R2
    sq = T("sq3", [128, 16, 512], BF16)
    rstd = T("rstd3", [128, 1024], F32)
    ob = [T("ob%d" % i, [128, 16, 512], F32) for i in range(1)]
    Bob = Buf()
    outTv = outT.rearrange("(kc p) t -> p kc t", p=128)
    for th in range(2):
        rms_stats(x1T[:, :, th * 512:(th + 1) * 512], allx1, 512, th, rstd[:, th * 512:(th + 1) * 512])
        for kc in range(16):
            P.op("dve", lambda e, kc=kc, th=th: e.scalar_tensor_tensor(
                out=ob[0][:, kc, :], in0=x1T[:, kc, th * 512:(th + 1) * 512], scalar=fg[:, kc:kc + 1], in1=rstd[:, th * 512:(th + 1) * 512],
                op0=ALU.mult, op1=ALU.mult), reads=[Brstd, Bc] + allx1, writes=[Bob])
        P.dma("sp", lambda e, th=th: e.dma_start(out=outTv[:, :, th * 512:(th + 1) * 512], in_=ob[0][:]), reads=[Bob])
    return end()


def _rope_tables(pos):
    inv = (10000.0 ** (-np.arange(16, dtype=np.float32) / 16)).astype(np.float32)
    row = (pos // 64).astype(np.float32)
    col = (pos % 64).astype(np.float32)
    C = np.zeros((128, 2048), np.float32)
    S = np.zeros((128, 2048), np.float32)
    for p in range(128):
        j = p % 64
        axis, r = j // 32, j % 32
        hf, i = r // 16, r % 16
        ang = (row if axis == 0 else col) * inv[i]
        C[p] = np.cos(ang)
        S[p] = np.sin(ang) * (-1.0 if hf == 0 else 1.0)
    return C, S


def _emat():
    e = np.zeros((64, 2, 2, 32, 128), np.float32)
    bb = np.arange(128, dtype=np.float32)
    for tp in range(64):
        q, t = tp // 32, tp % 32
        e[tp, 0, q, t, :] = 1.0
        e[tp, 1, q, t, :] = -bb
    return np.ascontiguousarray(e.reshape(128, 2, 4096))


def _core_inputs(k, I, shared):
    b, half = k // 2, k % 2
    own = slice(half * 1024, half * 1024 + 1024)
    oth = slice((1 - half) * 1024, (1 - half) * 1024 + 1024)
    x = I["x"][b]
    xT = np.ascontiguousarray(np.concatenate([x[own], x[oth], I["ctx"][b]], 0).T)
    cv = np.stack([I["c"][b].reshape(16, 128).T, I["c_ctx"].reshape(16, 128).T], -1).reshape(128, 32)
    pos = np.concatenate([np.arange(2048)[own], np.arange(2048)[oth]])
    C, S = _rope_tables(pos)
    t = np.arange(2048)
    ic = np.zeros((4, 1024), np.float32)
    for g, w in enumerate((2, 4, 8, 16)):
        lo = np.clip(t - w // 2, 0, 2048)
        hi = np.clip(t + w // 2, 0, 2048)
        ic[g] = (1.0 / (hi - lo).astype(np.float32))[own]
    halo = np.array([1.0 if half == 1 else 0.0, 1.0 if half == 0 else 0.0], np.float32)
    smallv = np.concatenate([shared["smallv"], np.broadcast_to(halo, (128, 2))], 1)
    d = dict(shared["common"])
    d.update(xT=xT, cvec=np.ascontiguousarray(cv, np.float32), smallv=np.ascontiguousarray(smallv, np.float32),
             ropeC=C, ropeS=S, icnt=np.ascontiguousarray(np.broadcast_to(ic.reshape(1, 4096), (128, 4096))))
    return d


def _shared(I):
    f = lambda a: np.ascontiguousarray(a, dtype=np.float32)
    pc = lambda v, n: v.reshape(n, 128).T
    smallv = np.concatenate([pc(I["ada_b"][0], 96), pc(I["norm1_g"][0], 16), pc(I["norm2_g"][0], 16), pc(I["final_g"], 16),
                             I["pool_b"][0].T, pc(I["pool_scale"][0], 4)], 1)
    pm = np.zeros((128, 128), np.float32)
    for m in range(128):
        r = (m % 64) % 32
        pm[m + 16 if r < 16 else m - 16, m] = 1.0
    cmat = np.concatenate([np.eye(128, dtype=np.float32), pm], 1)
    common = dict(
        ada_w=f(I["ada_w"][0]), w_in=f(I["w_in"][0]), pool_w=f(I["pool_w"][0]),
        dlam=f(np.broadcast_to(I["diff_lambda"][0].reshape(1, 256), (128, 256))),
        subg=f(np.broadcast_to(I["subln_g"][0].reshape(1, 128), (128, 128))),
        w_out=f(I["w_out"][0]), wq=f(I["peer_wq"][0]),
        keysT=f(I["peer_keys"][0].reshape(16, 128, 128).transpose(0, 2, 1)),
        uT=f(I["peer_u"][0].T), pv=f(I["peer_v"][0]), cmat=f(cmat), emat=_emat())
    return dict(smallv=f(smallv), common=common)


_NC = None


def kernel(**inputs):
    global _NC
    I = {k: np.asarray(v) for k, v in inputs.items()}
    if _NC is None:
        _NC = build()[0]
    shared = _shared(I)
    in_maps = [_core_inputs(k, I, shared) for k in range(8)]
    res = run_bass_kernel_spmd(_NC, in_maps, core_ids=list(range(8)))
    out = np.empty((4, 2048, 2048), np.float32)
    for k in range(8):
        b, half = k // 2, k % 2
        out[b, half * 1024:(half + 1) * 1024, :] = res.results[k]["outT"].T
    return out
```
